# Optimizing a Trainium2 kernel written in Bass

```python
import math
import jax, jax.numpy as jnp
from jax import lax
import numpy as np

D_MODEL = 1024
BATCH = 8
SEQ = 8192
DEPTH = 4

CHUNK = 64
N_MIXERS = 3
EPS = 1e-6
GLA_HEADS = 4
GLA_DK = D_MODEL // 2
GLA_DV = D_MODEL
GLA_GATE_RANK = 16
GLA_GATE_NORM = 16.0
MLSTM_HEADS = 4
MLSTM_INNER = 2 * D_MODEL
MLSTM_CONV = 4
MLSTM_QK_BLOCK = 4
S5_GROUP = 16
S5_GROUPS = D_MODEL // S5_GROUP
S5_STATE = 64
N_EXPERTS = 32
TOP_K = 4
D_FF = D_MODEL
SWIGLU_LIMIT = 7.0
SWIGLU_ALPHA = 1.702
N_LAYERS_A = (DEPTH + 2) // 3
N_LAYERS_B = (DEPTH + 1) // 3
N_LAYERS_C = DEPTH // 3
N_KEYS = 64

kernel_name = "chunk_causal_hybrid_gla_mlstm_s5_moe"


def _rmsnorm(x, g):
    x32 = x.astype(jnp.float32)
    y = x32 * lax.rsqrt(jnp.mean(x32 * x32, axis=-1, keepdims=True) + EPS)
    return (y * g.astype(jnp.float32)).astype(x.dtype)


def _to_chunks(t):
    b, l, h, d = t.shape
    return t.reshape(b, l // CHUNK, CHUNK, h, d).transpose(1, 0, 3, 2, 4)


def _from_chunks(t):
    nc, b, h, c, d = t.shape
    return t.transpose(1, 0, 3, 2, 4).reshape(b, nc * c, h * d)


def gla_mixer(h, w_in, w_gk1, w_gk2, b_gk, g_onorm, w_out):
    b, l, _ = h.shape
    dk = GLA_DK // GLA_HEADS
    dv = GLA_DV // GLA_HEADS
    proj = (h @ w_in).astype(jnp.float32)
    q, k, v, g = jnp.split(proj, [GLA_DK, 2 * GLA_DK, 2 * GLA_DK + GLA_DV], axis=-1)
    gk = ((h @ w_gk1) @ w_gk2 + b_gk).astype(jnp.float32)
    log_a = jax.nn.log_sigmoid(gk) / GLA_GATE_NORM
    heads = lambda t: _to_chunks(t.reshape(b, l, GLA_HEADS, -1))

    def step(state, inp):
        qc, kc, vc, ac = inp
        cum = jnp.cumsum(ac, axis=2)
        tot = cum[:, :, -1:, :]
        k_dec = kc * jnp.exp(tot - cum)
        state = jnp.exp(tot)[:, :, 0, :, None] * state + jnp.einsum('bhck,bhcv->bhkv', k_dec, vc)
        return state, jnp.einsum('bhck,bhkv->bhcv', qc, state)

    s0 = jnp.zeros((b, GLA_HEADS, dk, dv), jnp.float32)
    _, o = lax.scan(step, s0, (heads(q * dk ** -0.5), heads(k), heads(v), heads(log_a)))
    o = o * lax.rsqrt(jnp.mean(o * o, axis=-1, keepdims=True) + EPS) * g_onorm.astype(jnp.float32)
    o = _from_chunks(o) * jax.nn.silu(g)
    return o.astype(h.dtype) @ w_out


def _blockdiag(t, w):
    b, l, _ = t.shape
    nb, bs, _ = w.shape
    return jnp.einsum('blni,nio->blno', t.reshape(b, l, nb, bs), w).reshape(b, l, nb * bs)


def mlstm_mixer(h, w_up, conv_w, conv_b, w_q, w_k, w_v, w_if, b_if, skip, g_norm, w_down):
    b, l, _ = h.shape
    dh = MLSTM_INNER // MLSTM_HEADS
    xm, z = jnp.split(h @ w_up, 2, axis=-1)
    xc = lax.conv_general_dilated(xm, conv_w[:, None, :], window_strides=(1,),
                                  padding=[(MLSTM_CONV - 1, 0)],
                                  dimension_numbers=('NWC', 'WIO', 'NWC'),
                                  feature_group_count=MLSTM_INNER) + conv_b
    xc = jax.nn.silu(xc)
    q = _blockdiag(xc, w_q).astype(jnp.float32)
    k = _blockdiag(xc, w_k).astype(jnp.float32)
    v = _blockdiag(xm, w_v).astype(jnp.float32)
    gates = (jnp.concatenate([q, k, v], axis=-1) @ w_if + b_if).astype(jnp.float32)
    i_pre, f_pre = jnp.split(gates, 2, axis=-1)
    log_f = jax.nn.log_sigmoid(f_pre)
    heads = lambda t: _to_chunks(t.reshape(b, l, MLSTM_HEADS, -1))
    gate_chunks = lambda t: _to_chunks(t[..., None])[..., 0]

    def step(carry, inp):
        cmat, nvec, m = carry
        qc, kc, vc, ic, fc = inp
        cum = jnp.cumsum(fc, axis=-1)
        tot = cum[..., -1]
        logw = tot[..., None] - cum + ic
        m_new = jnp.maximum(tot + m, jnp.max(logw, axis=-1))
        decay = jnp.exp(tot + m - m_new)
        kw = kc * jnp.exp(logw - m_new[..., None])[..., None]
        cmat = decay[..., None, None] * cmat + jnp.einsum('bhck,bhcv->bhkv', kw, vc)
        nvec = decay[..., None] * nvec + jnp.sum(kw, axis=2)
        num = jnp.einsum('bhck,bhkv->bhcv', qc, cmat)
        den = jnp.maximum(jnp.abs(jnp.einsum('bhck,bhk->bhc', qc, nvec)),
                          jnp.exp(-m_new)[..., None])
        return (cmat, nvec, m_new), num / den[..., None]

    carry0 = (jnp.zeros((b, MLSTM_HEADS, dh, dh), jnp.float32),
              jnp.zeros((b, MLSTM_HEADS, dh), jnp.float32),
              jnp.zeros((b, MLSTM_HEADS), jnp.float32))
    _, hc = lax.scan(step, carry0, (heads(q), heads(k * dh ** -0.5), heads(v),
                                    gate_chunks(i_pre), gate_chunks(log_f)))
    mu = jnp.mean(hc, axis=-1, keepdims=True)
    var = jnp.var(hc, axis=-1, keepdims=True)
    hn = _from_chunks((hc - mu) * lax.rsqrt(var + EPS)) * g_norm.astype(jnp.float32)
    out = (hn + skip.astype(jnp.float32) * xc.astype(jnp.float32)) * jax.nn.silu(z.astype(jnp.float32))
    return out.astype(h.dtype) @ w_down


def s5_mixer(h, w_in, a_re, a_im, log_dt, b_re, b_im, c_re, c_im, d_skip, w_out):
    b, l, _ = h.shape
    u = (h @ w_in).astype(jnp.float32).reshape(b, l, S5_GROUPS, S5_GROUP)
    a = lax.complex(a_re.astype(jnp.float32), a_im.astype(jnp.float32))
    dt = jnp.exp(log_dt.astype(jnp.float32))[:, None]
    a_bar = jnp.exp(a * dt)
    b_mat = lax.complex(b_re.astype(jnp.float32), b_im.astype(jnp.float32))
    b_bar = ((a_bar - 1.0) / a)[..., None] * b_mat
    bu = jnp.einsum('blgc,gpc->blgp', u.astype(jnp.complex64), b_bar)
    a_seq = jnp.broadcast_to(a_bar, (1, l) + a_bar.shape)

    def combine(e1, e2):
        a1, s1 = e1
        a2, s2 = e2
        return a1 * a2, a2 * s1 + s2

    _, states = lax.associative_scan(combine, (a_seq, bu), axis=1)
    c_mat = lax.complex(c_re.astype(jnp.float32), c_im.astype(jnp.float32))
    y = jnp.einsum('blgp,gcp->blgc', states, c_mat).real \
        + d_skip.astype(jnp.float32).reshape(S5_GROUPS, S5_GROUP) * u
    y = jax.nn.gelu(y.reshape(b, l, D_MODEL)).astype(h.dtype)
    glu = y @ w_out
    return glu[..., :D_MODEL] * jax.nn.sigmoid(glu[..., D_MODEL:])


def moe_ffn(h, w_router, b_router, w_gu, b_gu, w_down, b_down):
    b, l, d = h.shape
    t = h.reshape(-1, d)
    logits = (t @ w_router).astype(jnp.float32) + b_router.astype(jnp.float32)
    top_v, top_i = lax.top_k(logits, TOP_K)
    top_w = jax.nn.softmax(top_v, axis=-1)
    comb = jnp.einsum('tk,tke->te', top_w,
                      jax.nn.one_hot(top_i, N_EXPERTS, dtype=jnp.float32)).astype(h.dtype)
    out = jnp.zeros_like(t)
    for e in range(N_EXPERTS):
        gu = t @ w_gu[e] + b_gu[e]
        gate = jnp.minimum(gu[:, :D_FF], SWIGLU_LIMIT)
        up = jnp.clip(gu[:, D_FF:], -SWIGLU_LIMIT, SWIGLU_LIMIT)
        act = (up + 1.0) * gate * jax.nn.sigmoid(SWIGLU_ALPHA * gate)
        out = out + comb[:, e:e + 1] * (act @ w_down[e] + b_down[e])
    return out.reshape(b, l, d)


def setup_inputs(seed: int = 0) -> dict:
    key = jax.random.key(seed)
    ks = iter(jax.random.split(key, N_KEYS))
    f32 = jnp.float32
    nrm = lambda shape, std: std * jax.random.normal(next(ks), shape, f32)
    gain = lambda shape: 1.0 + nrm(shape, 0.02)
    D, NA, NB, NC, H = D_MODEL, N_LAYERS_A, N_LAYERS_B, N_LAYERS_C, MLSTM_HEADS
    inp = {}
    inp['x'] = nrm((BATCH, SEQ, D), 1.0)
    inp['c'] = nrm((BATCH, D), 1.0)
    inp['g_mix'] = gain((DEPTH, D))
    inp['g_ffn'] = gain((DEPTH, D))
    inp['w_ada'] = nrm((DEPTH, D, 6 * D), 0.01)
    inp['b_ada'] = nrm((DEPTH, 6 * D), 0.02)
    inp['gla_w_in'] = nrm((NA, D, 2 * GLA_DK + 2 * GLA_DV), D ** -0.5)
    inp['gla_w_gk1'] = nrm((NA, D, GLA_GATE_RANK), D ** -0.5)
    inp['gla_w_gk2'] = nrm((NA, GLA_GATE_RANK, GLA_DK), GLA_GATE_RANK ** -0.5)
    inp['gla_b_gk'] = nrm((NA, GLA_DK), 0.1)
    inp['gla_g_onorm'] = gain((NA, GLA_DV // GLA_HEADS))
    inp['gla_w_out'] = nrm((NA, GLA_DV, D), GLA_DV ** -0.5)
    nblk = MLSTM_INNER // MLSTM_QK_BLOCK
    inp['ml_w_up'] = nrm((NB, D, 2 * MLSTM_INNER), D ** -0.5)
    inp['ml_conv_w'] = nrm((NB, MLSTM_CONV, MLSTM_INNER), MLSTM_CONV ** -0.5)
    inp['ml_conv_b'] = nrm((NB, MLSTM_INNER), 0.01)
    inp['ml_w_q'] = nrm((NB, nblk, MLSTM_QK_BLOCK, MLSTM_QK_BLOCK), MLSTM_QK_BLOCK ** -0.5)
    inp['ml_w_k'] = nrm((NB, nblk, MLSTM_QK_BLOCK, MLSTM_QK_BLOCK), MLSTM_QK_BLOCK ** -0.5)
    inp['ml_w_v'] = nrm((NB, nblk, MLSTM_QK_BLOCK, MLSTM_QK_BLOCK), MLSTM_QK_BLOCK ** -0.5)
    inp['ml_w_if'] = nrm((NB, 3 * MLSTM_INNER, 2 * H), 0.1 * (3 * MLSTM_INNER) ** -0.5)
    f_bias = jnp.broadcast_to(jnp.linspace(3.0, 6.0, H, dtype=f32), (NB, H))
    inp['ml_b_if'] = jnp.concatenate([nrm((NB, H), 0.1), f_bias + nrm((NB, H), 0.1)], axis=-1)
    inp['ml_skip'] = gain((NB, MLSTM_INNER))
    inp['ml_g_norm'] = gain((NB, MLSTM_INNER))
    inp['ml_w_down'] = nrm((NB, MLSTM_INNER, D), MLSTM_INNER ** -0.5)
    G, P = S5_GROUPS, S5_STATE
    inp['s5_w_in'] = nrm((NC, D, D), D ** -0.5)
    inp['s5_a_re'] = -0.5 + nrm((NC, G, P), 0.01)
    inp['s5_a_im'] = math.pi * jnp.broadcast_to(jnp.arange(P, dtype=f32), (NC, G, P)) + nrm((NC, G, P), 0.01)
    inp['s5_log_dt'] = jax.random.uniform(next(ks), (NC, G), f32, math.log(0.001), math.log(0.1))
    inp['s5_b_re'] = nrm((NC, G, P, S5_GROUP), (2.0 * S5_GROUP) ** -0.5)
    inp['s5_b_im'] = nrm((NC, G, P, S5_GROUP), (2.0 * S5_GROUP) ** -0.5)
    inp['s5_c_re'] = nrm((NC, G, S5_GROUP, P), (2.0 * P) ** -0.5)
    inp['s5_c_im'] = nrm((NC, G, S5_GROUP, P), (2.0 * P) ** -0.5)
    inp['s5_d'] = nrm((NC, D), 1.0)
    inp['s5_w_out'] = nrm((NC, D, 2 * D), D ** -0.5)
    inp['moe_w_router'] = nrm((DEPTH, D, N_EXPERTS), D ** -0.5)
    inp['moe_b_router'] = nrm((DEPTH, N_EXPERTS), 0.01)
    inp['moe_w_gu'] = nrm((DEPTH, N_EXPERTS, D, 2 * D_FF), D ** -0.5)
    inp['moe_b_gu'] = nrm((DEPTH, N_EXPERTS, 2 * D_FF), 0.01)
    inp['moe_w_down'] = nrm((DEPTH, N_EXPERTS, D_FF, D), D_FF ** -0.5)
    inp['moe_b_down'] = nrm((DEPTH, N_EXPERTS, D), 0.01)
    inp['g_final'] = gain((D,))
    return inp


def reference(x, c, g_mix, g_ffn, w_ada, b_ada,
              gla_w_in, gla_w_gk1, gla_w_gk2, gla_b_gk, gla_g_onorm, gla_w_out,
              ml_w_up, ml_conv_w, ml_conv_b, ml_w_q, ml_w_k, ml_w_v, ml_w_if, ml_b_if,
              ml_skip, ml_g_norm, ml_w_down,
              s5_w_in, s5_a_re, s5_a_im, s5_log_dt, s5_b_re, s5_b_im, s5_c_re, s5_c_im,
              s5_d, s5_w_out,
              moe_w_router, moe_b_router, moe_w_gu, moe_b_gu, moe_w_down, moe_b_down,
              g_final):
    c_act = jax.nn.silu(c)
    for i in range(DEPTH):
        mod = (c_act @ w_ada[i] + b_ada[i])[:, None, :]
        sh1, sc1, gt1, sh2, sc2, gt2 = jnp.split(mod, 6, axis=-1)
        h = _rmsnorm(x, g_mix[i]) * (1.0 + sc1) + sh1
        kind, j = i % N_MIXERS, i // N_MIXERS
        if kind == 0:
            y = gla_mixer(h, gla_w_in[j], gla_w_gk1[j], gla_w_gk2[j], gla_b_gk[j],
                          gla_g_onorm[j], gla_w_out[j])
        elif kind == 1:
            y = mlstm_mixer(h, ml_w_up[j], ml_conv_w[j], ml_conv_b[j], ml_w_q[j], ml_w_k[j],
                            ml_w_v[j], ml_w_if[j], ml_b_if[j], ml_skip[j], ml_g_norm[j],
                            ml_w_down[j])
        else:
            y = s5_mixer(h, s5_w_in[j], s5_a_re[j], s5_a_im[j], s5_log_dt[j], s5_b_re[j],
                         s5_b_im[j], s5_c_re[j], s5_c_im[j], s5_d[j], s5_w_out[j])
        x = x + gt1 * y
        h = _rmsnorm(x, g_ffn[i]) * (1.0 + sc2) + sh2
        x = x + gt2 * moe_ffn(h, moe_w_router[i], moe_b_router[i], moe_w_gu[i], moe_b_gu[i],
                              moe_w_down[i], moe_b_down[i])
    return _rmsnorm(x, g_final)
```

```python
import numpy as np
import concourse.bass as bass
import concourse.mybir as mybir

F32 = mybir.dt.float32
BF16 = mybir.dt.bfloat16
AF = mybir.ActivationFunctionType
ALU = mybir.AluOpType
AX = mybir.AxisListType

SAME_ENGINE_SYNC = True


class V:
    __slots__ = ("b", "ap")

    def __init__(self, b, ap):
        self.b = b
        self.ap = ap


class Buf:
    def __init__(self, k, t, kind):
        self.k = k
        self.t = t
        self.kind = kind
        self.w = None
        self.r = {}
        self.dsem = None
        self.dcnt = 0

    def __getitem__(self, idx):
        return V(self, self.t[idx])

    def v(self, ap):
        return V(self, ap)


class K:
    def __init__(self, nc):
        self.nc = nc
        self.E = dict(pe=nc.tensor, act=nc.scalar, dve=nc.vector, pool=nc.gpsimd, sp=nc.sync)
        self.sem = {k: nc.alloc_semaphore("sem_" + k) for k in self.E}
        self.cnt = {k: 0 for k in self.E}
        self.waited = {k: {} for k in self.E}
        self.semowner = {}
        self.n = 0
        self.ninstr = 0
        self.guards = []
        self.stage_bufs = []
        self.dsem_pool = []
        self.all_dsem_bufs = []

    def sb(self, name, shape, dt=F32):
        self.n += 1
        name = "%s_%d" % (name, self.n)
        if self.guards:
            g = self.nc.sbuf_tensor(name, list(shape), dt)
            t = g.__enter__()
            self.guards[-1].append(g)
        else:
            t = self.nc.alloc_sbuf_tensor(name, list(shape), dt)
        b = Buf(self, t, "sb")
        if self.stage_bufs:
            self.stage_bufs[-1].append(b)
        return b

    def ps(self, name, shape, dt=F32):
        self.n += 1
        name = "%s_%d" % (name, self.n)
        if self.guards:
            g = self.nc.psum_tensor(name, list(shape), dt)
            t = g.__enter__()
            self.guards[-1].append(g)
        else:
            t = self.nc.alloc_psum_tensor(name, list(shape), dt)
        b = Buf(self, t, "ps")
        if self.stage_bufs:
            self.stage_bufs[-1].append(b)
        return b

    def stage_begin(self):
        self.guards.append([])
        self.stage_bufs.append([])

    def stage_end(self):
        self.barrier()
        for b in self.stage_bufs.pop():
            if b.dsem is not None:
                self.dsem_pool.append((b.dsem, b.dcnt))
                b.dsem = None
        for g in reversed(self.guards.pop()):
            g.__exit__(None, None, None)

    def barrier(self):
        evs = [(self.sem[e], self.cnt[e], e) for e in self.E if self.cnt[e] > 0]
        dmas = [(b.dsem, 16 * b.dcnt, "dma") for b in self.all_dsem_bufs if b.dsem is not None and b.dcnt > 0]
        for e in self.E:
            for ev in evs:
                if ev[2] != e:
                    self._wait(e, ev)
            for ev in dmas:
                self._wait(e, ev)

    def dram(self, name, shape, dt=F32, kind="Internal"):
        return Buf(self, self.nc.dram_tensor(name, list(shape), dt, kind=kind).ap(), "dram")

    def region(self):
        return Buf(self, None, "dram")

    def _wait(self, e, ev):
        if ev is None:
            return
        sem, val, src = ev
        if src == e and (e == "pe" or not SAME_ENGINE_SYNC):
            return
        if src == "dma":
            val = 16 * self.semowner[id(sem)].dcnt
        w = self.waited[e]
        key = id(sem)
        if w.get(key, 0) >= val:
            return
        self.E[e].wait_ge(sem, val)
        w[key] = val

    def _deps(self, e, reads, writes):
        for b in reads:
            self._wait(e, b.w)
            if b.kind == "ps":
                for ke, ev in b.r.items():
                    if ke != e:
                        self._wait(e, ev)
        for b in writes:
            self._wait(e, b.w)
            for ev in b.r.values():
                self._wait(e, ev)

    def _commit(self, e, ins, reads, writes):
        self.cnt[e] += 1
        ev = (self.sem[e], self.cnt[e], e)
        ins.then_inc(self.sem[e], 1)
        for b in writes:
            b.w = ev
            b.r = {}
        ws = set(id(b) for b in writes)
        for b in reads:
            if id(b) not in ws:
                b.r[e] = ev
        self.ninstr += 1

    @staticmethod
    def _split(ops):
        bufs, aps = [], []
        for o in ops:
            if isinstance(o, V):
                bufs.append(o.b)
                aps.append(o.ap)
            else:
                aps.append(o)
        return bufs, aps

    def op(self, e, name, out, ins, extra_reads=(), **kw):
        rb, raps = self._split(ins)
        rb = rb + [x.b for x in extra_reads]
        kwv = {}
        wb = [out.b]
        for kk, vv in kw.items():
            if isinstance(vv, V):
                if kk == "accum_out":
                    wb.append(vv.b)
                else:
                    rb.append(vv.b)
                kwv[kk] = vv.ap
            else:
                kwv[kk] = vv
        self._deps(e, rb, wb)
        ins_ = getattr(self.E[e], name)(out.ap, *raps, **kwv)
        self._commit(e, ins_, rb, wb)
        return ins_

    def act(self, out, in_, func, bias=None, scale=None, e="act", accum_out=None):
        kw = {}
        if bias is not None:
            kw["bias"] = bias
        if scale is not None:
            kw["scale"] = scale
        rb = [in_.b]
        kwv = {}
        for kk, vv in kw.items():
            if isinstance(vv, V):
                rb.append(vv.b)
                kwv[kk] = vv.ap
            else:
                kwv[kk] = vv
        wb = [out.b]
        if accum_out is not None:
            wb.append(accum_out.b)
            kwv["accum_out"] = accum_out.ap
        self._deps("act", rb, wb)
        ins_ = self.nc.scalar.activation(out=out.ap, in_=in_.ap, func=func, **kwv)
        self._commit("act", ins_, rb, wb)

    def ts(self, e, out, in0, s1, s2, op0, op1=None):
        if op1 is None:
            return self.op(e, "tensor_scalar", out, [in0, s1, None, op0])
        return self.op(e, "tensor_scalar", out, [in0, s1, s2, op0, op1])

    def stt(self, out, in0, scalar, in1, op0, op1, e="dve", **kw):
        return self.op(e, "scalar_tensor_tensor", out, [in0, scalar, in1, op0, op1], **kw)

    def tt(self, e, out, in0, in1, op):
        return self.op(e, "tensor_tensor", out, [in0, in1, op])

    def copy(self, e, out, in_):
        if e == "act":
            return self.act(out, in_, AF.Copy)
        return self.op(e, "tensor_copy", out, [in_])

    def memset(self, e, out, val):
        self._deps(e, [], [out.b])
        ins_ = self.E[e].memset(out.ap, val)
        self._commit(e, ins_, [], [out.b])

    def mm(self, outbuf, mms, transpose=False):
        rb = []
        seen = set()
        for (o, l, r, st, sp) in mms:
            for x in (l, r):
                if id(x.b) not in seen:
                    seen.add(id(x.b))
                    rb.append(x.b)
        self._deps("pe", rb, [outbuf])
        ins_ = None
        for (o, l, r, st, sp) in mms:
            if transpose:
                ins_ = self.nc.tensor.transpose(o.ap, l.ap, r.ap)
            else:
                ins_ = self.nc.tensor.matmul(o.ap, l.ap, r.ap, start=st, stop=sp)
            self.ninstr += 1
        self._commit("pe", ins_, rb, [outbuf])

    def dma(self, q, out, in_, sem_buf=None, **kw):
        ob, ib = out.b, in_.b
        self._deps(q, [ib], [ob])
        sbuf = sem_buf or (ob if ob.kind == "sb" else ib)
        if sbuf.dsem is None:
            if self.dsem_pool:
                sbuf.dsem, sbuf.dcnt = self.dsem_pool.pop()
            else:
                sbuf.dsem = self.nc.alloc_semaphore("dsem%d" % self.n)
                self.n += 1
            self.semowner[id(sbuf.dsem)] = sbuf
            self.all_dsem_bufs.append(sbuf)
        sbuf.dcnt += 1
        ins_ = self.E[q].dma_start(out=out.ap, in_=in_.ap, **kw)
        ins_.then_inc(sbuf.dsem, 16)
        ev = (sbuf.dsem, 16 * sbuf.dcnt, "dma")
        ob.w = ev
        ob.r = {}
        if ib is not ob:
            ib.r[id(sbuf.dsem)] = ev
        self.ninstr += 1

    def finish(self, bufs, e="sp"):
        for b in bufs:
            self._wait(e, b.w)
            for ev in b.r.values():
                self._wait(e, ev)

from concourse.bass_utils import run_bass_kernel_spmd

D = 1024
EPS = 1e-6
NE = 32
ALPHA = 1.702


class Ctx:
    pass


def make_ctx(k, L, depth):
    c = Ctx()
    c.k = k
    c.L = L
    c.depth = depth
    c.ones = k.sb("ones", [128, 512], F32)
    c.ident = k.sb("ident", [128, 128], F32)
    c.identb = k.sb("identb", [128, 128], BF16)
    c.mod = k.sb("mod", [128, depth, 48], F32)
    c.geff = k.sb("geff", [128, depth, 16], F32)
    k.memset("dve", c.ones[:, 0:512], 1.0)
    k.op("pool", "affine_select", c.ident[:, :], [c.ones[:, 0:128]], pattern=[[-1, 128]],
         compare_op=ALU.is_equal, fill=0.0, base=0, channel_multiplier=1)
    k.copy("dve", c.identb[:, :], c.ident[:, :])
    return c


def load_x_tile(c, q, x32, xin, t0, T):
    for ch in range(8):
        c.k.dma(q, x32[ch][:, :T], xin.v(xin.t[ch, :, t0:t0 + T]))


def store_x_tile(c, q, x32, xout, t0, T):
    for ch in range(8):
        c.k.dma(q, xout.v(xout.t[ch, :, t0:t0 + T]), x32[ch][:, :T])


def rmsnorm_mod(c, x32, T, layer, which, sq, rstd, ps_stat, out_bf=None, off=0, cp_eng="pool"):
    k = c.k
    for ch in range(8):
        s = sq[ch % len(sq)]
        k.act(s[:, :T], x32[ch][:, :T], AF.Square)
        k.mm(ps_stat, [(ps_stat[:, :T], c.ones[:, 0:128], s[:, :T], ch == 0, ch == 7)])
    k.ts("dve", rstd[:, :T], ps_stat[:, :T], 1.0 / D, EPS, ALU.mult, ALU.add)
    k.act(rstd[:, :T], rstd[:, :T], AF.Sqrt)
    k.op("dve", "reciprocal", rstd[:, :T], [rstd[:, :T]])
    shb = 0 if which == 0 else 24
    for ch in range(8):
        k.stt(x32[ch][:, :T], x32[ch][:, :T], c.geff[:, layer, which * 8 + ch:which * 8 + ch + 1],
              rstd[:, :T], ALU.mult, ALU.mult)
        k.act(x32[ch][:, :T], x32[ch][:, :T], AF.Identity,
              bias=c.mod[:, layer, shb + ch:shb + ch + 1])
        if out_bf is not None:
            k.copy(cp_eng, out_bf[:, ch, off:off + T], x32[ch][:, :T])


def moe_stage(c, layer, xin, xout, W, ne=NE):
    k = c.k
    L = c.L
    ST = min(1024, L)
    NSUB = ST // 128
    NTS = ST // 512
    k.stage_begin()
    wgu = [k.sb("wgu", [128, 8, 2048], BF16) for _ in range(2)]
    wdn = [k.sb("wdn", [128, 8, 1024], BF16) for _ in range(2)]
    acc = [k.sb("acc", [128, 1024], F32) for _ in range(NSUB)]
    hbf = k.sb("hbf", [128, 8, ST], BF16)
    x32 = [k.sb("x32", [128, 512], F32) for _ in range(8)]
    actT = [k.sb("actT", [128, 8, 512], BF16) for _ in range(2)]
    tg = [k.sb("tg", [128, 512], F32) for _ in range(2)]
    tt_ = [k.sb("tt", [128, 512], F32) for _ in range(2)]
    tu = [k.sb("tu", [128, 512], F32) for _ in range(2)]
    sq = tg
    rstd = tt_[0]
    wr = k.sb("wr", [128, 8, 32], F32)
    br = k.sb("br", [1, 32], F32)
    bgu = k.sb("bgu", [128, ne, 16], F32)
    bdn = k.sb("bdn", [32, 1024], F32)
    comb = k.sb("comb", [128, NSUB, 32], F32)
    combs = k.sb("combs", [128, NSUB, 32], F32)
    combT = k.sb("combT", [32, ST], F32)
    lg = k.sb("lg", [128, 32], F32)
    m8 = k.sb("m8", [128, 8], F32)
    negm = k.sb("negm", [128, 1], F32)
    mask = k.sb("mask", [128, 32], F32)
    ex = k.sb("ex", [128, 32], F32)
    ssum = k.sb("ssum", [128, 1], F32)
    pg = [k.ps("pg", [128, 512]) for _ in range(2)]
    pu = [k.ps("pu", [128, 512]) for _ in range(2)]
    pd = [k.ps("pd", [128, 512]) for _ in range(2)]
    pm = [k.ps("pm", [128, 512]) for _ in range(2)]

    k.dma("sp", wr[:, :, :], W["w_router"].v(W["w_router"].t.rearrange("(kc p) e -> p kc e", p=128)))
    k.dma("sp", br[:, :], W["b_router"].v(W["b_router"].t))
    k.dma("sp", bgu[:, :, :], W["b_gu_l"].v(W["b_gu_l"].t))
    k.dma("sp", bdn[:ne, :], W["b_down"].v(W["b_down"].t))
    k.ts("dve", bgu[:, :, 8:16], bgu[:, :, 8:16], 1.0, None, ALU.add)

    def load_w(gi):
        e = gi % ne
        k.dma("pool", wgu[gi % 2][:, :, :],
              W["w_gu"].v(W["w_gu"].t[e].rearrange("(kc p) n -> p kc n", p=128)))
        k.dma("pool", wdn[gi % 2][:, :, :],
              W["w_down"].v(W["w_down"].t[e].rearrange("(kc p) n -> p kc n", p=128)))

    nst = L // ST
    load_w(0)
    pmi = 0
    cnt = 0
    for s in range(nst):
        for ts_ in range(NTS):
            t0 = s * ST + ts_ * 512
            load_x_tile(c, "sp", x32, xin, t0, 512)
            rmsnorm_mod(c, x32, 512, layer, 1, sq, rstd, pm[0], out_bf=hbf, off=ts_ * 512, cp_eng="dve")
            for j in range(4):
                sub = ts_ * 4 + j
                p = pm[1]
                mms = [(p[:, :32], x32[ch][:, j * 128:(j + 1) * 128], wr[:, ch, :], ch == 0, False)
                       for ch in range(8)]
                mms.append((p[:, :32], c.ones[0:1, 0:128], br[0:1, :], False, True))
                k.mm(p, mms)
                k.copy("dve", lg[:, :], p[:, :32])
                k.op("dve", "max", m8[:, :], [lg[:, :]])
                k.ts("dve", mask[:, :], lg[:, :], m8[:, 3:4], None, ALU.is_ge)
                k.ts("dve", negm[:, :], m8[:, 0:1], -1.0, None, ALU.mult)
                k.act(ex[:, :], lg[:, :], AF.Exp, bias=negm[:, 0:1], scale=1.0)
                k.stt(ex[:, :], ex[:, :], 1.0, mask[:, :], ALU.mult, ALU.mult, accum_out=ssum[:, :])
                k.op("dve", "reciprocal", ssum[:, :], [ssum[:, :]])
                k.ts("dve", comb[:, sub, :], ex[:, :], ssum[:, 0:1], None, ALU.mult)
                k.ts("dve", combs[:, sub, :], ex[:, :], ssum[:, 0:1], 1.0 / ALPHA, ALU.mult, ALU.mult)
                k.mm(p, [(p[:32, 128:256], comb[:, sub, :], c.ident[:, :], True, True)], transpose=True)
                k.copy("dve", combT[:, sub * 128:(sub + 1) * 128], p[:32, 128:256])
        for e in range(ne):
            gi = s * ne + e
            if gi + 1 < nst * ne:
                load_w(gi + 1)
            wg, wd = wgu[gi % 2], wdn[gi % 2]
            for ts_ in range(NTS):
                aT = actT[cnt % 2]
                cnt += 1
                for n in range(8):
                    a, b = pg[n % 2], pu[n % 2]
                    k.mm(a, [(a[:, :], wg[:, kc, n * 128:(n + 1) * 128], hbf[:, kc, ts_ * 512:(ts_ + 1) * 512],
                              kc == 0, kc == 7) for kc in range(8)])
                    k.mm(b, [(b[:, :], wg[:, kc, 1024 + n * 128:1024 + (n + 1) * 128],
                              hbf[:, kc, ts_ * 512:(ts_ + 1) * 512], kc == 0, kc == 7) for kc in range(8)])
                    g, t, u = tg[n % 2], tt_[n % 2], tu[n % 2]
                    k.ts("dve", g[:, :], a[:, :], bgu[:, e, n:n + 1], 7.0, ALU.add, ALU.min)
                    k.act(t[:, :], g[:, :], AF.Silu, scale=ALPHA)
                    k.ts("dve", u[:, :], b[:, :], bgu[:, e, 8 + n:9 + n], -6.0, ALU.add, ALU.max)
                    k.stt(aT[:, n, :], u[:, :], 8.0, t[:, :], ALU.min, ALU.mult)
                for j in range(4):
                    sub = ts_ * 4 + j
                    for half in range(2):
                        p = pd[(j * 2 + half) % 2]
                        k.mm(p, [(p[:, :], aT[:, kc, j * 128:(j + 1) * 128], wd[:, kc, half * 512:(half + 1) * 512],
                                  kc == 0, kc == 7) for kc in range(8)])
                        av = acc[sub][:, half * 512:(half + 1) * 512]
                        if e == 0:
                            k.ts("dve", av, p[:, :], combs[:, sub, e:e + 1], None, ALU.mult)
                        else:
                            k.stt(av, p[:, :], combs[:, sub, e:e + 1], av, ALU.mult, ALU.add)
        for sub in range(NSUB):
            for half in range(2):
                p = pm[(sub * 2 + half) % 2]
                k.mm(p, [(p[:, :], combT[:ne, sub * 128:(sub + 1) * 128], bdn[:ne, half * 512:(half + 1) * 512],
                          True, True)])
                av = acc[sub][:, half * 512:(half + 1) * 512]
                k.tt("dve", av, av, p[:, :], ALU.add)
        for ts_ in range(NTS):
            t0 = s * ST + ts_ * 512
            load_x_tile(c, "sp", x32, xin, t0, 512)
            for ch in range(8):
                p = pm[ch % 2]
                k.mm(p, [(p[:, j * 128:(j + 1) * 128], acc[ts_ * 4 + j][:, ch * 128:(ch + 1) * 128], c.ident[:, :],
                          True, True) for j in range(4)], transpose=True)
                k.stt(x32[ch][:, :], p[:, :], c.mod[:, layer, 40 + ch:41 + ch], x32[ch][:, :], ALU.mult, ALU.add)
            store_x_tile(c, "sp", x32, xout, t0, 512)
    k.stage_end()


def make_chunk_masks(c):
    k = c.k
    c.mrev = k.sb("mrev", [128, 128], F32)
    c.mch = k.sb("mch", [128, 2], F32)
    k.op("pool", "affine_select", c.mrev[:, :], [c.ones[:, 0:128]], pattern=[[-1, 128]],
         compare_op=ALU.is_gt, fill=0.0, base=0, channel_multiplier=1)
    k.op("pool", "affine_select", c.mrev[:, 0:64], [c.mrev[:, 0:64]], pattern=[[0, 64]],
         compare_op=ALU.is_gt, fill=0.0, base=64, channel_multiplier=-1)
    k.op("pool", "affine_select", c.mch[:, 0:1], [c.ones[:, 0:1]], pattern=[[0, 1]],
         compare_op=ALU.is_gt, fill=0.0, base=64, channel_multiplier=-1)
    k.op("pool", "affine_select", c.mch[:, 1:2], [c.ones[:, 0:1]], pattern=[[0, 1]],
         compare_op=ALU.is_ge, fill=0.0, base=-64, channel_multiplier=1)


def gla_stage(c, layer, xin, xout, W):
    k = c.k
    L = c.L
    T = 512
    k.stage_begin()
    w_in = k.sb("w_in", [128, 8, 3072], BF16)
    w_out = k.sb("w_out", [128, 8, 1024], BF16)
    w_gk1 = k.sb("w_gk1", [128, 8, 16], BF16)
    w_gk2 = k.sb("w_gk2", [16, 512], F32)
    b_gk = k.sb("b_gk", [1, 512], F32)
    g_on = k.sb("g_on", [128, 2], F32)
    x32 = [k.sb("x32", [128, T], F32) for _ in range(8)]
    hbf = k.sb("hbf", [128, 8, T], BF16)
    sq = [k.sb("sq", [128, T], F32) for _ in range(2)]
    rstd = k.sb("rstd", [128, T], F32)
    qT = [k.sb("qT", [128, T], BF16) for _ in range(4)]
    kdec = [k.sb("kdec", [128, 512], BF16) for _ in range(4)]
    vbf = [k.sb("vbf", [128, 1024], BF16) for _ in range(4)]
    gs = [k.sb("gs", [128, T], F32) for _ in range(8)]
    rT = k.sb("rT", [16, T], F32)
    la = [k.sb("la", [128, 512], F32) for _ in range(2)]
    edec = [k.sb("edec", [128, 512], F32) for _ in range(2)]
    dcy = k.sb("dcy", [128, 4, 8], F32)
    S = [k.sb("S", [128, 256], F32) for _ in range(4)]
    Sb = [[k.sb("Sb", [128, 256], BF16) for _ in range(2)] for _ in range(4)]
    oT = [k.sb("oT", [128, 2, T], F32) for _ in range(4)]
    onb = k.sb("onb", [128, 8, T], BF16)
    tmp = [k.sb("tmp", [128, T], F32) for _ in range(2)]
    pA = [k.ps("pA", [128, 512]) for _ in range(2)]
    pU = [k.ps("pU", [128, 512]) for _ in range(2)]
    pO = [k.ps("pO", [128, 512]) for _ in range(2)]
    ptot = k.ps("ptot", [128, 512])
    pst = k.ps("pst", [128, 512])

    k.dma("pool", w_in[:, :, :], W["w_in"].v(W["w_in"].t.rearrange("(kc p) n -> p kc n", p=128)))
    k.dma("pool", w_out[:, :, :], W["w_out"].v(W["w_out"].t.rearrange("(kc p) n -> p kc n", p=128)))
    k.dma("pool", w_gk1[:, :, :], W["w_gk1"].v(W["w_gk1"].t.rearrange("(kc p) n -> p kc n", p=128)))
    k.dma("sp", w_gk2[:, :], W["w_gk2"].v(W["w_gk2"].t))
    k.dma("sp", b_gk[:, :], W["b_gk"].v(W["b_gk"].t))
    k.dma("sp", g_on[:, :], W["g_onorm_l"].v(W["g_onorm_l"].t))
    for h in range(4):
        k.memset("dve", S[h][:, :], 0.0)

    pai = [0]

    def nextA():
        pai[0] += 1
        return pA[pai[0] % 2]

    for it in range(L // T):
        t0 = it * T
        load_x_tile(c, "sp", x32, xin, t0, T)
        rmsnorm_mod(c, x32, T, layer, 0, sq, rstd, pst, out_bf=hbf, off=0, cp_eng="dve")
        load_x_tile(c, "sp", x32, xin, t0, T)
        for h in range(4):
            p = nextA()
            k.mm(p, [(p[:, :], w_in[:, kc, h * 128:(h + 1) * 128], hbf[:, kc, :], kc == 0, kc == 7)
                     for kc in range(8)])
            k.act(qT[h][:, :], p[:, :], AF.Copy, scale=128.0 ** -0.5)
        for n in range(8):
            p = nextA()
            k.mm(p, [(p[:, :], w_in[:, kc, 2048 + n * 128:2048 + (n + 1) * 128], hbf[:, kc, :], kc == 0, kc == 7)
                     for kc in range(8)])
            k.act(gs[n][:, :], p[:, :], AF.Silu)
        p = nextA()
        k.mm(p, [(p[:16, :], w_gk1[:, kc, :], hbf[:, kc, :], kc == 0, kc == 7) for kc in range(8)])
        k.copy("dve", rT[:, :], p[:16, :])
        for j in range(4):
            for half in range(2):
                p = nextA()
                k.mm(p, [(p[:, :], hbf[:, kc, j * 128:(j + 1) * 128],
                          w_in[:, kc, 1024 + half * 512:1024 + (half + 1) * 512], kc == 0, kc == 7)
                         for kc in range(8)])
                k.copy("act", vbf[j][:, half * 512:(half + 1) * 512], p[:, :])
            p = nextA()
            k.mm(p, [(p[:, :], rT[:16, j * 128:(j + 1) * 128], w_gk2[:16, :], True, False),
                     (p[:, :], c.ones[0:1, 0:128], b_gk[0:1, :], False, True)])
            l_ = la[j % 2]
            k.act(l_[:, :], p[:, :], AF.Exp, scale=-1.0)
            k.act(l_[:, :], l_[:, :], AF.Ln, bias=1.0)
            p = nextA()
            k.mm(p, [(p[:, :], c.mrev[:, :], l_[:, :], True, True)])
            ed = edec[j % 2]
            k.act(ed[:, :], p[:, :], AF.Exp, scale=-1.0 / 16.0)
            for h in range(4):
                k.mm(ptot, [(ptot[:, h * 8 + 2 * j:h * 8 + 2 * j + 2], l_[:, h * 128:(h + 1) * 128], c.mch[:, :],
                             True, True)])
            p = nextA()
            k.mm(p, [(p[:, :], hbf[:, kc, j * 128:(j + 1) * 128], w_in[:, kc, 512:1024], kc == 0, kc == 7)
                     for kc in range(8)])
            k.tt("dve", kdec[j][:, :], p[:, :], ed[:, :], ALU.mult)
        k.act(dcy[:, :, :], ptot[:, 0:32], AF.Exp, scale=-1.0 / 16.0)
        n_u = 0
        for cc in range(8):
            j, hf = cc // 2, cc % 2
            for h in range(4):
                pu_ = pU[n_u % 2]
                po_ = pO[n_u % 2]
                n_u += 1
                k.mm(pu_, [(pu_[:, :256], kdec[j][64 * hf:64 * hf + 64, h * 128:(h + 1) * 128],
                            vbf[j][64 * hf:64 * hf + 64, h * 256:(h + 1) * 256], True, True)])
                k.stt(S[h][:, :], S[h][:, :], dcy[:, h, cc:cc + 1], pu_[:, :256], ALU.mult, ALU.add)
                sb_ = Sb[h][cc % 2]
                k.copy("act", sb_[:, :], S[h][:, :])
                k.mm(po_, [(po_[:, dv * 64:(dv + 1) * 64], sb_[:, dv * 128:(dv + 1) * 128],
                            qT[h][:, cc * 64:(cc + 1) * 64], True, True) for dv in range(2)])
                k.copy("act", oT[h][:, :, cc * 64:(cc + 1) * 64],
                       po_.v(po_.t[:, 0:128].rearrange("p (d t) -> p d t", d=2)))
        for h in range(4):
            for dv in range(2):
                s_ = sq[dv]
                k.act(s_[:, :], oT[h][:, dv, :], AF.Square)
                k.mm(pst, [(pst[:, :], c.ones[:, 0:128], s_[:, :], dv == 0, dv == 1)])
            k.ts("dve", rstd[:, :], pst[:, :], 1.0 / 256.0, EPS, ALU.mult, ALU.add)
            k.act(rstd[:, :], rstd[:, :], AF.Sqrt)
            k.op("dve", "reciprocal", rstd[:, :], [rstd[:, :]])
            for dv in range(2):
                t_ = tmp[dv]
                k.stt(t_[:, :], oT[h][:, dv, :], g_on[:, dv:dv + 1], rstd[:, :], ALU.mult, ALU.mult)
                k.tt("dve", onb[:, h * 2 + dv, :], t_[:, :], gs[h * 2 + dv][:, :], ALU.mult)
        for n in range(8):
            p = nextA()
            k.mm(p, [(p[:, :], w_out[:, kc, n * 128:(n + 1) * 128], onb[:, kc, :], kc == 0, kc == 7)
                     for kc in range(8)])
            k.stt(x32[n][:, :], p[:, :], c.mod[:, layer, 16 + n:17 + n], x32[n][:, :], ALU.mult, ALU.add)
        store_x_tile(c, "sp", x32, xout, t0, T)
    k.stage_end()


import math
PI = math.pi


def sin_turns(k, out, x, phase, y, yi, m):
    k.ts("dve", y, x, 1.0 / (2.0 * PI), phase, ALU.mult, ALU.add)
    k.copy("dve", yi, y)
    k.copy("dve", m, yi)
    k.tt("dve", y, y, m, ALU.subtract)
    k.ts("dve", m, y, 0.5, None, ALU.is_gt)
    k.tt("dve", y, y, m, ALU.subtract)
    k.ts("dve", m, y, -0.5, None, ALU.is_lt)
    k.tt("dve", y, y, m, ALU.add)
    k.act(out, y, AF.Sin, scale=2.0 * PI)


def s5_stage(c, layer, xin, xout, W):
    k = c.k
    L = c.L
    T = 512
    TS = 128
    NB = 32
    PE_ = "dve"
    k.stage_begin()
    w_in = k.sb("w_in", [128, 8, 1024], BF16)
    w_out = k.sb("w_out", [128, 8, 2048], BF16)
    Bbr = k.sb("Bbr", [128, NB, 128], BF16)
    Bbi = k.sb("Bbi", [128, NB, 128], BF16)
    Cbr = k.sb("Cbr", [128, NB, 128], BF16)
    Cbi = k.sb("Cbi", [128, NB, 128], BF16)
    cost = k.sb("cost", [128, NB, TS], F32)
    sint = k.sb("sint", [128, NB, TS], F32)
    rcol = k.sb("rcol", [128, NB], F32)
    dl = k.sb("dl", [128, 8], F32)
    spr = k.sb("spr", [128, NB], F32)
    spi = k.sb("spi", [128, NB], F32)
    k.dma("pool", w_in[:, :, :], W["w_in"].v(W["w_in"].t.rearrange("(kc p) n -> p kc n", p=128)))
    k.dma("pool", w_out[:, :, :], W["w_out"].v(W["w_out"].t.rearrange("(kc p) n -> p kc n", p=128)))
    k.dma("sp", dl[:, :], W["d_l"].v(W["d_l"].t))
    k.memset("dve", spr[:, :], 0.0)
    k.memset("dve", spi[:, :], 0.0)

    PH = 9
    k.stage_begin()
    Q = 1024
    nm = ["are", "aim", "ldt", "bre", "bim", "t0", "t1", "t2", "t3", "t4", "t5", "t6"]
    A = {n: k.sb(n, [128, Q], F32) for n in nm}
    A["y"] = k.sb("y", [128, Q], F32)
    A["yi"] = k.sb("yi", [128, Q], mybir.dt.int32)
    for q in range(4 if PH >= 1 else 0):
        sl = slice(q * Q, (q + 1) * Q)
        for n, src in (("are", "Ablk_re"), ("aim", "Ablk_im"), ("ldt", "Lblk"), ("bre", "Bblk_re"), ("bim", "Bblk_im")):
            k.dma("sp", A[n][:, :], W[src].v(W[src].t[:, sl]))
        dt_, ar, ai, er, sn, cs, t6 = A["t0"], A["t1"], A["t2"], A["t3"], A["t4"], A["t5"], A["t6"]
        k.act(dt_[:, :], A["ldt"][:, :], AF.Exp)
        k.tt("dve", ar[:, :], A["are"][:, :], dt_[:, :], ALU.mult)
        k.tt("dve", ai[:, :], A["aim"][:, :], dt_[:, :], ALU.mult)
        k.act(er[:, :], ar[:, :], AF.Exp)
        sin_turns(k, sn[:, :], ai[:, :], 0.0, A["y"][:, :], A["yi"][:, :], t6[:, :])
        sin_turns(k, cs[:, :], ai[:, :], 0.25, A["y"][:, :], A["yi"][:, :], t6[:, :])
        nr, ni = ar, ai
        k.tt("dve", nr[:, :], er[:, :], cs[:, :], ALU.mult)
        k.ts("dve", nr[:, :], nr[:, :], -1.0, None, ALU.add)
        k.tt("dve", ni[:, :], er[:, :], sn[:, :], ALU.mult)
        den = er
        k.tt("dve", den[:, :], A["are"][:, :], A["are"][:, :], ALU.mult)
        k.tt("dve", t6[:, :], A["aim"][:, :], A["aim"][:, :], ALU.mult)
        k.tt("dve", den[:, :], den[:, :], t6[:, :], ALU.add)
        k.op("dve", "reciprocal", den[:, :], [den[:, :]])
        cr, ci = sn, cs
        k.tt("dve", cr[:, :], nr[:, :], A["are"][:, :], ALU.mult)
        k.tt("dve", t6[:, :], ni[:, :], A["aim"][:, :], ALU.mult)
        k.tt("dve", cr[:, :], cr[:, :], t6[:, :], ALU.add)
        k.tt("dve", cr[:, :], cr[:, :], den[:, :], ALU.mult)
        k.tt("dve", ci[:, :], ni[:, :], A["are"][:, :], ALU.mult)
        k.tt("dve", t6[:, :], nr[:, :], A["aim"][:, :], ALU.mult)
        k.tt("dve", ci[:, :], ci[:, :], t6[:, :], ALU.subtract)
        k.tt("dve", ci[:, :], ci[:, :], den[:, :], ALU.mult)
        x1, x2 = ar, ai
        k.tt("dve", x1[:, :], cr[:, :], A["bre"][:, :], ALU.mult)
        k.tt("dve", x2[:, :], ci[:, :], A["bim"][:, :], ALU.mult)
        k.tt("dve", Bbr.v(Bbr.t[:, q * 8:(q + 1) * 8, :].rearrange("p b m -> p (b m)")), x1[:, :], x2[:, :], ALU.subtract)
        k.tt("dve", x1[:, :], cr[:, :], A["bim"][:, :], ALU.mult)
        k.tt("dve", x2[:, :], ci[:, :], A["bre"][:, :], ALU.mult)
        k.tt("dve", Bbi.v(Bbi.t[:, q * 8:(q + 1) * 8, :].rearrange("p b m -> p (b m)")), x1[:, :], x2[:, :], ALU.add)
        k.dma("sp", A["bre"][:, :], W["Cblk_re"].v(W["Cblk_re"].t[:, sl]))
        k.dma("sp", A["bim"][:, :], W["Cblk_im"].v(W["Cblk_im"].t[:, sl]))
        k.copy("dve", Cbr.v(Cbr.t[:, q * 8:(q + 1) * 8, :].rearrange("p b m -> p (b m)")), A["bre"][:, :])
        k.ts("dve", Cbi.v(Cbi.t[:, q * 8:(q + 1) * 8, :].rearrange("p b m -> p (b m)")), A["bim"][:, :], -1.0, None, ALU.mult)
    k.stage_end()
    k.stage_begin()
    acr = k.sb("acr", [128, NB], F32)
    aci = k.sb("aci", [128, NB], F32)
    lc = k.sb("lc", [128, NB], F32)
    th = k.sb("th", [128, NB], F32)
    tI_i = k.sb("tIi", [128, TS], mybir.dt.int32)
    tI = k.sb("tI", [128, TS], F32)
    ang = k.sb("ang", [128, NB, TS], F32)
    yy = k.sb("yy", [128, NB * TS], F32)
    yyi = k.sb("yyi", [128, NB * TS], mybir.dt.int32)
    mm_ = k.sb("mm_", [128, NB * TS], F32)
    if PH < 2:
        k.stage_end()
        k.stage_end()
        return
    k.dma("sp", acr[:, :], W["Acol_re"].v(W["Acol_re"].t))
    k.dma("sp", aci[:, :], W["Acol_im"].v(W["Acol_im"].t))
    k.dma("sp", lc[:, :], W["Lcol"].v(W["Lcol"].t))
    k.act(lc[:, :], lc[:, :], AF.Exp)
    k.tt("dve", th[:, :], aci[:, :], lc[:, :], ALU.mult)
    k.tt("dve", acr[:, :], acr[:, :], lc[:, :], ALU.mult)
    k.act(rcol[:, :], acr[:, :], AF.Exp)
    k.op("pool", "iota", tI_i[:, :], [], pattern=[[1, TS]], base=1, channel_multiplier=0)
    k.copy("dve", tI[:, :], tI_i[:, :])
    for bi in range(NB):
        k.ts("dve", ang[:, bi, :], tI[:, :], th[:, bi:bi + 1], None, ALU.mult)
    fl = lambda b: b.v(b.t[:, :, :].rearrange("p b t -> p (b t)"))
    sin_turns(k, fl(sint), fl(ang), 0.0, yy[:, :], yyi[:, :], mm_[:, :])
    sin_turns(k, fl(cost), fl(ang), 0.25, yy[:, :], yyi[:, :], mm_[:, :])
    k.stage_end()
    if PH < 3:
        k.stage_end()
        return

    x32 = [k.sb("x32", [128, T], F32) for _ in range(8)]
    hbf = k.sb("hbf", [128, 8, T], BF16)
    ubf = k.sb("ubf", [128, 8, T], BF16)
    ybf = k.sb("ybf", [128, 8, T], BF16)
    sq = [k.sb("sq", [128, T], F32) for _ in range(2)]
    rstd = k.sb("rstd", [128, T], F32)
    bre = k.sb("bre", [128, T], F32)
    bim = k.sb("bim", [128, T], F32)
    ta = k.sb("ta", [128, T], F32)
    tb = k.sb("tb", [128, T], F32)
    wre = k.sb("wre", [128, T], F32)
    wim = k.sb("wim", [128, T], F32)
    zre = k.sb("zre", [128, T], F32)
    zim = k.sb("zim", [128, T], F32)
    rfull = k.sb("rfull", [128, TS], F32)
    sm1 = k.sb("sm1", [128, 1], F32)
    sm2 = k.sb("sm2", [128, 1], F32)
    inr = k.sb("inr", [128, 1], F32)
    ini = k.sb("ini", [128, 1], F32)
    sbr = [[k.sb("sbr", [128, T], BF16) for _ in range(4)] for _ in range(2)]
    sbi = [[k.sb("sbi", [128, T], BF16) for _ in range(4)] for _ in range(2)]
    pA = [k.ps("pA", [128, 512]) for _ in range(2)]
    pR = [k.ps("pR", [128, 512]) for _ in range(2)]
    pI = [k.ps("pI", [128, 512]) for _ in range(2)]
    pY = k.ps("pY", [128, 512])
    pst = k.ps("pst", [128, 512])
    pai = [0]

    def nextA():
        pai[0] += 1
        return pA[pai[0] % 2]

    TB = False

    def v3(b):
        return b.v(b.t[:, :].rearrange("p (s t) -> p s t", s=T // TS))

    def tb3(tab, bi):
        return tab.v(tab.t[:, bi, :].unsqueeze(1).to_broadcast([128, T // TS, TS]))

    def ttab(e, out, in0, tab, bi, op):
        if TB:
            k.tt(e, v3(out), v3(in0), tb3(tab, bi), op)
        else:
            for s_ in range(T // TS):
                sl = slice(s_ * TS, (s_ + 1) * TS)
                k.tt(e, out[:, sl], in0[:, sl], tab[:, bi, :], op)

    nblk = 0
    for it in range(L // T):
        t0 = it * T
        load_x_tile(c, "sp", x32, xin, t0, T)
        rmsnorm_mod(c, x32, T, layer, 0, sq, rstd, pst, out_bf=hbf, off=0, cp_eng="dve")
        for ch in range(8):
            p = nextA()
            k.mm(p, [(p[:, :], w_in[:, kc, ch * 128:(ch + 1) * 128], hbf[:, kc, :], kc == 0, kc == 7)
                     for kc in range(8)])
            k.copy("act", x32[ch][:, :], p[:, :])
            k.copy("dve", ubf[:, ch, :], x32[ch][:, :])
        for j in range(8):
            par = j % 2
            for m in range(4):
                bi = j * 4 + m
                pr, pi_ = pR[nblk % 2], pI[nblk % 2]
                nblk += 1
                k.mm(pr, [(pr[:, :], Bbr[:, bi, :], ubf[:, j, :], True, True)])
                k.mm(pi_, [(pi_[:, :], Bbi[:, bi, :], ubf[:, j, :], True, True)])
                k.copy("act", bre[:, :], pr[:, :])
                k.copy("act", bim[:, :], pi_[:, :])
                ttab("dve", ta, bre, cost, bi, ALU.mult)
                ttab(PE_, tb, bim, sint, bi, ALU.mult)
                k.tt("dve", wre[:, :], ta[:, :], tb[:, :], ALU.add)
                ttab(PE_, ta, bim, cost, bi, ALU.mult)
                ttab("dve", tb, bre, sint, bi, ALU.mult)
                k.tt(PE_, wim[:, :], ta[:, :], tb[:, :], ALU.subtract)
                if False:
                    rb = rcol.v(rcol.t[:, bi:bi + 1].to_broadcast([128, TS]))
                else:
                    k.ts("dve", rfull[:, :], c.ones[:, :TS], rcol[:, bi:bi + 1], None, ALU.mult)
                    rb = rfull[:, :]
                for s_ in range(T // TS):
                    sl = slice(s_ * TS, (s_ + 1) * TS)
                    i_r = spr[:, bi:bi + 1] if s_ == 0 else inr[:, :]
                    i_i = spi[:, bi:bi + 1] if s_ == 0 else ini[:, :]
                    k.op("dve", "tensor_tensor_scan", zre[:, sl], [rb, wre[:, sl], i_r, ALU.mult, ALU.add])
                    k.op("dve", "tensor_tensor_scan", zim[:, sl], [rb, wim[:, sl], i_i, ALU.mult, ALU.add])
                    last = s_ == T // TS - 1
                    o_r = spr[:, bi:bi + 1] if last else inr[:, :]
                    o_i = spi[:, bi:bi + 1] if last else ini[:, :]
                    e_ = (s_ + 1) * TS - 1
                    cT, sT = cost[:, bi, TS - 1:TS], sint[:, bi, TS - 1:TS]
                    k.ts("dve", sm1[:, :], zim[:, e_:e_ + 1], sT, None, ALU.mult)
                    k.ts("dve", sm2[:, :], zim[:, e_:e_ + 1], cT, None, ALU.mult)
                    k.stt(o_r, zre[:, e_:e_ + 1], cT, sm1[:, :], ALU.mult, ALU.subtract)
                    k.stt(o_i, zre[:, e_:e_ + 1], sT, sm2[:, :], ALU.mult, ALU.add)
                ttab("dve", ta, zre, cost, bi, ALU.mult)
                ttab(PE_, tb, zim, sint, bi, ALU.mult)
                k.tt("dve", sbr[par][m][:, :], ta[:, :], tb[:, :], ALU.subtract)
                ttab(PE_, ta, zre, sint, bi, ALU.mult)
                ttab("dve", tb, zim, cost, bi, ALU.mult)
                k.tt(PE_, sbi[par][m][:, :], ta[:, :], tb[:, :], ALU.add)
            mms = []
            for m in range(4):
                bi = j * 4 + m
                mms.append((pY[:, :], Cbr[:, bi, :], sbr[par][m][:, :], m == 0, False))
                mms.append((pY[:, :], Cbi[:, bi, :], sbi[par][m][:, :], False, m == 3))
            k.mm(pY, mms)
            k.stt(ta[:, :], x32[j][:, :], dl[:, j:j + 1], pY[:, :], ALU.mult, ALU.add)
            k.act(ybf[:, j, :], ta[:, :], AF.Gelu)
        load_x_tile(c, "sp", x32, xin, t0, T)
        for n in range(8):
            pa, pb = pR[n % 2], pI[n % 2]
            k.mm(pa, [(pa[:, :], w_out[:, kc, n * 128:(n + 1) * 128], ybf[:, kc, :], kc == 0, kc == 7)
                      for kc in range(8)])
            k.mm(pb, [(pb[:, :], w_out[:, kc, 1024 + n * 128:1024 + (n + 1) * 128], ybf[:, kc, :], kc == 0, kc == 7)
                      for kc in range(8)])
            k.act(tb[:, :], pb[:, :], AF.Sigmoid)
            k.tt("dve", ta[:, :], pa[:, :], tb[:, :], ALU.mult)
            k.stt(x32[n][:, :], ta[:, :], c.mod[:, layer, 16 + n:17 + n], x32[n][:, :], ALU.mult, ALU.add)
        store_x_tile(c, "sp", x32, xout, t0, T)
    k.stage_end()


def s5_host_layout(a_re, a_im, log_dt, b_re, b_im, c_re, c_im, d):
    NB = 32
    out = {}
    Ablk_re = np.zeros((128, NB, 128), np.float32); Ablk_im = np.zeros_like(Ablk_re); Lblk = np.zeros_like(Ablk_re)
    Bblk_re = np.zeros_like(Ablk_re); Bblk_im = np.zeros_like(Ablk_re)
    Cblk_re = np.zeros_like(Ablk_re); Cblk_im = np.zeros_like(Ablk_re)
    Acol_re = np.zeros((128, NB), np.float32); Acol_im = np.zeros_like(Acol_re); Lcol = np.zeros_like(Acol_re)
    for j in range(8):
        for m in range(4):
            bi = j * 4 + m
            for gg in range(2):
                g = 8 * j + 2 * m + gg
                gl = 2 * m + gg
                cs = slice(gg * 64, (gg + 1) * 64)
                Ablk_re[:, bi, cs] = a_re[g][None, :]
                Ablk_im[:, bi, cs] = a_im[g][None, :]
                Lblk[:, bi, cs] = log_dt[g]
                Bblk_re[gl * 16:(gl + 1) * 16, bi, cs] = b_re[g].T
                Bblk_im[gl * 16:(gl + 1) * 16, bi, cs] = b_im[g].T
                Cblk_re[cs, bi, gl * 16:(gl + 1) * 16] = c_re[g].T
                Cblk_im[cs, bi, gl * 16:(gl + 1) * 16] = c_im[g].T
                Acol_re[cs, bi] = a_re[g]
                Acol_im[cs, bi] = a_im[g]
                Lcol[cs, bi] = log_dt[g]
    f = lambda a: np.ascontiguousarray(a.reshape(128, NB * 128))
    return dict(Ablk_re=f(Ablk_re), Ablk_im=f(Ablk_im), Lblk=f(Lblk), Bblk_re=f(Bblk_re), Bblk_im=f(Bblk_im),
                Cblk_re=f(Cblk_re), Cblk_im=f(Cblk_im), Acol_re=Acol_re, Acol_im=Acol_im, Lcol=Lcol,
                d_l=np.ascontiguousarray(d.reshape(8, 128).T))


def mlstm_stage(c, layer, xin, xout, W):
    k = c.k
    L = c.L
    T = 256
    NC_ = T // 64
    NS = T // 128
    DH = 512
    k.stage_begin()
    wbuf = [k.sb("wbuf", [128, 8, 1024], BF16) for _ in range(3)]
    bdq = k.sb("bdq", [128, 16, 128], BF16)
    bdk = k.sb("bdk", [128, 16, 128], BF16)
    bdv = k.sb("bdv", [128, 16, 128], BF16)
    weffc = k.sb("weffc", [128, 16, 8], BF16)
    weffm = k.sb("weffm", [128, 16, 8], BF16)
    convl = k.sb("convl", [128, 16, 5], F32)
    bif = k.sb("bif", [1, 8], F32)
    skipl = k.sb("skipl", [128, 16], F32)
    gnl = k.sb("gnl", [128, 16], F32)
    cmat = [[k.sb("cmat", [128, 512], F32) for _ in range(4)] for _ in range(4)]
    cb = [[k.sb("cb", [128, 512], BF16) for _ in range(4)] for _ in range(4)]
    nvec = [k.sb("nvec", [128, 4], F32) for _ in range(4)]
    nvb = [k.sb("nvb", [128, 4], BF16) for _ in range(4)]
    halo = k.sb("halo", [128, 16, 3], F32)
    mcar = k.sb("mcar", [4, 1], F32)
    cmask = k.sb("cmask", [4, T], F32)
    sel = k.sb("sel", [4, 4, 128], F32)
    onesb = k.sb("onesb", [128, 1], BF16)
    for n_, src in (("bdq", bdq), ("bdk", bdk), ("bdv", bdv)):
        k.dma("pool", src[:, :, :], W[n_].v(W[n_].t))
    k.dma("sp", convl[:, :, :], W["conv_l"].v(W["conv_l"].t))
    k.dma("sp", bif[:, :], W["b_if"].v(W["b_if"].t))
    k.dma("sp", skipl[:, :], W["skip_l"].v(W["skip_l"].t))
    k.dma("sp", gnl[:, :], W["gnorm_l"].v(W["gnorm_l"].t))
    for h in range(4):
        for kc in range(4):
            k.memset("dve", cmat[h][kc][:, :], 0.0)
        k.memset("dve", nvec[h][:, :], 0.0)
        k.ts("dve", sel[:, h, :], c.ones[0:4, 0:128], c.ident[0:4, h:h + 1], None, ALU.mult)
    k.memset("dve", halo[:, :, :], 0.0)
    k.memset("dve", mcar[:, :], 0.0)
    k.memset("dve", cmask[:, :], 1.0)
    k.memset("dve", cmask.v(cmask.t[:, :].rearrange("p (c t) -> p c t", t=64)[:, :, 0:1]), 0.0)
    k.memset("dve", onesb[:, :], 1.0)
    k.stage_begin()
    bT = {n_: k.sb(n_, [128, 16, 128], F32) for n_ in ("bdqT", "bdkT", "bdvT")}
    wif = k.sb("wif", [128, 48, 8], F32)
    pw = k.ps("pw", [128, 512])
    for n_ in bT:
        k.dma("sp", bT[n_][:, :, :], W[n_].v(W[n_].t))
    k.dma("sp", wif[:, :, :], W["wif_l"].v(W["wif_l"].t))
    for ch in range(16):
        k.mm(pw, [(pw[:, ch * 8:ch * 8 + 8], bT["bdqT"][:, ch, :], wif[:, ch, :], True, False),
                  (pw[:, ch * 8:ch * 8 + 8], bT["bdkT"][:, ch, :], wif[:, 16 + ch, :], False, True)])
    k.copy("dve", weffc.v(weffc.t[:, :, :].rearrange("p c g -> p (c g)")), pw[:, 0:128])
    for ch in range(16):
        k.mm(pw, [(pw[:, 128 + ch * 8:128 + ch * 8 + 8], bT["bdvT"][:, ch, :], wif[:, 32 + ch, :], True, True)])
    k.copy("dve", weffm.v(weffm.t[:, :, :].rearrange("p c g -> p (c g)")), pw[:, 128:256])
    k.stage_end()

    x32 = [k.sb("x32", [128, T], F32) for _ in range(8)]
    hbf = k.sb("hbf", [128, 8, T], BF16)
    sq = [k.sb("sq", [128, T], F32) for _ in range(2)]
    rstd = k.sb("rstd", [128, T], F32)
    xmb = k.sb("xmb", [128, 16, T], BF16)
    xcb = k.sb("xcb", [128, 16, T], BF16)
    zsb = k.sb("zsb", [128, 16, T], BF16)
    qTb = k.sb("qTb", [128, 16, T], BF16)
    kw = [k.sb("kw", [128, 2048], BF16) for _ in range(NS)]
    vt = [k.sb("vt", [128, 2048], BF16) for _ in range(NS)]
    xt = [k.sb("xt", [128, T + 3], F32) for _ in range(2)]
    ca = [k.sb("ca", [128, T], F32) for _ in range(2)]
    gi = k.sb("gi", [4, T], F32)
    nl = k.sb("nl", [4, T], F32)
    cum = k.sb("cum", [4, T], F32)
    lw = k.sb("lw", [4, T], F32)
    wT = k.sb("wT", [4, T], F32)
    enx = k.sb("enx", [4, T], F32)
    g4 = {n_: k.sb(n_, [4, NC_], F32) for n_ in ("ntot", "mx", "mnew", "mp", "dec", "negm", "en")}
    wtm = [k.sb("wtm", [128, 4], F32) for _ in range(NS)]
    entm = [k.sb("entm", [64, 4], F32) for _ in range(NC_)]
    decb = k.sb("decb", [128, 4, NC_], F32)
    hc = [k.sb("hc", [64, 512], F32) for _ in range(2)]
    hnb = [k.sb("hnb", [64, 512], BF16) for _ in range(2)]
    st6 = k.sb("st6", [64, 6], F32)
    mv = k.sb("mv", [64, 2], F32)
    den = k.sb("den", [64, 1], F32)
    tmp64 = [k.sb("tmp64", [128, 64], F32) for _ in range(2)]
    pA = [k.ps("pA", [128, 512]) for _ in range(2)]
    pU = [k.ps("pU", [128, 512]) for _ in range(2)]
    pN = k.ps("pN", [128, 512])
    pM = k.ps("pM", [128, 512])
    pT = k.ps("pT", [128, 512], BF16)
    pst = k.ps("pst", [128, 512])
    pai = [0]

    def nextA():
        pai[0] += 1
        return pA[pai[0] % 2]

    wq = [0]

    def load_piece(src, rows=None, cols=None):
        b = wbuf[wq[0] % 3]
        wq[0] += 1
        t = W[src].t
        if cols is not None:
            ap = t[:, cols[0]:cols[1]].rearrange("(kc p) n -> p kc n", p=128)
        else:
            ap = t[rows[0]:rows[1], :].rearrange("(kc p) n -> p kc n", p=128)
        k.dma("pool", b[:, :, :], W[src].v(ap))
        return b

    for it in range(L // T):
        t0 = it * T
        load_x_tile(c, "sp", x32, xin, t0, T)
        rmsnorm_mod(c, x32, T, layer, 0, sq, rstd, pst, out_bf=hbf, off=0, cp_eng="dve")
        load_x_tile(c, "sp", x32, xin, t0, T)
        for pc in range(4):
            wb = load_piece("w_up", cols=(pc * 1024, (pc + 1) * 1024))
            for cc in range(8):
                ch = (pc % 2) * 8 + cc
                p = nextA()
                k.mm(p, [(p[:, :T], wb[:, kc, cc * 128:(cc + 1) * 128], hbf[:, kc, :], kc == 0, kc == 7)
                         for kc in range(8)])
                if pc < 2:
                    x_ = xt[ch % 2]
                    k.copy("dve", x_[:, 0:3], halo[:, ch, :])
                    k.copy("act", x_[:, 3:T + 3], p[:, :T])
                    k.copy("dve", halo[:, ch, :], x_[:, T:T + 3])
                    k.copy("dve", xmb[:, ch, :], x_[:, 3:T + 3])
                    a_ = ca[ch % 2]
                    k.ts("dve", a_[:, :], x_[:, 3:T + 3], convl[:, ch, 3:4], convl[:, ch, 4:5], ALU.mult, ALU.add)
                    for j in range(3):
                        k.stt(a_[:, :], x_[:, j:j + T], convl[:, ch, j:j + 1], a_[:, :], ALU.mult, ALU.add)
                    k.act(xcb[:, ch, :], a_[:, :], AF.Silu)
                else:
                    k.act(zsb[:, ch, :], p[:, :T], AF.Silu)
        for gsel, dst in ((0, gi), (1, nl)):
            p = pM
            mms = []
            for ch in range(16):
                mms.append((p[:4, :T], weffc[:, ch, gsel * 4:gsel * 4 + 4], xcb[:, ch, :], ch == 0, False))
                mms.append((p[:4, :T], weffm[:, ch, gsel * 4:gsel * 4 + 4], xmb[:, ch, :], False, False))
            mms.append((p[:4, :T], bif[0:1, gsel * 4:gsel * 4 + 4], c.ones[0:1, :T], False, True))
            k.mm(p, mms)
            if gsel == 0:
                k.copy("dve", gi[:, :], p[:4, :T])
            else:
                k.act(nl[:, :], p[:4, :T], AF.Exp, scale=-1.0)
                k.act(nl[:, :], nl[:, :], AF.Ln, bias=1.0)
        k.op("dve", "tensor_tensor_scan", cum[:, :], [cmask[:, :], nl[:, :], 0.0, ALU.mult, ALU.add])
        cum3 = cum.v(cum.t[:, :].rearrange("p (c t) -> p c t", t=64))
        k.ts("dve", g4["ntot"][:, :], cum.v(cum.t[:, :].rearrange("p (c t) -> p c t", t=64)[:, :, 63]), -1.0, None, ALU.mult)
        for cc in range(NC_):
            sl = slice(cc * 64, (cc + 1) * 64)
            k.stt(lw[:, sl], cum[:, sl], g4["ntot"][:, cc:cc + 1], gi[:, sl], ALU.add, ALU.add)
        k.op("dve", "tensor_reduce", g4["mx"][:, :], [lw.v(lw.t[:, :].rearrange("p (c t) -> p c t", t=64))],
             axis=AX.X, op=ALU.max)
        k.op("dve", "tensor_tensor_scan", g4["mnew"][:, :], [g4["ntot"][:, :], g4["mx"][:, :], mcar[:, 0:1], ALU.add, ALU.max])
        k.copy("dve", g4["mp"][:, 0:1], mcar[:, :])
        k.copy("dve", g4["mp"][:, 1:NC_], g4["mnew"][:, 0:NC_ - 1])
        k.copy("dve", mcar[:, :], g4["mnew"][:, NC_ - 1:NC_])
        k.tt("dve", g4["dec"][:, :], g4["ntot"][:, :], g4["mp"][:, :], ALU.add)
        k.tt("dve", g4["dec"][:, :], g4["dec"][:, :], g4["mnew"][:, :], ALU.subtract)
        k.act(g4["dec"][:, :], g4["dec"][:, :], AF.Exp)
        k.ts("dve", g4["negm"][:, :], g4["mnew"][:, :], -1.0, None, ALU.mult)
        k.act(g4["en"][:, :], g4["negm"][:, :], AF.Exp)
        for cc in range(NC_):
            sl = slice(cc * 64, (cc + 1) * 64)
            k.act(wT[:, sl], lw[:, sl], AF.Exp, bias=g4["negm"][:, cc:cc + 1], scale=1.0)
            k.ts("dve", enx[:, sl], c.ones[0:4, 0:64], g4["en"][:, cc:cc + 1], None, ALU.mult)
        for s_ in range(NS):
            k.mm(pM, [(pM[:, 32 + 4 * s_:36 + 4 * s_], wT[:, s_ * 128:(s_ + 1) * 128], c.ident[0:4, 0:4], True, True)],
                 transpose=True)
            k.ts("dve", wtm[s_][:, :], pM[:, 32 + 4 * s_:36 + 4 * s_], DH ** -0.5, None, ALU.mult)
        for cc in range(NC_):
            k.mm(pM, [(pM[:64, 64 + 4 * cc:68 + 4 * cc], enx[:, cc * 64:(cc + 1) * 64], c.ident[0:4, 0:4], True, True)],
                 transpose=True)
            k.copy("dve", entm[cc][:, :], pM[:64, 64 + 4 * cc:68 + 4 * cc])
        for h in range(4):
            k.mm(pM, [(pM[:, 128 + h * NC_:128 + (h + 1) * NC_], sel[:, h, :], g4["dec"][:, :], True, True)])
        k.copy("dve", decb.v(decb.t[:, :, :].rearrange("p h c -> p (h c)")), pM[:, 128:128 + 4 * NC_])
        for ch in range(16):
            p = nextA()
            k.mm(p, [(p[:, :T], bdq[:, ch, :], xcb[:, ch, :], True, True)])
            k.copy("act", qTb[:, ch, :], p[:, :T])
        for s_ in range(NS):
            for h in range(4):
                p = nextA()
                k.mm(p, [(p[:, kc * 128:(kc + 1) * 128], xcb[:, h * 4 + kc, s_ * 128:(s_ + 1) * 128], bdk[:, h * 4 + kc, :],
                          True, True) for kc in range(4)])
                k.ts("dve", kw[s_][:, h * 512:(h + 1) * 512], p[:, :], wtm[s_][:, h:h + 1], None, ALU.mult)
                p = nextA()
                k.mm(p, [(p[:, kc * 128:(kc + 1) * 128], xmb[:, h * 4 + kc, s_ * 128:(s_ + 1) * 128], bdv[:, h * 4 + kc, :],
                          True, True) for kc in range(4)])
                k.copy("act", vt[s_][:, h * 512:(h + 1) * 512], p[:, :])
        for ch in range(16):
            k.ts("dve", xcb[:, ch, :], xcb[:, ch, :], skipl[:, ch:ch + 1], None, ALU.mult)
        nu = 0
        for cc in range(NC_):
            s_, hf = cc // 2, cc % 2
            r0 = 64 * hf
            cs_ = slice(cc * 64, (cc + 1) * 64)
            for h in range(4):
                for kc in range(4):
                    pu_ = pU[nu % 2]
                    nu += 1
                    k.mm(pu_, [(pu_[:, :], kw[s_][r0:r0 + 64, h * 512 + kc * 128:h * 512 + (kc + 1) * 128],
                                vt[s_][r0:r0 + 64, h * 512:(h + 1) * 512], True, True)])
                    k.stt(cmat[h][kc][:, :], cmat[h][kc][:, :], decb[:, h, cc:cc + 1], pu_[:, :], ALU.mult, ALU.add)
                    k.copy("act", cb[h][kc][:, :], cmat[h][kc][:, :])
                k.mm(pM, [(pM[:, kc:kc + 1], kw[s_][r0:r0 + 64, h * 512 + kc * 128:h * 512 + (kc + 1) * 128],
                           onesb[r0:r0 + 64, 0:1], True, True) for kc in range(4)])
                k.stt(nvec[h][:, :], nvec[h][:, :], decb[:, h, cc:cc + 1], pM[:, 0:4], ALU.mult, ALU.add)
                k.copy("dve", nvb[h][:, :], nvec[h][:, :])
                k.mm(pN, [(pN[:64, :], qTb[:, h * 4 + kc, cs_], cb[h][kc][:, :], kc == 0, kc == 3) for kc in range(4)])
                k.mm(pM, [(pM[:64, 8:9], qTb[:, h * 4 + kc, cs_], nvb[h][:, kc:kc + 1], kc == 0, kc == 3)
                          for kc in range(4)])
                k.copy("dve", den[:, :], pM[:64, 8:9])
                k.stt(den[:, :], den[:, :], -1.0, den[:, :], ALU.mult, ALU.max)
                k.ts("dve", den[:, :], den[:, :], entm[cc][:, h:h + 1], None, ALU.max)
                k.op("dve", "reciprocal", den[:, :], [den[:, :]])
                hc_ = hc[h % 2]
                k.act(hc_[:, :], pN[:64, :], AF.Copy, scale=den[:, 0:1])
                k.op("dve", "bn_stats", st6[:, :], [hc_[:, :]])
                k.op("dve", "bn_aggr", mv[:, :], [st6[:, :]])
                k.ts("dve", mv[:, 1:2], mv[:, 1:2], EPS, None, ALU.add)
                k.act(mv[:, 1:2], mv[:, 1:2], AF.Sqrt)
                k.op("dve", "reciprocal", mv[:, 1:2], [mv[:, 1:2]])
                hb_ = hnb[h % 2]
                k.ts("dve", hb_[:, :], hc_[:, :], mv[:, 0:1], mv[:, 1:2], ALU.subtract, ALU.mult)
                k.mm(pT, [(pT[:, kc * 64:(kc + 1) * 64], hb_[:, kc * 128:(kc + 1) * 128], c.identb[0:64, 0:64], True, True)
                          for kc in range(4)], transpose=True)
                for kc in range(4):
                    ch = h * 4 + kc
                    t_ = tmp64[kc % 2]
                    k.stt(t_[:, :], pT[:, kc * 64:(kc + 1) * 64], gnl[:, ch:ch + 1], xcb[:, ch, cs_], ALU.mult, ALU.add)
                    k.tt("dve", zsb[:, ch, cs_], t_[:, :], zsb[:, ch, cs_], ALU.mult)
        wd0 = load_piece("w_down", rows=(0, 1024))
        wd1 = load_piece("w_down", rows=(1024, 2048))
        for n in range(8):
            p = nextA()
            mms = [(p[:, :T], wd0[:, kc, n * 128:(n + 1) * 128], zsb[:, kc, :], kc == 0, False) for kc in range(8)]
            mms += [(p[:, :T], wd1[:, kc, n * 128:(n + 1) * 128], zsb[:, 8 + kc, :], False, kc == 7) for kc in range(8)]
            k.mm(p, mms)
            k.stt(x32[n][:, :], p[:, :T], c.mod[:, layer, 16 + n:17 + n], x32[n][:, :], ALU.mult, ALU.add)
        store_x_tile(c, "sp", x32, xout, t0, T)
    k.stage_end()


def mlstm_host_layout(conv_w, conv_b, w_q, w_k, w_v, w_if, skip, g_norm):
    def bd(w, transpose):
        o = np.zeros((128, 16, 128), np.float32)
        for ch in range(16):
            for b in range(32):
                blk = w[ch * 32 + b]
                sl = slice(b * 4, b * 4 + 4)
                o[sl, ch, sl] = blk.T if transpose else blk
        return o
    conv_l = np.zeros((128, 16, 5), np.float32)
    conv_l[:, :, 0:4] = conv_w.reshape(4, 16, 128).transpose(2, 1, 0)
    conv_l[:, :, 4] = conv_b.reshape(16, 128).T
    return dict(bdq=bd(w_q, False), bdk=bd(w_k, False), bdv=bd(w_v, False),
                bdqT=bd(w_q, True), bdkT=bd(w_k, True), bdvT=bd(w_v, True),
                conv_l=conv_l,
                wif_l=np.ascontiguousarray(w_if.reshape(48, 128, 8).transpose(1, 0, 2)),
                skip_l=np.ascontiguousarray(skip.reshape(16, 128).T),
                gnorm_l=np.ascontiguousarray(g_norm.reshape(16, 128).T))


DEPTH = 4
SEQ = 8192
BATCH = 8


def prologue_stage(c, xd, xT, Wd):
    k = c.k
    L = c.L
    depth = c.depth
    k.stage_begin()
    cl = k.sb("cl", [128, 8], F32)
    wa = [k.sb("wa", [128, 8, 1024], F32) for _ in range(2)]
    bada = k.sb("bada", [128, depth, 48], F32)
    gl = k.sb("gl", [128, depth + 1, 16], F32)
    pm = k.ps("pm", [128, 512])
    k.dma("sp", cl[:, :], Wd["c_l"].v(Wd["c_l"].t))
    k.dma("sp", bada[:, :, :], Wd["b_ada_l"].v(Wd["b_ada_l"].t))
    k.dma("sp", gl[:, :, :], Wd["g_l"].v(Wd["g_l"].t))
    k.act(cl[:, :], cl[:, :], AF.Silu)
    n = 0
    for l in range(depth):
        for pc in range(6):
            w_ = wa[n % 2]
            n += 1
            k.dma("sp", w_[:, :, :], Wd["w_ada"].v(Wd["w_ada"].t[l][:, pc * 1024:(pc + 1) * 1024]
                                                  .rearrange("(kc p) n -> p kc n", p=128)))
            for nn in range(8):
                col = l * 48 + pc * 8 + nn
                k.mm(pm, [(pm[:, col:col + 1], w_[:, kc, nn * 128:(nn + 1) * 128], cl[:, kc:kc + 1], kc == 0, kc == 7)
                          for kc in range(8)])
    k.tt("dve", c.mod.v(c.mod.t[:, 0:depth, :].rearrange("p l n -> p (l n)")), pm[:, 0:depth * 48],
         bada.v(bada.t[:, :, :].rearrange("p l n -> p (l n)")), ALU.add)
    for l in range(depth):
        k.stt(c.geff[:, l, 0:8], c.mod[:, l, 8:16], 1.0, gl[:, l, 0:8], ALU.add, ALU.mult)
        k.stt(c.geff[:, l, 8:16], c.mod[:, l, 32:40], 1.0, gl[:, l, 8:16], ALU.add, ALU.mult)
    k.copy("dve", c.geff[:, depth, 0:8], gl[:, depth, 0:8])
    k.memset("dve", c.mod[:, depth, :], 0.0)
    xtm = [k.sb("xtm", [128, 1024], F32) for _ in range(4)]
    x32 = [k.sb("x32", [128, 512], F32) for _ in range(8)]
    pt = [k.ps("pt", [128, 512]) for _ in range(2)]
    for it in range(L // 512):
        for j in range(4):
            r0 = it * 512 + j * 128
            k.dma("sp", xtm[j][:, :], xd.v(xd.t[r0:r0 + 128, :]))
        for ch in range(8):
            p = pt[ch % 2]
            k.mm(p, [(p[:, j * 128:(j + 1) * 128], xtm[j][:, ch * 128:(ch + 1) * 128], c.ident[:, :], True, True)
                     for j in range(4)], transpose=True)
            k.copy("act" if ch % 2 else "dve", x32[ch][:, :], p[:, :])
        store_x_tile(c, "sp", x32, xT, it * 512, 512)
    k.stage_end()


def final_stage(c, xT, outd):
    k = c.k
    L = c.L
    k.stage_begin()
    x32 = [k.sb("x32", [128, 512], F32) for _ in range(8)]
    sq = [k.sb("sq", [128, 512], F32) for _ in range(2)]
    rstd = k.sb("rstd", [128, 512], F32)
    ytm = [k.sb("ytm", [128, 1024], F32) for _ in range(2)]
    pst = k.ps("pst", [128, 512])
    pt = [k.ps("pt", [128, 512]) for _ in range(2)]
    n = 0
    for it in range(L // 512):
        load_x_tile(c, "sp", x32, xT, it * 512, 512)
        rmsnorm_mod(c, x32, 512, c.depth, 0, sq, rstd, pst)
        for j in range(4):
            y_ = ytm[j % 2]
            for half in range(2):
                p = pt[n % 2]
                n += 1
                k.mm(p, [(p[:, cc * 128:(cc + 1) * 128], x32[half * 4 + cc][:, j * 128:(j + 1) * 128], c.ident[:, :],
                          True, True) for cc in range(4)], transpose=True)
                k.copy("act" if half else "dve", y_[:, half * 512:(half + 1) * 512], p[:, :])
            r0 = it * 512 + j * 128
            k.dma("sp", outd.v(outd.t[r0:r0 + 128, :]), y_[:, :])
    k.stage_end()


def build_program(L=SEQ, depth=DEPTH, ne=NE):
    nc = bass.Bass("TRN2", target_bir_lowering=False)
    k = K(nc)
    c = make_ctx(k, L, depth + 1)
    c.depth = depth
    make_chunk_masks(c)
    ext = lambda name, shape: k.dram(name, shape, F32, kind="ExternalInput")
    xd = ext("x", [L, D])
    outd = k.dram("out", [L, D], F32, kind="ExternalOutput")
    xa = k.dram("xa", [8, 128, L], F32)
    xb = k.dram("xb", [8, 128, L], F32)
    NA, NB_, NC3 = (depth + 2) // 3, (depth + 1) // 3, depth // 3
    Wd = dict(c_l=ext("c_l", [128, 8]), b_ada_l=ext("b_ada_l", [128, depth, 48]), g_l=ext("g_l", [128, depth + 1, 16]),
              w_ada=ext("w_ada", [depth, D, 6 * D]))
    gla = dict(w_in=ext("gla_w_in", [NA, D, 3072]), w_gk1=ext("gla_w_gk1", [NA, D, 16]),
               w_gk2=ext("gla_w_gk2", [NA, 16, 512]), b_gk=ext("gla_b_gk", [NA, 1, 512]),
               g_onorm_l=ext("gla_g_onorm_l", [NA, 128, 2]), w_out=ext("gla_w_out", [NA, D, D]))
    ml = {}
    if NB_:
        ml = dict(w_up=ext("ml_w_up", [NB_, D, 4096]), w_down=ext("ml_w_down", [NB_, 2048, D]),
                  b_if=ext("ml_b_if", [NB_, 1, 8]))
        for n_, shp in (("bdq", [128, 16, 128]), ("bdk", [128, 16, 128]), ("bdv", [128, 16, 128]),
                        ("bdqT", [128, 16, 128]), ("bdkT", [128, 16, 128]), ("bdvT", [128, 16, 128]),
                        ("conv_l", [128, 16, 5]), ("wif_l", [128, 48, 8]), ("skip_l", [128, 16]), ("gnorm_l", [128, 16])):
            ml[n_] = ext("ml_" + n_, [NB_] + shp)
    s5 = {}
    if NC3:
        s5 = dict(w_in=ext("s5_w_in", [NC3, D, D]), w_out=ext("s5_w_out", [NC3, D, 2 * D]), d_l=ext("s5_d_l", [NC3, 128, 8]))
        for n_ in ("Ablk_re", "Ablk_im", "Lblk", "Bblk_re", "Bblk_im", "Cblk_re", "Cblk_im"):
            s5[n_] = ext("s5_" + n_, [NC3, 128, 32 * 128])
        for n_ in ("Acol_re", "Acol_im", "Lcol"):
            s5[n_] = ext("s5_" + n_, [NC3, 128, 32])
    moe = dict(w_router=ext("moe_w_router", [depth, D, 32]), b_router=ext("moe_b_router", [depth, 1, 32]),
               w_gu=ext("moe_w_gu", [depth, ne, D, 2 * D]), b_gu_l=ext("moe_b_gu_l", [depth, 128, ne, 16]),
               w_down=ext("moe_w_down", [depth, ne, D, D]), b_down=ext("moe_b_down", [depth, ne, D]))

    def sub(dct, j):
        return {n_: Buf(k, b.t[j], "dram") for n_, b in dct.items()}

    prologue_stage(c, xd, xa, Wd)
    for i in range(depth):
        kind, j = i % 3, i // 3
        if kind == 0:
            gla_stage(c, i, xa, xb, sub(gla, j))
        elif kind == 1:
            mlstm_stage(c, i, xa, xb, sub(ml, j))
        else:
            s5_stage(c, i, xa, xb, sub(s5, j))
        moe_stage(c, i, xb, xa, sub(moe, i), ne=ne)
    final_stage(c, xa, outd)
    k.finish([outd])
    return nc, k


def host_layouts(inp, depth=DEPTH):
    f32 = lambda a: np.ascontiguousarray(np.asarray(a, dtype=np.float32))
    col = lambda v, n: f32(np.asarray(v).reshape(n, 128).T)
    sh = {}
    sh["b_ada_l"] = f32(np.asarray(inp["b_ada"]).reshape(depth, 48, 128).transpose(2, 0, 1))
    gl = np.zeros((128, depth + 1, 16), np.float32)
    for l in range(depth):
        gl[:, l, 0:8] = col(inp["g_mix"][l], 8)
        gl[:, l, 8:16] = col(inp["g_ffn"][l], 8)
    gl[:, depth, 0:8] = col(inp["g_final"], 8)
    sh["g_l"] = gl
    sh["w_ada"] = f32(inp["w_ada"])
    for n_ in ("gla_w_in", "gla_w_gk1", "gla_w_gk2", "gla_w_out", "ml_w_up", "ml_w_down", "s5_w_in", "s5_w_out",
               "moe_w_router", "moe_w_gu", "moe_w_down", "moe_b_down"):
        sh[n_] = f32(inp[n_])
    na = np.asarray(inp["gla_b_gk"]).shape[0]
    sh["gla_b_gk"] = f32(np.asarray(inp["gla_b_gk"]).reshape(na, 1, 512))
    sh["gla_g_onorm_l"] = f32(np.stack([col(g, 2) for g in np.asarray(inp["gla_g_onorm"])]))
    nb = np.asarray(inp["ml_w_up"]).shape[0]
    sh["ml_b_if"] = f32(np.asarray(inp["ml_b_if"]).reshape(nb, 1, 8))
    mls = [mlstm_host_layout(*[np.asarray(inp["ml_" + n_][j]) for n_ in
                               ("conv_w", "conv_b", "w_q", "w_k", "w_v", "w_if", "skip", "g_norm")]) for j in range(nb)]
    for n_ in mls[0]:
        sh["ml_" + n_] = f32(np.stack([m[n_] for m in mls]))
    n3 = np.asarray(inp["s5_w_in"]).shape[0]
    s5s = [s5_host_layout(*[np.asarray(inp["s5_" + n_][j]) for n_ in
                            ("a_re", "a_im", "log_dt", "b_re", "b_im", "c_re", "c_im", "d")]) for j in range(n3)]
    for n_ in s5s[0]:
        sh["s5_" + n_] = f32(np.stack([m[n_] for m in s5s]))
    sh["moe_b_router"] = f32(np.asarray(inp["moe_b_router"]).reshape(depth, 1, 32))
    bgu = np.asarray(inp["moe_b_gu"])
    ne = bgu.shape[1]
    sh["moe_b_gu_l"] = f32(bgu.reshape(depth, ne, 16, 128).transpose(0, 3, 1, 2))
    return sh


_PROG = {}


def kernel(**inputs):
    x = np.asarray(inputs["x"], dtype=np.float32)
    cvec = np.asarray(inputs["c"], dtype=np.float32)
    B, L, _ = x.shape
    key = (L,)
    if key not in _PROG:
        _PROG[key] = build_program(L=L)[0]
    nc = _PROG[key]
    sh = host_layouts(inputs)
    in_maps = []
    for b in range(B):
        m = dict(sh)
        m["x"] = np.ascontiguousarray(x[b])
        m["c_l"] = np.ascontiguousarray(cvec[b].reshape(8, 128).T)
        in_maps.append(m)
    res = run_bass_kernel_spmd(nc, in_maps, core_ids=list(range(B)))
    return np.stack([np.asarray(r["out"], dtype=np.float32) for r in res.results], axis=0)
```

```python
import numpy as np
import concourse.bass as bass
import concourse.mybir as mybir

F32 = mybir.dt.float32
BF16 = mybir.dt.bfloat16
AF = mybir.ActivationFunctionType
ALU = mybir.AluOpType
AX = mybir.AxisListType

SAME_ENGINE_SYNC = True


class V:
    __slots__ = ("b", "ap")

    def __init__(self, b, ap):
        self.b = b
        self.ap = ap


class Buf:
    def __init__(self, k, t, kind):
        self.k = k
        self.t = t
        self.kind = kind
        self.w = None
        self.r = {}
        self.ds = {}

    def __getitem__(self, idx):
        return V(self, self.t[idx])

    def v(self, ap):
        return V(self, ap)


class K:
    def __init__(self, nc):
        self.nc = nc
        self.E = dict(pe=nc.tensor, act=nc.scalar, dve=nc.vector, pool=nc.gpsimd, sp=nc.sync)
        self.sem = {k: nc.alloc_semaphore("sem_" + k) for k in self.E}
        self.cnt = {k: 0 for k in self.E}
        self.waited = {k: {} for k in self.E}
        self.semowner = {}
        self.n = 0
        self.ninstr = 0
        self.guards = []
        self.stage_bufs = []
        self.dsem_pool = {'hw': [], 'sw': []}
        self.all_dsem_bufs = []

    def sb(self, name, shape, dt=F32):
        self.n += 1
        name = "%s_%d" % (name, self.n)
        if self.guards:
            g = self.nc.sbuf_tensor(name, list(shape), dt)
            t = g.__enter__()
            self.guards[-1].append(g)
        else:
            t = self.nc.alloc_sbuf_tensor(name, list(shape), dt)
        b = Buf(self, t, "sb")
        if self.stage_bufs:
            self.stage_bufs[-1].append(b)
        return b

    def ps(self, name, shape, dt=F32):
        self.n += 1
        name = "%s_%d" % (name, self.n)
        if self.guards:
            g = self.nc.psum_tensor(name, list(shape), dt)
            t = g.__enter__()
            self.guards[-1].append(g)
        else:
            t = self.nc.alloc_psum_tensor(name, list(shape), dt)
        b = Buf(self, t, "ps")
        if self.stage_bufs:
            self.stage_bufs[-1].append(b)
        return b

    def stage_begin(self):
        self.guards.append([])
        self.stage_bufs.append([])

    def stage_end(self):
        self.barrier()
        for b in self.stage_bufs.pop():
            for kind, sc in b.ds.items():
                self.dsem_pool[kind].append(sc)
            b.ds = {}
        for g in reversed(self.guards.pop()):
            g.__exit__(None, None, None)

    def barrier(self):
        evs = [(self.sem[e], self.cnt[e], e) for e in self.E if self.cnt[e] > 0]
        dmas = [(sc[0], 16 * sc[1], "dma") for b in self.all_dsem_bufs for sc in b.ds.values() if sc[1] > 0]
        for e in self.E:
            for ev in evs:
                if ev[2] != e:
                    self._wait(e, ev)
            for ev in dmas:
                self._wait(e, ev)

    def dram(self, name, shape, dt=F32, kind="Internal"):
        return Buf(self, self.nc.dram_tensor(name, list(shape), dt, kind=kind).ap(), "dram")

    def region(self):
        return Buf(self, None, "dram")

    def _wait(self, e, ev):
        if ev is None:
            return
        sem, val, src = ev
        if src == e and (e == "pe" or not SAME_ENGINE_SYNC):
            return
        if src == "dma":
            val = 16 * self.semowner[id(sem)][1]
        w = self.waited[e]
        key = id(sem)
        if w.get(key, 0) >= val:
            return
        self.E[e].wait_ge(sem, val)
        w[key] = val

    def _deps(self, e, reads, writes):
        for b in reads:
            self._wait(e, b.w)
            if b.kind == "ps":
                for ke, ev in b.r.items():
                    if ke != e:
                        self._wait(e, ev)
        for b in writes:
            self._wait(e, b.w)
            for ev in b.r.values():
                self._wait(e, ev)

    def _commit(self, e, ins, reads, writes):
        self.cnt[e] += 1
        ev = (self.sem[e], self.cnt[e], e)
        ins.then_inc(self.sem[e], 1)
        for b in writes:
            b.w = ev
            b.r = {}
        ws = set(id(b) for b in writes)
        for b in reads:
            if id(b) not in ws:
                b.r[e] = ev
        self.ninstr += 1

    @staticmethod
    def _split(ops):
        bufs, aps = [], []
        for o in ops:
            if isinstance(o, V):
                bufs.append(o.b)
                aps.append(o.ap)
            else:
                aps.append(o)
        return bufs, aps

    def op(self, e, name, out, ins, extra_reads=(), **kw):
        rb, raps = self._split(ins)
        rb = rb + [x.b for x in extra_reads]
        kwv = {}
        wb = [out.b]
        for kk, vv in kw.items():
            if isinstance(vv, V):
                if kk == "accum_out":
                    wb.append(vv.b)
                else:
                    rb.append(vv.b)
                kwv[kk] = vv.ap
            else:
                kwv[kk] = vv
        self._deps(e, rb, wb)
        ins_ = getattr(self.E[e], name)(out.ap, *raps, **kwv)
        self._commit(e, ins_, rb, wb)
        return ins_

    def act(self, out, in_, func, bias=None, scale=None, e="act", accum_out=None):
        kw = {}
        if bias is not None:
            kw["bias"] = bias
        if scale is not None:
            kw["scale"] = scale
        rb = [in_.b]
        kwv = {}
        for kk, vv in kw.items():
            if isinstance(vv, V):
                rb.append(vv.b)
                kwv[kk] = vv.ap
            else:
                kwv[kk] = vv
        wb = [out.b]
        if accum_out is not None:
            wb.append(accum_out.b)
            kwv["accum_out"] = accum_out.ap
        self._deps("act", rb, wb)
        ins_ = self.nc.scalar.activation(out=out.ap, in_=in_.ap, func=func, **kwv)
        self._commit("act", ins_, rb, wb)

    def ts(self, e, out, in0, s1, s2, op0, op1=None):
        if op1 is None:
            return self.op(e, "tensor_scalar", out, [in0, s1, None, op0])
        return self.op(e, "tensor_scalar", out, [in0, s1, s2, op0, op1])

    def stt(self, out, in0, scalar, in1, op0, op1, e="dve", **kw):
        return self.op(e, "scalar_tensor_tensor", out, [in0, scalar, in1, op0, op1], **kw)

    def tt(self, e, out, in0, in1, op):
        return self.op(e, "tensor_tensor", out, [in0, in1, op])

    def copy(self, e, out, in_):
        if e == "act":
            return self.act(out, in_, AF.Copy)
        return self.op(e, "tensor_copy", out, [in_])

    def memset(self, e, out, val):
        self._deps(e, [], [out.b])
        ins_ = self.E[e].memset(out.ap, val)
        self._commit(e, ins_, [], [out.b])

    def mm(self, outbuf, mms, transpose=False):
        rb = []
        seen = set()
        for (o, l, r, st, sp) in mms:
            for x in (l, r):
                if id(x.b) not in seen:
                    seen.add(id(x.b))
                    rb.append(x.b)
        self._deps("pe", rb, [outbuf])
        ins_ = None
        for (o, l, r, st, sp) in mms:
            if transpose:
                ins_ = self.nc.tensor.transpose(o.ap, l.ap, r.ap)
            else:
                ins_ = self.nc.tensor.matmul(o.ap, l.ap, r.ap, start=st, stop=sp)
            self.ninstr += 1
        self._commit("pe", ins_, rb, [outbuf])

    def _dsem(self, sbuf, kind):
        if kind not in sbuf.ds:
            if self.dsem_pool[kind]:
                sc = self.dsem_pool[kind].pop()
            else:
                sc = [self.nc.alloc_semaphore("dsem%d" % self.n), 0]
                self.n += 1
            sbuf.ds[kind] = sc
            self.semowner[id(sc[0])] = sc
            if sbuf not in self.all_dsem_bufs:
                self.all_dsem_bufs.append(sbuf)
        sc = sbuf.ds[kind]
        sc[1] += 1
        return sc

    def dma(self, q, out, in_, sem_buf=None, **kw):
        ob, ib = out.b, in_.b
        self._deps(q, [ib], [ob])
        sbuf = sem_buf or (ob if ob.kind == "sb" else ib)
        sc = self._dsem(sbuf, "sw" if q == "pool" else "hw")
        ins_ = self.E[q].dma_start(out=out.ap, in_=in_.ap, **kw)
        ins_.then_inc(sc[0], 16)
        ev = (sc[0], 16 * sc[1], "dma")
        ob.w = ev
        ob.r = {}
        if ib is not ob:
            ib.r[id(sc[0])] = ev
        self.ninstr += 1

    def idma(self, out, in_, idx, scatter=False, **kw):
        ob, ib = out.b, in_.b
        self._deps("pool", [ib, idx.b], [ob])
        sbuf = ob if ob.kind == "sb" else ib
        sc = self._dsem(sbuf, "sw")
        off = bass.IndirectOffsetOnAxis(ap=idx.ap, axis=0)
        if scatter:
            ins_ = self.nc.gpsimd.indirect_dma_start(out=out.ap, out_offset=off, in_=in_.ap, in_offset=None, **kw)
        else:
            ins_ = self.nc.gpsimd.indirect_dma_start(out=out.ap, out_offset=None, in_=in_.ap, in_offset=off, **kw)
        ins_.then_inc(sc[0], 16)
        ev = (sc[0], 16 * sc[1], "dma")
        ob.w = ev
        ob.r = {}
        ib.r[id(sc[0])] = ev
        idx.b.r[id(sc[0])] = ev
        self.ninstr += 1

    def vload(self, e, v, lo, hi):
        self._deps(e, [v.b], [])
        return self.E[e].value_load(v.ap, min_val=lo, max_val=hi)

    def finish(self, bufs, e="sp"):
        for b in bufs:
            self._wait(e, b.w)
            for ev in b.r.values():
                self._wait(e, ev)

from concourse.bass_utils import run_bass_kernel_spmd

D = 1024
EPS = 1e-6
NE = 32
ALPHA = 1.702


class Ctx:
    pass


def make_ctx(k, L, depth):
    c = Ctx()
    c.k = k
    c.L = L
    c.depth = depth
    c.ones = k.sb("ones", [128, 512], F32)
    c.ident = k.sb("ident", [128, 128], F32)
    c.identb = k.sb("identb", [128, 128], BF16)
    c.mod = k.sb("mod", [128, depth, 48], F32)
    c.geff = k.sb("geff", [128, depth, 16], F32)
    k.memset("dve", c.ones[:, 0:512], 1.0)
    k.op("pool", "affine_select", c.ident[:, :], [c.ones[:, 0:128]], pattern=[[-1, 128]],
         compare_op=ALU.is_equal, fill=0.0, base=0, channel_multiplier=1)
    k.copy("dve", c.identb[:, :], c.ident[:, :])
    return c


def load_x_tile(c, q, x32, xin, t0, T):
    for ch in range(8):
        c.k.dma(q, x32[ch][:, :T], xin.v(xin.t[ch, :, t0:t0 + T]))


def store_x_tile(c, q, x32, xout, t0, T):
    for ch in range(8):
        c.k.dma(q, xout.v(xout.t[ch, :, t0:t0 + T]), x32[ch][:, :T])


def rmsnorm_mod(c, x32, T, layer, which, sq, rstd, ps_stat, out_bf=None, off=0, cp_eng="pool"):
    k = c.k
    for ch in range(8):
        s = sq[ch % len(sq)]
        k.act(s[:, :T], x32[ch][:, :T], AF.Square)
        k.mm(ps_stat, [(ps_stat[:, :T], c.ones[:, 0:128], s[:, :T], ch == 0, ch == 7)])
    k.ts("dve", rstd[:, :T], ps_stat[:, :T], 1.0 / D, EPS, ALU.mult, ALU.add)
    k.act(rstd[:, :T], rstd[:, :T], AF.Sqrt)
    k.op("dve", "reciprocal", rstd[:, :T], [rstd[:, :T]])
    shb = 0 if which == 0 else 24
    for ch in range(8):
        k.stt(x32[ch][:, :T], x32[ch][:, :T], c.geff[:, layer, which * 8 + ch:which * 8 + ch + 1],
              rstd[:, :T], ALU.mult, ALU.mult)
        k.act(x32[ch][:, :T], x32[ch][:, :T], AF.Identity,
              bias=c.mod[:, layer, shb + ch:shb + ch + 1])
        if out_bf is not None:
            k.copy(cp_eng, out_bf[:, ch, off:off + T], x32[ch][:, :T])


def moe_stage(c, layer, xin, xout, W, ne=NE):
    k = c.k
    L = c.L
    ST = min(1024, L)
    NSUB = ST // 128
    NTS = ST // 512
    k.stage_begin()
    wgu = [k.sb("wgu", [128, 8, 2048], BF16) for _ in range(2)]
    wdn = [k.sb("wdn", [128, 8, 1024], BF16) for _ in range(2)]
    acc = [k.sb("acc", [128, 1024], F32) for _ in range(NSUB)]
    hbf = k.sb("hbf", [128, 8, ST], BF16)
    x32 = [k.sb("x32", [128, 512], F32) for _ in range(8)]
    actT = [k.sb("actT", [128, 8, 512], BF16) for _ in range(2)]
    tg = [k.sb("tg", [128, 512], F32) for _ in range(2)]
    tt_ = [k.sb("tt", [128, 512], F32) for _ in range(2)]
    tu = [k.sb("tu", [128, 512], F32) for _ in range(2)]
    sq = tg
    rstd = tt_[0]
    wr = k.sb("wr", [128, 8, 32], F32)
    br = k.sb("br", [1, 32], F32)
    bgu = k.sb("bgu", [128, ne, 16], F32)
    bdn = k.sb("bdn", [32, 1024], F32)
    comb = k.sb("comb", [128, NSUB, 32], F32)
    combs = k.sb("combs", [128, NSUB, 32], F32)
    combT = k.sb("combT", [32, ST], F32)
    lg = k.sb("lg", [128, 32], F32)
    m8 = k.sb("m8", [128, 8], F32)
    negm = k.sb("negm", [128, 1], F32)
    mask = k.sb("mask", [128, 32], F32)
    ex = k.sb("ex", [128, 32], F32)
    ssum = k.sb("ssum", [128, 1], F32)
    pg = [k.ps("pg", [128, 512]) for _ in range(2)]
    pu = [k.ps("pu", [128, 512]) for _ in range(2)]
    pd = [k.ps("pd", [128, 512]) for _ in range(2)]
    pm = [k.ps("pm", [128, 512]) for _ in range(2)]

    k.dma("sp", wr[:, :, :], W["w_router"].v(W["w_router"].t.rearrange("(kc p) e -> p kc e", p=128)))
    k.dma("sp", br[:, :], W["b_router"].v(W["b_router"].t))
    k.dma("sp", bgu[:, :, :], W["b_gu_l"].v(W["b_gu_l"].t))
    k.dma("sp", bdn[:ne, :], W["b_down"].v(W["b_down"].t))
    k.ts("dve", bgu[:, :, 8:16], bgu[:, :, 8:16], 1.0, None, ALU.add)

    def load_w(gi):
        e = gi % ne
        k.dma("pool", wgu[gi % 2][:, :, :],
              W["w_gu"].v(W["w_gu"].t[e].rearrange("(kc p) n -> p kc n", p=128)))
        k.dma("pool", wdn[gi % 2][:, :, :],
              W["w_down"].v(W["w_down"].t[e].rearrange("(kc p) n -> p kc n", p=128)))

    nst = L // ST
    load_w(0)
    pmi = 0
    cnt = 0
    for s in range(nst):
        for ts_ in range(NTS):
            t0 = s * ST + ts_ * 512
            load_x_tile(c, "sp", x32, xin, t0, 512)
            rmsnorm_mod(c, x32, 512, layer, 1, sq, rstd, pm[0], out_bf=hbf, off=ts_ * 512, cp_eng="dve")
            for j in range(4):
                sub = ts_ * 4 + j
                p = pm[1]
                mms = [(p[:, :32], x32[ch][:, j * 128:(j + 1) * 128], wr[:, ch, :], ch == 0, False)
                       for ch in range(8)]
                mms.append((p[:, :32], c.ones[0:1, 0:128], br[0:1, :], False, True))
                k.mm(p, mms)
                k.copy("dve", lg[:, :], p[:, :32])
                k.op("dve", "max", m8[:, :], [lg[:, :]])
                k.ts("dve", mask[:, :], lg[:, :], m8[:, 3:4], None, ALU.is_ge)
                k.ts("dve", negm[:, :], m8[:, 0:1], -1.0, None, ALU.mult)
                k.act(ex[:, :], lg[:, :], AF.Exp, bias=negm[:, 0:1], scale=1.0)
                k.stt(ex[:, :], ex[:, :], 1.0, mask[:, :], ALU.mult, ALU.mult, accum_out=ssum[:, :])
                k.op("dve", "reciprocal", ssum[:, :], [ssum[:, :]])
                k.ts("dve", comb[:, sub, :], ex[:, :], ssum[:, 0:1], None, ALU.mult)
                k.ts("dve", combs[:, sub, :], ex[:, :], ssum[:, 0:1], 1.0 / ALPHA, ALU.mult, ALU.mult)
                k.mm(p, [(p[:32, 128:256], comb[:, sub, :], c.ident[:, :], True, True)], transpose=True)
                k.copy("dve", combT[:, sub * 128:(sub + 1) * 128], p[:32, 128:256])
        for e in range(ne):
            gi = s * ne + e
            if gi + 1 < nst * ne:
                load_w(gi + 1)
            wg, wd = wgu[gi % 2], wdn[gi % 2]
            for ts_ in range(NTS):
                aT = actT[cnt % 2]
                cnt += 1
                for n in range(8):
                    a, b = pg[n % 2], pu[n % 2]
                    k.mm(a, [(a[:, :], wg[:, kc, n * 128:(n + 1) * 128], hbf[:, kc, ts_ * 512:(ts_ + 1) * 512],
                              kc == 0, kc == 7) for kc in range(8)])
                    k.mm(b, [(b[:, :], wg[:, kc, 1024 + n * 128:1024 + (n + 1) * 128],
                              hbf[:, kc, ts_ * 512:(ts_ + 1) * 512], kc == 0, kc == 7) for kc in range(8)])
                    g, t, u = tg[n % 2], tt_[n % 2], tu[n % 2]
                    k.ts("dve", g[:, :], a[:, :], bgu[:, e, n:n + 1], 7.0, ALU.add, ALU.min)
                    k.act(t[:, :], g[:, :], AF.Silu, scale=ALPHA)
                    k.ts("dve", u[:, :], b[:, :], bgu[:, e, 8 + n:9 + n], -6.0, ALU.add, ALU.max)
                    k.stt(aT[:, n, :], u[:, :], 8.0, t[:, :], ALU.min, ALU.mult)
                for j in range(4):
                    sub = ts_ * 4 + j
                    for half in range(2):
                        p = pd[(j * 2 + half) % 2]
                        k.mm(p, [(p[:, :], aT[:, kc, j * 128:(j + 1) * 128], wd[:, kc, half * 512:(half + 1) * 512],
                                  kc == 0, kc == 7) for kc in range(8)])
                        av = acc[sub][:, half * 512:(half + 1) * 512]
                        if e == 0:
                            k.ts("dve", av, p[:, :], combs[:, sub, e:e + 1], None, ALU.mult)
                        else:
                            k.stt(av, p[:, :], combs[:, sub, e:e + 1], av, ALU.mult, ALU.add)
        for sub in range(NSUB):
            for half in range(2):
                p = pm[(sub * 2 + half) % 2]
                k.mm(p, [(p[:, :], combT[:ne, sub * 128:(sub + 1) * 128], bdn[:ne, half * 512:(half + 1) * 512],
                          True, True)])
                av = acc[sub][:, half * 512:(half + 1) * 512]
                k.tt("dve", av, av, p[:, :], ALU.add)
        for ts_ in range(NTS):
            t0 = s * ST + ts_ * 512
            load_x_tile(c, "sp", x32, xin, t0, 512)
            for ch in range(8):
                p = pm[ch % 2]
                k.mm(p, [(p[:, j * 128:(j + 1) * 128], acc[ts_ * 4 + j][:, ch * 128:(ch + 1) * 128], c.ident[:, :],
                          True, True) for j in range(4)], transpose=True)
                k.stt(x32[ch][:, :], p[:, :], c.mod[:, layer, 40 + ch:41 + ch], x32[ch][:, :], ALU.mult, ALU.add)
            store_x_tile(c, "sp", x32, xout, t0, 512)
    k.stage_end()


def make_chunk_masks(c):
    k = c.k
    c.mrev = k.sb("mrev", [128, 128], F32)
    c.mch = k.sb("mch", [128, 2], F32)
    k.op("pool", "affine_select", c.mrev[:, :], [c.ones[:, 0:128]], pattern=[[-1, 128]],
         compare_op=ALU.is_gt, fill=0.0, base=0, channel_multiplier=1)
    k.op("pool", "affine_select", c.mrev[:, 0:64], [c.mrev[:, 0:64]], pattern=[[0, 64]],
         compare_op=ALU.is_gt, fill=0.0, base=64, channel_multiplier=-1)
    k.op("pool", "affine_select", c.mch[:, 0:1], [c.ones[:, 0:1]], pattern=[[0, 1]],
         compare_op=ALU.is_gt, fill=0.0, base=64, channel_multiplier=-1)
    k.op("pool", "affine_select", c.mch[:, 1:2], [c.ones[:, 0:1]], pattern=[[0, 1]],
         compare_op=ALU.is_ge, fill=0.0, base=-64, channel_multiplier=1)


def gla_stage(c, layer, xin, xout, W):
    k = c.k
    L = c.L
    T = 512
    k.stage_begin()
    w_in = k.sb("w_in", [128, 8, 3072], BF16)
    w_out = k.sb("w_out", [128, 8, 1024], BF16)
    w_gk1 = k.sb("w_gk1", [128, 8, 16], BF16)
    w_gk2 = k.sb("w_gk2", [16, 512], F32)
    b_gk = k.sb("b_gk", [1, 512], F32)
    g_on = k.sb("g_on", [128, 2], F32)
    x32 = [k.sb("x32", [128, T], F32) for _ in range(8)]
    hbf = k.sb("hbf", [128, 8, T], BF16)
    sq = [k.sb("sq", [128, T], F32) for _ in range(2)]
    rstd = k.sb("rstd", [128, T], F32)
    qT = [k.sb("qT", [128, T], BF16) for _ in range(4)]
    kdec = [k.sb("kdec", [128, 512], BF16) for _ in range(4)]
    vbf = [k.sb("vbf", [128, 1024], BF16) for _ in range(4)]
    gs = [k.sb("gs", [128, T], F32) for _ in range(8)]
    rT = k.sb("rT", [16, T], F32)
    la = [k.sb("la", [128, 512], F32) for _ in range(2)]
    edec = [k.sb("edec", [128, 512], F32) for _ in range(2)]
    dcy = k.sb("dcy", [128, 4, 8], F32)
    S = [k.sb("S", [128, 256], F32) for _ in range(4)]
    Sb = [[k.sb("Sb", [128, 256], BF16) for _ in range(2)] for _ in range(4)]
    oT = [k.sb("oT", [128, 2, T], F32) for _ in range(4)]
    onb = k.sb("onb", [128, 8, T], BF16)
    tmp = [k.sb("tmp", [128, T], F32) for _ in range(2)]
    pA = [k.ps("pA", [128, 512]) for _ in range(2)]
    pU = [k.ps("pU", [128, 512]) for _ in range(2)]
    pO = [k.ps("pO", [128, 512]) for _ in range(2)]
    ptot = k.ps("ptot", [128, 512])
    pst = k.ps("pst", [128, 512])

    k.dma("pool", w_in[:, :, :], W["w_in"].v(W["w_in"].t.rearrange("(kc p) n -> p kc n", p=128)))
    k.dma("pool", w_out[:, :, :], W["w_out"].v(W["w_out"].t.rearrange("(kc p) n -> p kc n", p=128)))
    k.dma("pool", w_gk1[:, :, :], W["w_gk1"].v(W["w_gk1"].t.rearrange("(kc p) n -> p kc n", p=128)))
    k.dma("sp", w_gk2[:, :], W["w_gk2"].v(W["w_gk2"].t))
    k.dma("sp", b_gk[:, :], W["b_gk"].v(W["b_gk"].t))
    k.dma("sp", g_on[:, :], W["g_onorm_l"].v(W["g_onorm_l"].t))
    for h in range(4):
        k.memset("dve", S[h][:, :], 0.0)

    pai = [0]

    def nextA():
        pai[0] += 1
        return pA[pai[0] % 2]

    for it in range(L // T):
        t0 = it * T
        load_x_tile(c, "sp", x32, xin, t0, T)
        rmsnorm_mod(c, x32, T, layer, 0, sq, rstd, pst, out_bf=hbf, off=0, cp_eng="dve")
        load_x_tile(c, "sp", x32, xin, t0, T)
        for h in range(4):
            p = nextA()
            k.mm(p, [(p[:, :], w_in[:, kc, h * 128:(h + 1) * 128], hbf[:, kc, :], kc == 0, kc == 7)
                     for kc in range(8)])
            k.act(qT[h][:, :], p[:, :], AF.Copy, scale=128.0 ** -0.5)
        for n in range(8):
            p = nextA()
            k.mm(p, [(p[:, :], w_in[:, kc, 2048 + n * 128:2048 + (n + 1) * 128], hbf[:, kc, :], kc == 0, kc == 7)
                     for kc in range(8)])
            k.act(gs[n][:, :], p[:, :], AF.Silu)
        p = nextA()
        k.mm(p, [(p[:16, :], w_gk1[:, kc, :], hbf[:, kc, :], kc == 0, kc == 7) for kc in range(8)])
        k.copy("dve", rT[:, :], p[:16, :])
        for j in range(4):
            for half in range(2):
                p = nextA()
                k.mm(p, [(p[:, :], hbf[:, kc, j * 128:(j + 1) * 128],
                          w_in[:, kc, 1024 + half * 512:1024 + (half + 1) * 512], kc == 0, kc == 7)
                         for kc in range(8)])
                k.copy("act", vbf[j][:, half * 512:(half + 1) * 512], p[:, :])
            p = nextA()
            k.mm(p, [(p[:, :], rT[:16, j * 128:(j + 1) * 128], w_gk2[:16, :], True, False),
                     (p[:, :], c.ones[0:1, 0:128], b_gk[0:1, :], False, True)])
            l_ = la[j % 2]
            k.act(l_[:, :], p[:, :], AF.Exp, scale=-1.0)
            k.act(l_[:, :], l_[:, :], AF.Ln, bias=1.0)
            p = nextA()
            k.mm(p, [(p[:, :], c.mrev[:, :], l_[:, :], True, True)])
            ed = edec[j % 2]
            k.act(ed[:, :], p[:, :], AF.Exp, scale=-1.0 / 16.0)
            for h in range(4):
                k.mm(ptot, [(ptot[:, h * 8 + 2 * j:h * 8 + 2 * j + 2], l_[:, h * 128:(h + 1) * 128], c.mch[:, :],
                             True, True)])
            p = nextA()
            k.mm(p, [(p[:, :], hbf[:, kc, j * 128:(j + 1) * 128], w_in[:, kc, 512:1024], kc == 0, kc == 7)
                     for kc in range(8)])
            k.tt("dve", kdec[j][:, :], p[:, :], ed[:, :], ALU.mult)
        k.act(dcy[:, :, :], ptot[:, 0:32], AF.Exp, scale=-1.0 / 16.0)
        n_u = 0
        for cc in range(8):
            j, hf = cc // 2, cc % 2
            for h in range(4):
                pu_ = pU[n_u % 2]
                po_ = pO[n_u % 2]
                n_u += 1
                k.mm(pu_, [(pu_[:, :256], kdec[j][64 * hf:64 * hf + 64, h * 128:(h + 1) * 128],
                            vbf[j][64 * hf:64 * hf + 64, h * 256:(h + 1) * 256], True, True)])
                k.stt(S[h][:, :], S[h][:, :], dcy[:, h, cc:cc + 1], pu_[:, :256], ALU.mult, ALU.add)
                sb_ = Sb[h][cc % 2]
                k.copy("act", sb_[:, :], S[h][:, :])
                k.mm(po_, [(po_[:, dv * 64:(dv + 1) * 64], sb_[:, dv * 128:(dv + 1) * 128],
                            qT[h][:, cc * 64:(cc + 1) * 64], True, True) for dv in range(2)])
                k.copy("act", oT[h][:, :, cc * 64:(cc + 1) * 64],
                       po_.v(po_.t[:, 0:128].rearrange("p (d t) -> p d t", d=2)))
        for h in range(4):
            for dv in range(2):
                s_ = sq[dv]
                k.act(s_[:, :], oT[h][:, dv, :], AF.Square)
                k.mm(pst, [(pst[:, :], c.ones[:, 0:128], s_[:, :], dv == 0, dv == 1)])
            k.ts("dve", rstd[:, :], pst[:, :], 1.0 / 256.0, EPS, ALU.mult, ALU.add)
            k.act(rstd[:, :], rstd[:, :], AF.Sqrt)
            k.op("dve", "reciprocal", rstd[:, :], [rstd[:, :]])
            for dv in range(2):
                t_ = tmp[dv]
                k.stt(t_[:, :], oT[h][:, dv, :], g_on[:, dv:dv + 1], rstd[:, :], ALU.mult, ALU.mult)
                k.tt("dve", onb[:, h * 2 + dv, :], t_[:, :], gs[h * 2 + dv][:, :], ALU.mult)
        for n in range(8):
            p = nextA()
            k.mm(p, [(p[:, :], w_out[:, kc, n * 128:(n + 1) * 128], onb[:, kc, :], kc == 0, kc == 7)
                     for kc in range(8)])
            k.stt(x32[n][:, :], p[:, :], c.mod[:, layer, 16 + n:17 + n], x32[n][:, :], ALU.mult, ALU.add)
        store_x_tile(c, "sp", x32, xout, t0, T)
    k.stage_end()


import math
PI = math.pi


def sin_turns(k, out, x, phase, y, yi, m):
    k.ts("dve", y, x, 1.0 / (2.0 * PI), phase, ALU.mult, ALU.add)
    k.copy("dve", yi, y)
    k.copy("dve", m, yi)
    k.tt("dve", y, y, m, ALU.subtract)
    k.ts("dve", m, y, 0.5, None, ALU.is_gt)
    k.tt("dve", y, y, m, ALU.subtract)
    k.ts("dve", m, y, -0.5, None, ALU.is_lt)
    k.tt("dve", y, y, m, ALU.add)
    k.act(out, y, AF.Sin, scale=2.0 * PI)


def s5_stage(c, layer, xin, xout, W):
    k = c.k
    L = c.L
    T = 512
    TS = 128
    NB = 32
    PE_ = "dve"
    k.stage_begin()
    w_in = k.sb("w_in", [128, 8, 1024], BF16)
    w_out = k.sb("w_out", [128, 8, 2048], BF16)
    Bbr = k.sb("Bbr", [128, NB, 128], BF16)
    Bbi = k.sb("Bbi", [128, NB, 128], BF16)
    Cbr = k.sb("Cbr", [128, NB, 128], BF16)
    Cbi = k.sb("Cbi", [128, NB, 128], BF16)
    cost = k.sb("cost", [128, NB, TS], F32)
    sint = k.sb("sint", [128, NB, TS], F32)
    rcol = k.sb("rcol", [128, NB], F32)
    dl = k.sb("dl", [128, 8], F32)
    spr = k.sb("spr", [128, NB], F32)
    spi = k.sb("spi", [128, NB], F32)
    k.dma("pool", w_in[:, :, :], W["w_in"].v(W["w_in"].t.rearrange("(kc p) n -> p kc n", p=128)))
    k.dma("pool", w_out[:, :, :], W["w_out"].v(W["w_out"].t.rearrange("(kc p) n -> p kc n", p=128)))
    k.dma("sp", dl[:, :], W["d_l"].v(W["d_l"].t))
    k.memset("dve", spr[:, :], 0.0)
    k.memset("dve", spi[:, :], 0.0)

    PH = 9
    k.stage_begin()
    Q = 1024
    nm = ["are", "aim", "ldt", "bre", "bim", "t0", "t1", "t2", "t3", "t4", "t5", "t6"]
    A = {n: k.sb(n, [128, Q], F32) for n in nm}
    A["y"] = k.sb("y", [128, Q], F32)
    A["yi"] = k.sb("yi", [128, Q], mybir.dt.int32)
    for q in range(4 if PH >= 1 else 0):
        sl = slice(q * Q, (q + 1) * Q)
        for n, src in (("are", "Ablk_re"), ("aim", "Ablk_im"), ("ldt", "Lblk"), ("bre", "Bblk_re"), ("bim", "Bblk_im")):
            k.dma("sp", A[n][:, :], W[src].v(W[src].t[:, sl]))
        dt_, ar, ai, er, sn, cs, t6 = A["t0"], A["t1"], A["t2"], A["t3"], A["t4"], A["t5"], A["t6"]
        k.act(dt_[:, :], A["ldt"][:, :], AF.Exp)
        k.tt("dve", ar[:, :], A["are"][:, :], dt_[:, :], ALU.mult)
        k.tt("dve", ai[:, :], A["aim"][:, :], dt_[:, :], ALU.mult)
        k.act(er[:, :], ar[:, :], AF.Exp)
        sin_turns(k, sn[:, :], ai[:, :], 0.0, A["y"][:, :], A["yi"][:, :], t6[:, :])
        sin_turns(k, cs[:, :], ai[:, :], 0.25, A["y"][:, :], A["yi"][:, :], t6[:, :])
        nr, ni = ar, ai
        k.tt("dve", nr[:, :], er[:, :], cs[:, :], ALU.mult)
        k.ts("dve", nr[:, :], nr[:, :], -1.0, None, ALU.add)
        k.tt("dve", ni[:, :], er[:, :], sn[:, :], ALU.mult)
        den = er
        k.tt("dve", den[:, :], A["are"][:, :], A["are"][:, :], ALU.mult)
        k.tt("dve", t6[:, :], A["aim"][:, :], A["aim"][:, :], ALU.mult)
        k.tt("dve", den[:, :], den[:, :], t6[:, :], ALU.add)
        k.op("dve", "reciprocal", den[:, :], [den[:, :]])
        cr, ci = sn, cs
        k.tt("dve", cr[:, :], nr[:, :], A["are"][:, :], ALU.mult)
        k.tt("dve", t6[:, :], ni[:, :], A["aim"][:, :], ALU.mult)
        k.tt("dve", cr[:, :], cr[:, :], t6[:, :], ALU.add)
        k.tt("dve", cr[:, :], cr[:, :], den[:, :], ALU.mult)
        k.tt("dve", ci[:, :], ni[:, :], A["are"][:, :], ALU.mult)
        k.tt("dve", t6[:, :], nr[:, :], A["aim"][:, :], ALU.mult)
        k.tt("dve", ci[:, :], ci[:, :], t6[:, :], ALU.subtract)
        k.tt("dve", ci[:, :], ci[:, :], den[:, :], ALU.mult)
        x1, x2 = ar, ai
        k.tt("dve", x1[:, :], cr[:, :], A["bre"][:, :], ALU.mult)
        k.tt("dve", x2[:, :], ci[:, :], A["bim"][:, :], ALU.mult)
        k.tt("dve", Bbr.v(Bbr.t[:, q * 8:(q + 1) * 8, :].rearrange("p b m -> p (b m)")), x1[:, :], x2[:, :], ALU.subtract)
        k.tt("dve", x1[:, :], cr[:, :], A["bim"][:, :], ALU.mult)
        k.tt("dve", x2[:, :], ci[:, :], A["bre"][:, :], ALU.mult)
        k.tt("dve", Bbi.v(Bbi.t[:, q * 8:(q + 1) * 8, :].rearrange("p b m -> p (b m)")), x1[:, :], x2[:, :], ALU.add)
        k.dma("sp", A["bre"][:, :], W["Cblk_re"].v(W["Cblk_re"].t[:, sl]))
        k.dma("sp", A["bim"][:, :], W["Cblk_im"].v(W["Cblk_im"].t[:, sl]))
        k.copy("dve", Cbr.v(Cbr.t[:, q * 8:(q + 1) * 8, :].rearrange("p b m -> p (b m)")), A["bre"][:, :])
        k.ts("dve", Cbi.v(Cbi.t[:, q * 8:(q + 1) * 8, :].rearrange("p b m -> p (b m)")), A["bim"][:, :], -1.0, None, ALU.mult)
    k.stage_end()
    k.stage_begin()
    acr = k.sb("acr", [128, NB], F32)
    aci = k.sb("aci", [128, NB], F32)
    lc = k.sb("lc", [128, NB], F32)
    th = k.sb("th", [128, NB], F32)
    tI_i = k.sb("tIi", [128, TS], mybir.dt.int32)
    tI = k.sb("tI", [128, TS], F32)
    ang = k.sb("ang", [128, NB, TS], F32)
    yy = k.sb("yy", [128, NB * TS], F32)
    yyi = k.sb("yyi", [128, NB * TS], mybir.dt.int32)
    mm_ = k.sb("mm_", [128, NB * TS], F32)
    if PH < 2:
        k.stage_end()
        k.stage_end()
        return
    k.dma("sp", acr[:, :], W["Acol_re"].v(W["Acol_re"].t))
    k.dma("sp", aci[:, :], W["Acol_im"].v(W["Acol_im"].t))
    k.dma("sp", lc[:, :], W["Lcol"].v(W["Lcol"].t))
    k.act(lc[:, :], lc[:, :], AF.Exp)
    k.tt("dve", th[:, :], aci[:, :], lc[:, :], ALU.mult)
    k.tt("dve", acr[:, :], acr[:, :], lc[:, :], ALU.mult)
    k.act(rcol[:, :], acr[:, :], AF.Exp)
    k.op("pool", "iota", tI_i[:, :], [], pattern=[[1, TS]], base=1, channel_multiplier=0)
    k.copy("dve", tI[:, :], tI_i[:, :])
    for bi in range(NB):
        k.ts("dve", ang[:, bi, :], tI[:, :], th[:, bi:bi + 1], None, ALU.mult)
    fl = lambda b: b.v(b.t[:, :, :].rearrange("p b t -> p (b t)"))
    sin_turns(k, fl(sint), fl(ang), 0.0, yy[:, :], yyi[:, :], mm_[:, :])
    sin_turns(k, fl(cost), fl(ang), 0.25, yy[:, :], yyi[:, :], mm_[:, :])
    k.stage_end()
    if PH < 3:
        k.stage_end()
        return

    x32 = [k.sb("x32", [128, T], F32) for _ in range(8)]
    hbf = k.sb("hbf", [128, 8, T], BF16)
    ubf = k.sb("ubf", [128, 8, T], BF16)
    ybf = k.sb("ybf", [128, 8, T], BF16)
    sq = [k.sb("sq", [128, T], F32) for _ in range(2)]
    rstd = k.sb("rstd", [128, T], F32)
    bre = k.sb("bre", [128, T], F32)
    bim = k.sb("bim", [128, T], F32)
    ta = k.sb("ta", [128, T], F32)
    tb = k.sb("tb", [128, T], F32)
    wre = k.sb("wre", [128, T], F32)
    wim = k.sb("wim", [128, T], F32)
    zre = k.sb("zre", [128, T], F32)
    zim = k.sb("zim", [128, T], F32)
    rfull = k.sb("rfull", [128, TS], F32)
    sm1 = k.sb("sm1", [128, 1], F32)
    sm2 = k.sb("sm2", [128, 1], F32)
    inr = k.sb("inr", [128, 1], F32)
    ini = k.sb("ini", [128, 1], F32)
    sbr = [[k.sb("sbr", [128, T], BF16) for _ in range(4)] for _ in range(2)]
    sbi = [[k.sb("sbi", [128, T], BF16) for _ in range(4)] for _ in range(2)]
    pA = [k.ps("pA", [128, 512]) for _ in range(2)]
    pR = [k.ps("pR", [128, 512]) for _ in range(2)]
    pI = [k.ps("pI", [128, 512]) for _ in range(2)]
    pY = k.ps("pY", [128, 512])
    pst = k.ps("pst", [128, 512])
    pai = [0]

    def nextA():
        pai[0] += 1
        return pA[pai[0] % 2]

    TB = False

    def v3(b):
        return b.v(b.t[:, :].rearrange("p (s t) -> p s t", s=T // TS))

    def tb3(tab, bi):
        return tab.v(tab.t[:, bi, :].unsqueeze(1).to_broadcast([128, T // TS, TS]))

    def ttab(e, out, in0, tab, bi, op):
        if TB:
            k.tt(e, v3(out), v3(in0), tb3(tab, bi), op)
        else:
            for s_ in range(T // TS):
                sl = slice(s_ * TS, (s_ + 1) * TS)
                k.tt(e, out[:, sl], in0[:, sl], tab[:, bi, :], op)

    nblk = 0
    for it in range(L // T):
        t0 = it * T
        load_x_tile(c, "sp", x32, xin, t0, T)
        rmsnorm_mod(c, x32, T, layer, 0, sq, rstd, pst, out_bf=hbf, off=0, cp_eng="dve")
        for ch in range(8):
            p = nextA()
            k.mm(p, [(p[:, :], w_in[:, kc, ch * 128:(ch + 1) * 128], hbf[:, kc, :], kc == 0, kc == 7)
                     for kc in range(8)])
            k.copy("act", x32[ch][:, :], p[:, :])
            k.copy("dve", ubf[:, ch, :], x32[ch][:, :])
        for j in range(8):
            par = j % 2
            for m in range(4):
                bi = j * 4 + m
                pr, pi_ = pR[nblk % 2], pI[nblk % 2]
                nblk += 1
                k.mm(pr, [(pr[:, :], Bbr[:, bi, :], ubf[:, j, :], True, True)])
                k.mm(pi_, [(pi_[:, :], Bbi[:, bi, :], ubf[:, j, :], True, True)])
                k.copy("act", bre[:, :], pr[:, :])
                k.copy("act", bim[:, :], pi_[:, :])
                ttab("dve", ta, bre, cost, bi, ALU.mult)
                ttab(PE_, tb, bim, sint, bi, ALU.mult)
                k.tt("dve", wre[:, :], ta[:, :], tb[:, :], ALU.add)
                ttab(PE_, ta, bim, cost, bi, ALU.mult)
                ttab("dve", tb, bre, sint, bi, ALU.mult)
                k.tt(PE_, wim[:, :], ta[:, :], tb[:, :], ALU.subtract)
                if False:
                    rb = rcol.v(rcol.t[:, bi:bi + 1].to_broadcast([128, TS]))
                else:
                    k.ts("dve", rfull[:, :], c.ones[:, :TS], rcol[:, bi:bi + 1], None, ALU.mult)
                    rb = rfull[:, :]
                for s_ in range(T // TS):
                    sl = slice(s_ * TS, (s_ + 1) * TS)
                    i_r = spr[:, bi:bi + 1] if s_ == 0 else inr[:, :]
                    i_i = spi[:, bi:bi + 1] if s_ == 0 else ini[:, :]
                    k.op("dve", "tensor_tensor_scan", zre[:, sl], [rb, wre[:, sl], i_r, ALU.mult, ALU.add])
                    k.op("dve", "tensor_tensor_scan", zim[:, sl], [rb, wim[:, sl], i_i, ALU.mult, ALU.add])
                    last = s_ == T // TS - 1
                    o_r = spr[:, bi:bi + 1] if last else inr[:, :]
                    o_i = spi[:, bi:bi + 1] if last else ini[:, :]
                    e_ = (s_ + 1) * TS - 1
                    cT, sT = cost[:, bi, TS - 1:TS], sint[:, bi, TS - 1:TS]
                    k.ts("dve", sm1[:, :], zim[:, e_:e_ + 1], sT, None, ALU.mult)
                    k.ts("dve", sm2[:, :], zim[:, e_:e_ + 1], cT, None, ALU.mult)
                    k.stt(o_r, zre[:, e_:e_ + 1], cT, sm1[:, :], ALU.mult, ALU.subtract)
                    k.stt(o_i, zre[:, e_:e_ + 1], sT, sm2[:, :], ALU.mult, ALU.add)
                ttab("dve", ta, zre, cost, bi, ALU.mult)
                ttab(PE_, tb, zim, sint, bi, ALU.mult)
                k.tt("dve", sbr[par][m][:, :], ta[:, :], tb[:, :], ALU.subtract)
                ttab(PE_, ta, zre, sint, bi, ALU.mult)
                ttab("dve", tb, zim, cost, bi, ALU.mult)
                k.tt(PE_, sbi[par][m][:, :], ta[:, :], tb[:, :], ALU.add)
            mms = []
            for m in range(4):
                bi = j * 4 + m
                mms.append((pY[:, :], Cbr[:, bi, :], sbr[par][m][:, :], m == 0, False))
                mms.append((pY[:, :], Cbi[:, bi, :], sbi[par][m][:, :], False, m == 3))
            k.mm(pY, mms)
            k.stt(ta[:, :], x32[j][:, :], dl[:, j:j + 1], pY[:, :], ALU.mult, ALU.add)
            k.act(ybf[:, j, :], ta[:, :], AF.Gelu)
        load_x_tile(c, "sp", x32, xin, t0, T)
        for n in range(8):
            pa, pb = pR[n % 2], pI[n % 2]
            k.mm(pa, [(pa[:, :], w_out[:, kc, n * 128:(n + 1) * 128], ybf[:, kc, :], kc == 0, kc == 7)
                      for kc in range(8)])
            k.mm(pb, [(pb[:, :], w_out[:, kc, 1024 + n * 128:1024 + (n + 1) * 128], ybf[:, kc, :], kc == 0, kc == 7)
                      for kc in range(8)])
            k.act(tb[:, :], pb[:, :], AF.Sigmoid)
            k.tt("dve", ta[:, :], pa[:, :], tb[:, :], ALU.mult)
            k.stt(x32[n][:, :], ta[:, :], c.mod[:, layer, 16 + n:17 + n], x32[n][:, :], ALU.mult, ALU.add)
        store_x_tile(c, "sp", x32, xout, t0, T)
    k.stage_end()


def s5_host_layout(a_re, a_im, log_dt, b_re, b_im, c_re, c_im, d):
    NB = 32
    out = {}
    Ablk_re = np.zeros((128, NB, 128), np.float32); Ablk_im = np.zeros_like(Ablk_re); Lblk = np.zeros_like(Ablk_re)
    Bblk_re = np.zeros_like(Ablk_re); Bblk_im = np.zeros_like(Ablk_re)
    Cblk_re = np.zeros_like(Ablk_re); Cblk_im = np.zeros_like(Ablk_re)
    Acol_re = np.zeros((128, NB), np.float32); Acol_im = np.zeros_like(Acol_re); Lcol = np.zeros_like(Acol_re)
    for j in range(8):
        for m in range(4):
            bi = j * 4 + m
            for gg in range(2):
                g = 8 * j + 2 * m + gg
                gl = 2 * m + gg
                cs = slice(gg * 64, (gg + 1) * 64)
                Ablk_re[:, bi, cs] = a_re[g][None, :]
                Ablk_im[:, bi, cs] = a_im[g][None, :]
                Lblk[:, bi, cs] = log_dt[g]
                Bblk_re[gl * 16:(gl + 1) * 16, bi, cs] = b_re[g].T
                Bblk_im[gl * 16:(gl + 1) * 16, bi, cs] = b_im[g].T
                Cblk_re[cs, bi, gl * 16:(gl + 1) * 16] = c_re[g].T
                Cblk_im[cs, bi, gl * 16:(gl + 1) * 16] = c_im[g].T
                Acol_re[cs, bi] = a_re[g]
                Acol_im[cs, bi] = a_im[g]
                Lcol[cs, bi] = log_dt[g]
    f = lambda a: np.ascontiguousarray(a.reshape(128, NB * 128))
    return dict(Ablk_re=f(Ablk_re), Ablk_im=f(Ablk_im), Lblk=f(Lblk), Bblk_re=f(Bblk_re), Bblk_im=f(Bblk_im),
                Cblk_re=f(Cblk_re), Cblk_im=f(Cblk_im), Acol_re=Acol_re, Acol_im=Acol_im, Lcol=Lcol,
                d_l=np.ascontiguousarray(d.reshape(8, 128).T))


def mlstm_stage(c, layer, xin, xout, W):
    k = c.k
    L = c.L
    T = 256
    NC_ = T // 64
    NS = T // 128
    DH = 512
    k.stage_begin()
    wbuf = [k.sb("wbuf", [128, 8, 1024], BF16) for _ in range(3)]
    bdq = k.sb("bdq", [128, 16, 128], BF16)
    bdk = k.sb("bdk", [128, 16, 128], BF16)
    bdv = k.sb("bdv", [128, 16, 128], BF16)
    weffc = k.sb("weffc", [128, 16, 8], BF16)
    weffm = k.sb("weffm", [128, 16, 8], BF16)
    convl = k.sb("convl", [128, 16, 5], F32)
    bif = k.sb("bif", [1, 8], F32)
    skipl = k.sb("skipl", [128, 16], F32)
    gnl = k.sb("gnl", [128, 16], F32)
    cmat = [[k.sb("cmat", [128, 512], F32) for _ in range(4)] for _ in range(4)]
    cb = [[k.sb("cb", [128, 512], BF16) for _ in range(4)] for _ in range(4)]
    nvec = [k.sb("nvec", [128, 4], F32) for _ in range(4)]
    nvb = [k.sb("nvb", [128, 4], BF16) for _ in range(4)]
    halo = k.sb("halo", [128, 16, 3], F32)
    mcar = k.sb("mcar", [4, 1], F32)
    cmask = k.sb("cmask", [4, T], F32)
    sel = k.sb("sel", [4, 4, 128], F32)
    onesb = k.sb("onesb", [128, 1], BF16)
    for n_, src in (("bdq", bdq), ("bdk", bdk), ("bdv", bdv)):
        k.dma("pool", src[:, :, :], W[n_].v(W[n_].t))
    k.dma("sp", convl[:, :, :], W["conv_l"].v(W["conv_l"].t))
    k.dma("sp", bif[:, :], W["b_if"].v(W["b_if"].t))
    k.dma("sp", skipl[:, :], W["skip_l"].v(W["skip_l"].t))
    k.dma("sp", gnl[:, :], W["gnorm_l"].v(W["gnorm_l"].t))
    for h in range(4):
        for kc in range(4):
            k.memset("dve", cmat[h][kc][:, :], 0.0)
        k.memset("dve", nvec[h][:, :], 0.0)
        k.ts("dve", sel[:, h, :], c.ones[0:4, 0:128], c.ident[0:4, h:h + 1], None, ALU.mult)
    k.memset("dve", halo[:, :, :], 0.0)
    k.memset("dve", mcar[:, :], 0.0)
    k.memset("dve", cmask[:, :], 1.0)
    k.memset("dve", cmask.v(cmask.t[:, :].rearrange("p (c t) -> p c t", t=64)[:, :, 0:1]), 0.0)
    k.memset("dve", onesb[:, :], 1.0)
    k.stage_begin()
    bT = {n_: k.sb(n_, [128, 16, 128], F32) for n_ in ("bdqT", "bdkT", "bdvT")}
    wif = k.sb("wif", [128, 48, 8], F32)
    pw = k.ps("pw", [128, 512])
    for n_ in bT:
        k.dma("sp", bT[n_][:, :, :], W[n_].v(W[n_].t))
    k.dma("sp", wif[:, :, :], W["wif_l"].v(W["wif_l"].t))
    for ch in range(16):
        k.mm(pw, [(pw[:, ch * 8:ch * 8 + 8], bT["bdqT"][:, ch, :], wif[:, ch, :], True, False),
                  (pw[:, ch * 8:ch * 8 + 8], bT["bdkT"][:, ch, :], wif[:, 16 + ch, :], False, True)])
    k.copy("dve", weffc.v(weffc.t[:, :, :].rearrange("p c g -> p (c g)")), pw[:, 0:128])
    for ch in range(16):
        k.mm(pw, [(pw[:, 128 + ch * 8:128 + ch * 8 + 8], bT["bdvT"][:, ch, :], wif[:, 32 + ch, :], True, True)])
    k.copy("dve", weffm.v(weffm.t[:, :, :].rearrange("p c g -> p (c g)")), pw[:, 128:256])
    k.stage_end()

    x32 = [k.sb("x32", [128, T], F32) for _ in range(8)]
    hbf = k.sb("hbf", [128, 8, T], BF16)
    sq = [k.sb("sq", [128, T], F32) for _ in range(2)]
    rstd = k.sb("rstd", [128, T], F32)
    xmb = k.sb("xmb", [128, 16, T], BF16)
    xcb = k.sb("xcb", [128, 16, T], BF16)
    zsb = k.sb("zsb", [128, 16, T], BF16)
    qTb = k.sb("qTb", [128, 16, T], BF16)
    kw = [k.sb("kw", [128, 2048], BF16) for _ in range(NS)]
    vt = [k.sb("vt", [128, 2048], BF16) for _ in range(NS)]
    xt = [k.sb("xt", [128, T + 3], F32) for _ in range(2)]
    ca = [k.sb("ca", [128, T], F32) for _ in range(2)]
    gi = k.sb("gi", [4, T], F32)
    nl = k.sb("nl", [4, T], F32)
    cum = k.sb("cum", [4, T], F32)
    lw = k.sb("lw", [4, T], F32)
    wT = k.sb("wT", [4, T], F32)
    enx = k.sb("enx", [4, T], F32)
    g4 = {n_: k.sb(n_, [4, NC_], F32) for n_ in ("ntot", "mx", "mnew", "mp", "dec", "negm", "en")}
    wtm = [k.sb("wtm", [128, 4], F32) for _ in range(NS)]
    entm = [k.sb("entm", [64, 4], F32) for _ in range(NC_)]
    decb = k.sb("decb", [128, 4, NC_], F32)
    hc = [k.sb("hc", [64, 512], F32) for _ in range(2)]
    hnb = [k.sb("hnb", [64, 512], BF16) for _ in range(2)]
    st6 = k.sb("st6", [64, 6], F32)
    mv = k.sb("mv", [64, 2], F32)
    den = k.sb("den", [64, 1], F32)
    tmp64 = [k.sb("tmp64", [128, 64], F32) for _ in range(2)]
    pA = [k.ps("pA", [128, 512]) for _ in range(2)]
    pU = [k.ps("pU", [128, 512]) for _ in range(2)]
    pN = k.ps("pN", [128, 512])
    pM = k.ps("pM", [128, 512])
    pT = k.ps("pT", [128, 512], BF16)
    pst = k.ps("pst", [128, 512])
    pai = [0]

    def nextA():
        pai[0] += 1
        return pA[pai[0] % 2]

    wq = [0]

    def load_piece(src, rows=None, cols=None):
        b = wbuf[wq[0] % 3]
        wq[0] += 1
        t = W[src].t
        if cols is not None:
            ap = t[:, cols[0]:cols[1]].rearrange("(kc p) n -> p kc n", p=128)
        else:
            ap = t[rows[0]:rows[1], :].rearrange("(kc p) n -> p kc n", p=128)
        k.dma("pool", b[:, :, :], W[src].v(ap))
        return b

    for it in range(L // T):
        t0 = it * T
        load_x_tile(c, "sp", x32, xin, t0, T)
        rmsnorm_mod(c, x32, T, layer, 0, sq, rstd, pst, out_bf=hbf, off=0, cp_eng="dve")
        load_x_tile(c, "sp", x32, xin, t0, T)
        for pc in range(4):
            wb = load_piece("w_up", cols=(pc * 1024, (pc + 1) * 1024))
            for cc in range(8):
                ch = (pc % 2) * 8 + cc
                p = nextA()
                k.mm(p, [(p[:, :T], wb[:, kc, cc * 128:(cc + 1) * 128], hbf[:, kc, :], kc == 0, kc == 7)
                         for kc in range(8)])
                if pc < 2:
                    x_ = xt[ch % 2]
                    k.copy("dve", x_[:, 0:3], halo[:, ch, :])
                    k.copy("act", x_[:, 3:T + 3], p[:, :T])
                    k.copy("dve", halo[:, ch, :], x_[:, T:T + 3])
                    k.copy("dve", xmb[:, ch, :], x_[:, 3:T + 3])
                    a_ = ca[ch % 2]
                    k.ts("dve", a_[:, :], x_[:, 3:T + 3], convl[:, ch, 3:4], convl[:, ch, 4:5], ALU.mult, ALU.add)
                    for j in range(3):
                        k.stt(a_[:, :], x_[:, j:j + T], convl[:, ch, j:j + 1], a_[:, :], ALU.mult, ALU.add)
                    k.act(xcb[:, ch, :], a_[:, :], AF.Silu)
                else:
                    k.act(zsb[:, ch, :], p[:, :T], AF.Silu)
        for gsel, dst in ((0, gi), (1, nl)):
            p = pM
            mms = []
            for ch in range(16):
                mms.append((p[:4, :T], weffc[:, ch, gsel * 4:gsel * 4 + 4], xcb[:, ch, :], ch == 0, False))
                mms.append((p[:4, :T], weffm[:, ch, gsel * 4:gsel * 4 + 4], xmb[:, ch, :], False, False))
            mms.append((p[:4, :T], bif[0:1, gsel * 4:gsel * 4 + 4], c.ones[0:1, :T], False, True))
            k.mm(p, mms)
            if gsel == 0:
                k.copy("dve", gi[:, :], p[:4, :T])
            else:
                k.act(nl[:, :], p[:4, :T], AF.Exp, scale=-1.0)
                k.act(nl[:, :], nl[:, :], AF.Ln, bias=1.0)
        k.op("dve", "tensor_tensor_scan", cum[:, :], [cmask[:, :], nl[:, :], 0.0, ALU.mult, ALU.add])
        cum3 = cum.v(cum.t[:, :].rearrange("p (c t) -> p c t", t=64))
        k.ts("dve", g4["ntot"][:, :], cum.v(cum.t[:, :].rearrange("p (c t) -> p c t", t=64)[:, :, 63]), -1.0, None, ALU.mult)
        for cc in range(NC_):
            sl = slice(cc * 64, (cc + 1) * 64)
            k.stt(lw[:, sl], cum[:, sl], g4["ntot"][:, cc:cc + 1], gi[:, sl], ALU.add, ALU.add)
        k.op("dve", "tensor_reduce", g4["mx"][:, :], [lw.v(lw.t[:, :].rearrange("p (c t) -> p c t", t=64))],
             axis=AX.X, op=ALU.max)
        k.op("dve", "tensor_tensor_scan", g4["mnew"][:, :], [g4["ntot"][:, :], g4["mx"][:, :], mcar[:, 0:1], ALU.add, ALU.max])
        k.copy("dve", g4["mp"][:, 0:1], mcar[:, :])
        k.copy("dve", g4["mp"][:, 1:NC_], g4["mnew"][:, 0:NC_ - 1])
        k.copy("dve", mcar[:, :], g4["mnew"][:, NC_ - 1:NC_])
        k.tt("dve", g4["dec"][:, :], g4["ntot"][:, :], g4["mp"][:, :], ALU.add)
        k.tt("dve", g4["dec"][:, :], g4["dec"][:, :], g4["mnew"][:, :], ALU.subtract)
        k.act(g4["dec"][:, :], g4["dec"][:, :], AF.Exp)
        k.ts("dve", g4["negm"][:, :], g4["mnew"][:, :], -1.0, None, ALU.mult)
        k.act(g4["en"][:, :], g4["negm"][:, :], AF.Exp)
        for cc in range(NC_):
            sl = slice(cc * 64, (cc + 1) * 64)
            k.act(wT[:, sl], lw[:, sl], AF.Exp, bias=g4["negm"][:, cc:cc + 1], scale=1.0)
            k.ts("dve", enx[:, sl], c.ones[0:4, 0:64], g4["en"][:, cc:cc + 1], None, ALU.mult)
        for s_ in range(NS):
            k.mm(pM, [(pM[:, 32 + 4 * s_:36 + 4 * s_], wT[:, s_ * 128:(s_ + 1) * 128], c.ident[0:4, 0:4], True, True)],
                 transpose=True)
            k.ts("dve", wtm[s_][:, :], pM[:, 32 + 4 * s_:36 + 4 * s_], DH ** -0.5, None, ALU.mult)
        for cc in range(NC_):
            k.mm(pM, [(pM[:64, 64 + 4 * cc:68 + 4 * cc], enx[:, cc * 64:(cc + 1) * 64], c.ident[0:4, 0:4], True, True)],
                 transpose=True)
            k.copy("dve", entm[cc][:, :], pM[:64, 64 + 4 * cc:68 + 4 * cc])
        for h in range(4):
            k.mm(pM, [(pM[:, 128 + h * NC_:128 + (h + 1) * NC_], sel[:, h, :], g4["dec"][:, :], True, True)])
        k.copy("dve", decb.v(decb.t[:, :, :].rearrange("p h c -> p (h c)")), pM[:, 128:128 + 4 * NC_])
        for ch in range(16):
            p = nextA()
            k.mm(p, [(p[:, :T], bdq[:, ch, :], xcb[:, ch, :], True, True)])
            k.copy("act", qTb[:, ch, :], p[:, :T])
        for s_ in range(NS):
            for h in range(4):
                p = nextA()
                k.mm(p, [(p[:, kc * 128:(kc + 1) * 128], xcb[:, h * 4 + kc, s_ * 128:(s_ + 1) * 128], bdk[:, h * 4 + kc, :],
                          True, True) for kc in range(4)])
                k.ts("dve", kw[s_][:, h * 512:(h + 1) * 512], p[:, :], wtm[s_][:, h:h + 1], None, ALU.mult)
                p = nextA()
                k.mm(p, [(p[:, kc * 128:(kc + 1) * 128], xmb[:, h * 4 + kc, s_ * 128:(s_ + 1) * 128], bdv[:, h * 4 + kc, :],
                          True, True) for kc in range(4)])
                k.copy("act", vt[s_][:, h * 512:(h + 1) * 512], p[:, :])
        for ch in range(16):
            k.ts("dve", xcb[:, ch, :], xcb[:, ch, :], skipl[:, ch:ch + 1], None, ALU.mult)
        nu = 0
        for cc in range(NC_):
            s_, hf = cc // 2, cc % 2
            r0 = 64 * hf
            cs_ = slice(cc * 64, (cc + 1) * 64)
            for h in range(4):
                for kc in range(4):
                    pu_ = pU[nu % 2]
                    nu += 1
                    k.mm(pu_, [(pu_[:, :], kw[s_][r0:r0 + 64, h * 512 + kc * 128:h * 512 + (kc + 1) * 128],
                                vt[s_][r0:r0 + 64, h * 512:(h + 1) * 512], True, True)])
                    k.stt(cmat[h][kc][:, :], cmat[h][kc][:, :], decb[:, h, cc:cc + 1], pu_[:, :], ALU.mult, ALU.add)
                    k.copy("act", cb[h][kc][:, :], cmat[h][kc][:, :])
                k.mm(pM, [(pM[:, kc:kc + 1], kw[s_][r0:r0 + 64, h * 512 + kc * 128:h * 512 + (kc + 1) * 128],
                           onesb[r0:r0 + 64, 0:1], True, True) for kc in range(4)])
                k.stt(nvec[h][:, :], nvec[h][:, :], decb[:, h, cc:cc + 1], pM[:, 0:4], ALU.mult, ALU.add)
                k.copy("dve", nvb[h][:, :], nvec[h][:, :])
                k.mm(pN, [(pN[:64, :], qTb[:, h * 4 + kc, cs_], cb[h][kc][:, :], kc == 0, kc == 3) for kc in range(4)])
                k.mm(pM, [(pM[:64, 8:9], qTb[:, h * 4 + kc, cs_], nvb[h][:, kc:kc + 1], kc == 0, kc == 3)
                          for kc in range(4)])
                k.copy("dve", den[:, :], pM[:64, 8:9])
                k.stt(den[:, :], den[:, :], -1.0, den[:, :], ALU.mult, ALU.max)
                k.ts("dve", den[:, :], den[:, :], entm[cc][:, h:h + 1], None, ALU.max)
                k.op("dve", "reciprocal", den[:, :], [den[:, :]])
                hc_ = hc[h % 2]
                k.act(hc_[:, :], pN[:64, :], AF.Copy, scale=den[:, 0:1])
                k.op("dve", "bn_stats", st6[:, :], [hc_[:, :]])
                k.op("dve", "bn_aggr", mv[:, :], [st6[:, :]])
                k.ts("dve", mv[:, 1:2], mv[:, 1:2], EPS, None, ALU.add)
                k.act(mv[:, 1:2], mv[:, 1:2], AF.Sqrt)
                k.op("dve", "reciprocal", mv[:, 1:2], [mv[:, 1:2]])
                hb_ = hnb[h % 2]
                k.ts("dve", hb_[:, :], hc_[:, :], mv[:, 0:1], mv[:, 1:2], ALU.subtract, ALU.mult)
                k.mm(pT, [(pT[:, kc * 64:(kc + 1) * 64], hb_[:, kc * 128:(kc + 1) * 128], c.identb[0:64, 0:64], True, True)
                          for kc in range(4)], transpose=True)
                for kc in range(4):
                    ch = h * 4 + kc
                    t_ = tmp64[kc % 2]
                    k.stt(t_[:, :], pT[:, kc * 64:(kc + 1) * 64], gnl[:, ch:ch + 1], xcb[:, ch, cs_], ALU.mult, ALU.add)
                    k.tt("dve", zsb[:, ch, cs_], t_[:, :], zsb[:, ch, cs_], ALU.mult)
        wd0 = load_piece("w_down", rows=(0, 1024))
        wd1 = load_piece("w_down", rows=(1024, 2048))
        for n in range(8):
            p = nextA()
            mms = [(p[:, :T], wd0[:, kc, n * 128:(n + 1) * 128], zsb[:, kc, :], kc == 0, False) for kc in range(8)]
            mms += [(p[:, :T], wd1[:, kc, n * 128:(n + 1) * 128], zsb[:, 8 + kc, :], False, kc == 7) for kc in range(8)]
            k.mm(p, mms)
            k.stt(x32[n][:, :], p[:, :T], c.mod[:, layer, 16 + n:17 + n], x32[n][:, :], ALU.mult, ALU.add)
        store_x_tile(c, "sp", x32, xout, t0, T)
    k.stage_end()


def mlstm_host_layout(conv_w, conv_b, w_q, w_k, w_v, w_if, skip, g_norm):
    def bd(w, transpose):
        o = np.zeros((128, 16, 128), np.float32)
        for ch in range(16):
            for b in range(32):
                blk = w[ch * 32 + b]
                sl = slice(b * 4, b * 4 + 4)
                o[sl, ch, sl] = blk.T if transpose else blk
        return o
    conv_l = np.zeros((128, 16, 5), np.float32)
    conv_l[:, :, 0:4] = conv_w.reshape(4, 16, 128).transpose(2, 1, 0)
    conv_l[:, :, 4] = conv_b.reshape(16, 128).T
    return dict(bdq=bd(w_q, False), bdk=bd(w_k, False), bdv=bd(w_v, False),
                bdqT=bd(w_q, True), bdkT=bd(w_k, True), bdvT=bd(w_v, True),
                conv_l=conv_l,
                wif_l=np.ascontiguousarray(w_if.reshape(48, 128, 8).transpose(1, 0, 2)),
                skip_l=np.ascontiguousarray(skip.reshape(16, 128).T),
                gnorm_l=np.ascontiguousarray(g_norm.reshape(16, 128).T))


I32 = mybir.dt.int32


def moe_sparse_stage(c, layer, xin, xout, W, S, ne=NE, eoff=0):
    k = c.k
    L = c.L
    NBLK = L // 128
    NG = (4 * L + ne * 511) // 512
    NSLOT = NG * 512
    k.stage_begin()
    slots_all = k.sb("slots_all", [128, NBLK, 4], I32)
    egrp = k.sb("egrp", [128, NG], F32)
    eg256 = k.sb("eg256", [128, NG], F32)
    eg128 = k.sb("eg128", [128, NG], F32)
    iop = k.sb("iop", [128, 1], F32)
    iorow = k.sb("iorow", [128, 8], F32)
    ioi = k.sb("ioi", [128, 8], I32)
    k.op("pool", "iota", ioi[:, :], [], pattern=[[128, 8]], base=0, channel_multiplier=1)
    k.copy("dve", iorow[:, :], ioi[:, :])
    k.copy("dve", iop[:, :], ioi[:, 0:1])

    k.stage_begin()
    z = k.sb("z", [128, NSLOT * 2 // 128], I32)
    k.memset("dve", z[:, :], 0)
    k.dma("sp", S["rec"].v(S["rec"].t.rearrange("(p r) c -> p (r c)", p=128)), z[:, :])
    k.stage_end()

    k.stage_begin()
    x32 = [k.sb("x32", [128, 512], F32) for _ in range(8)]
    hbf = k.sb("hbf", [128, 8, 512], BF16)
    sq = [k.sb("sq", [128, 512], F32) for _ in range(2)]
    rstd = k.sb("rstd", [128, 512], F32)
    htm = [k.sb("htm", [128, 1024], BF16) for _ in range(2)]
    wr = k.sb("wr", [128, 8, 32], F32)
    br = k.sb("br", [1, 32], F32)
    lg_all = k.sb("lg_all", [128, NBLK, 32], F32)
    comb_all = k.sb("comb_all", [128, NBLK, 32], F32)
    pos_all = k.sb("pos_all", [128, NBLK, 32], F32)
    m4_all = k.sb("m4_all", [128, NBLK, 4], F32)
    runc = k.sb("runc", [128, 32], F32)
    lmat = k.sb("lmat", [128, 128], F32)
    m8 = k.sb("m8", [128, 8], F32)
    negm = k.sb("negm", [128, 1], F32)
    mask = k.sb("mask", [128, 32], F32)
    ex = k.sb("ex", [128, 32], F32)
    ssum = k.sb("ssum", [128, 1], F32)
    pm = [k.ps("pm", [128, 512]) for _ in range(2)]
    pp = k.ps("pp", [128, 512])
    ptb = [k.ps("ptb", [128, 1024], BF16) for _ in range(2)]
    k.dma("sp", wr[:, :, :], W["w_router"].v(W["w_router"].t.rearrange("(kc p) e -> p kc e", p=128)))
    k.dma("sp", br[:, :], W["b_router"].v(W["b_router"].t))
    k.memset("dve", runc[:, :], 0.0)
    k.op("pool", "affine_select", lmat[:, :], [c.ones[:, 0:128]], pattern=[[1, 128]],
         compare_op=ALU.is_gt, fill=0.0, base=0, channel_multiplier=-1)
    for it in range(L // 512):
        t0 = it * 512
        load_x_tile(c, "sp", x32, xin, t0, 512)
        rmsnorm_mod(c, x32, 512, layer, 1, sq, rstd, pm[0], out_bf=hbf, off=0, cp_eng="dve")
        for j in range(4):
            blk = it * 4 + j
            p = pm[1]
            mms = [(p[:, :32], x32[ch][:, j * 128:(j + 1) * 128], wr[:, ch, :], ch == 0, False) for ch in range(8)]
            mms.append((p[:, :32], c.ones[0:1, 0:128], br[0:1, :], False, True))
            k.mm(p, mms)
            lg = lg_all[:, blk, :]
            k.copy("dve", lg, p[:, :32])
            k.op("dve", "max", m8[:, :], [lg])
            k.copy("dve", m4_all[:, blk, :], m8[:, 0:4])
            k.ts("dve", mask[:, :], lg, m8[:, 3:4], None, ALU.is_ge)
            k.ts("dve", negm[:, :], m8[:, 0:1], -1.0, None, ALU.mult)
            k.act(ex[:, :], lg, AF.Exp, bias=negm[:, 0:1], scale=1.0)
            k.stt(ex[:, :], ex[:, :], 1.0, mask[:, :], ALU.mult, ALU.mult, accum_out=ssum[:, :])
            k.op("dve", "reciprocal", ssum[:, :], [ssum[:, :]])
            k.ts("dve", comb_all[:, blk, :], ex[:, :], ssum[:, 0:1], None, ALU.mult)
            k.mm(pp, [(pp[:, 0:32], lmat[:, :], mask[:, :], True, True),
                      (pp[:, 32:64], c.ones[:, 0:128], mask[:, :], True, True)])
            k.tt("dve", pos_all[:, blk, :], pp[:, 0:32], runc[:, :], ALU.add)
            k.tt("dve", runc[:, :], runc[:, :], pp[:, 32:64], ALU.add)
            pt_ = ptb[blk % 2]
            k.mm(pt_, [(pt_[:, ch * 128:(ch + 1) * 128], hbf[:, ch, j * 128:(j + 1) * 128], c.identb[:, :], True, True)
                       for ch in range(8)], transpose=True)
            h_ = htm[blk % 2]
            k.copy("act", h_[:, :], pt_[:, :])
            reg = k.region()
            k.dma("sp", reg.v(S["h_tm"].t[blk * 128:(blk + 1) * 128, :]), h_[:, :])
    gsz = k.sb("gsz", [128, 32], F32)
    gszi = k.sb("gszi", [128, 32], I32)
    gtmp = k.sb("gtmp", [128, 32], F32)
    ginc = k.sb("ginc", [128, 32], F32)
    base = k.sb("base", [128, 32], F32)
    gidx = k.sb("gidx", [128, NG], F32)
    gidi = k.sb("gidi", [128, NG], I32)
    gcmp = k.sb("gcmp", [128, NG], F32)
    k.ts("dve", gtmp[:, :], runc[:, :], 511.0, 1.0 / 512.0, ALU.add, ALU.mult)
    k.copy("dve", gszi[:, :], gtmp[:, :])
    k.copy("dve", gsz[:, :], gszi[:, :])
    k.tt("dve", mask[:, :], gsz[:, :], gtmp[:, :], ALU.is_gt)
    k.tt("dve", gsz[:, :], gsz[:, :], mask[:, :], ALU.subtract)
    k.op("dve", "tensor_tensor_scan", ginc[:, :], [c.ones[:, 0:32], gsz[:, :], 0.0, ALU.mult, ALU.add])
    k.tt("dve", base[:, :], ginc[:, :], gsz[:, :], ALU.subtract)
    k.ts("dve", base[:, :], base[:, :], 512.0, None, ALU.mult)
    k.op("pool", "iota", gidi[:, :], [], pattern=[[1, NG]], base=0, channel_multiplier=0)
    k.copy("dve", gidx[:, :], gidi[:, :])
    k.memset("dve", egrp[:, :], 0.0)
    for e in range(ne):
        k.ts("dve", gcmp[:, :], gidx[:, :], ginc[:, e:e + 1], None, ALU.is_ge)
        k.tt("dve", egrp[:, :], egrp[:, :], gcmp[:, :], ALU.add)
    k.ts("dve", egrp[:, :], egrp[:, :], float(ne - 1), None, ALU.min)
    k.ts("dve", egrp[:, :], egrp[:, :], float(eoff), None, ALU.add)
    k.ts("dve", eg256[:, :], egrp[:, :], 256.0, None, ALU.mult)
    k.ts("dve", eg128[:, :], egrp[:, :], 128.0, None, ALU.mult)
    slotf = k.sb("slotf", [128, 32], F32)
    oh = k.sb("oh", [128, 32], F32)
    junk = k.sb("junk", [128, 32], F32)
    sj = k.sb("sj", [128, 1], F32)
    wj = k.sb("wj", [128, 1], F32)
    tokf = k.sb("tokf", [128, 1], F32)
    recs = [k.sb("recs", [128, 2], I32) for _ in range(4)]
    sji = [k.sb("sji", [128, 1], I32) for _ in range(4)]
    nsc = 0
    for blk in range(NBLK):
        k.tt("dve", slotf[:, :], pos_all[:, blk, :], base[:, :], ALU.add)
        k.ts("dve", tokf[:, :], iop[:, :], float(blk * 128), None, ALU.add)
        for j in range(4):
            k.ts("dve", oh[:, :], lg_all[:, blk, :], m4_all[:, blk, j:j + 1], None, ALU.is_equal)
            k.stt(junk[:, :], oh[:, :], 1.0, slotf[:, :], ALU.mult, ALU.mult, accum_out=sj[:, :])
            k.stt(junk[:, :], oh[:, :], 1.0, comb_all[:, blk, :], ALU.mult, ALU.mult, accum_out=wj[:, :])
            r_ = recs[nsc % 4]
            si_ = sji[nsc % 4]
            nsc += 1
            k.copy("dve", r_[:, 0:1], tokf[:, :])
            k.copy("dve", r_.v(r_.t[:, 1:2].bitcast(F32)), wj[:, :])
            k.copy("dve", si_[:, :], sj[:, :])
            k.copy("dve", slots_all[:, blk, j:j + 1], sj[:, :])
            reg = k.region()
            k.idma(reg.v(S["rec"].t), r_[:, :], si_[:, 0:1], scatter=True)
    k.stage_end()

    k.stage_begin()
    wgu = [[k.sb("wgu", [128, 4, 2048], BF16) for _ in range(2)] for _ in range(2)]
    wdn = [k.sb("wdn", [128, 8, 1024], BF16) for _ in range(2)]
    widx = [k.sb("widx", [128, 2], I32) for _ in range(2)]
    bidx = [k.sb("bidx", [128, 2], I32) for _ in range(2)]
    wtmp = k.sb("wtmp", [128, 8], F32)
    wtmp2 = k.sb("wtmp2", [128, 2], F32)
    bgu = [k.sb("bgu", [128, 16], F32) for _ in range(2)]
    bdn = [k.sb("bdn", [128, 1024], F32) for _ in range(2)]
    recg = [[k.sb("recg", [128, 2], I32) for _ in range(4)] for _ in range(2)]
    wsc = [k.sb("wsc", [128, 1], F32) for _ in range(4)]
    hg = [[k.sb("hg", [128, 1024], BF16) for _ in range(4)] for _ in range(2)]
    hT = [k.sb("hT", [128, 8, 512], BF16) for _ in range(2)]
    actT = [k.sb("actT", [128, 8, 512], BF16) for _ in range(2)]
    tg = [k.sb("tg", [128, 512], F32) for _ in range(2)]
    tt_ = [k.sb("tt", [128, 512], F32) for _ in range(2)]
    tu = [k.sb("tu", [128, 512], F32) for _ in range(2)]
    otm = [k.sb("otm", [128, 1024], F32) for _ in range(4)]
    pg = [k.ps("pg", [128, 512]) for _ in range(2)]
    pu = [k.ps("pu", [128, 512]) for _ in range(2)]
    pd = [k.ps("pd", [128, 512]) for _ in range(2)]
    pt2 = [k.ps("pt2", [128, 1024], BF16) for _ in range(2)]
    for hh in hg:
        for t_ in hh:
            k.memset("dve", t_[:, :], 0.0)
    h_src = Buf(k, S["h_tm"].t, "dram")
    rec_src = Buf(k, S["rec"].t, "dram")

    def prefetch(g):
        b = g % 2
        k.stt(wtmp[:, 0:2], c.ones[:, 0:2], eg256[:, g:g + 1], iorow[:, 0:2], ALU.mult, ALU.add)
        k.copy("dve", widx[b][:, :], wtmp[:, 0:2])
        k.ts("dve", wtmp2[:, 0:1], iop[:, :], eg128[:, g:g + 1], None, ALU.add)
        k.copy("dve", wtmp2[:, 1:2], egrp[:, g:g + 1])
        k.copy("dve", bidx[b][:, :], wtmp2[:, :])
        k.idma(bgu[b][:, :], W["b_gu_rows"].v(W["b_gu_rows"].t), bidx[b][:, 0:1])
        k.idma(bdn[b][:, :], W["b_down"].v(W["b_down"].t), bidx[b][:, 1:2])
        for hf in range(2):
            k.idma(wgu[b][hf].v(wgu[b][hf].t[:, :, :].rearrange("p k n -> p (k n)")),
                   W["w_gu_rows"].v(W["w_gu_rows"].t), widx[b][:, hf:hf + 1])
        k.idma(wdn[b].v(wdn[b].t[:, :, :].rearrange("p k n -> p (k n)")),
               W["w_down_rows"].v(W["w_down_rows"].t), bidx[b][:, 0:1])

    def prefetch_tok(g):
        b = g % 2
        for sb_ in range(4):
            s0 = g * 512 + sb_ * 128
            k.dma("sp", recg[b][sb_][:, :], rec_src.v(rec_src.t[s0:s0 + 128, :]))
        for sb_ in range(4):
            k.idma(hg[b][sb_][:, :], h_src.v(h_src.t), recg[b][sb_][:, 0:1])

    NGX = NG
    prefetch_tok(0)
    prefetch(0)
    for g in range(NGX):
        b = g % 2
        if g + 1 < NGX:
            prefetch_tok(g + 1)
            prefetch(g + 1)
        hT_ = hT[b]
        k.ts("dve", bgu[b][:, 8:16], bgu[b][:, 8:16], 1.0, None, ALU.add)
        k.ts("dve", bdn[b][0:1, :], bdn[b][0:1, :], ALPHA, None, ALU.mult)
        for sb_ in range(4):
            k.ts("dve", wsc[sb_][:, :], recg[b][sb_].v(recg[b][sb_].t[:, 1:2].bitcast(F32)), 1.0 / ALPHA, None, ALU.mult)
        for ch in range(8):
            p = pt2[ch % 2]
            k.mm(p, [(p[:, sb_ * 128:(sb_ + 1) * 128], hg[b][sb_][:, ch * 128:(ch + 1) * 128], c.identb[:, :], True, True)
                     for sb_ in range(4)], transpose=True)
            k.copy("act", hT_[:, ch, :], p[:, 0:512])
        aT = actT[b]
        for n in range(8):
            a, b_ = pg[n % 2], pu[n % 2]
            k.mm(a, [(a[:, :], wgu[b][kc // 4][:, kc % 4, n * 128:(n + 1) * 128], hT_[:, kc, :], kc == 0, kc == 7) for kc in range(8)])
            k.mm(b_, [(b_[:, :], wgu[b][kc // 4][:, kc % 4, 1024 + n * 128:1024 + (n + 1) * 128], hT_[:, kc, :], kc == 0, kc == 7)
                      for kc in range(8)])
            g_, t_, u_ = tg[n % 2], tt_[n % 2], tu[n % 2]
            k.ts("dve", g_[:, :], a[:, :], bgu[b][:, n:n + 1], 7.0, ALU.add, ALU.min)
            k.act(t_[:, :], g_[:, :], AF.Silu, scale=ALPHA)
            k.ts("dve", u_[:, :], b_[:, :], bgu[b][:, 8 + n:9 + n], -6.0, ALU.add, ALU.max)
            k.stt(aT[:, n, :], u_[:, :], 8.0, t_[:, :], ALU.min, ALU.mult)
        for sb_ in range(4):
            for half in range(2):
                p = pd[(sb_ * 2 + half) % 2]
                mms = [(p[:, :], aT[:, kc, sb_ * 128:(sb_ + 1) * 128], wdn[b][:, kc, half * 512:(half + 1) * 512],
                        kc == 0, False) for kc in range(8)]
                mms.append((p[:, :], c.ones[0:1, 0:128], bdn[b][0:1, half * 512:(half + 1) * 512], False, True))
                k.mm(p, mms)
                k.act(otm[sb_][:, half * 512:(half + 1) * 512], p[:, :], AF.Copy, scale=wsc[sb_][:, 0:1])
            s0 = g * 512 + sb_ * 128
            reg = k.region()
            k.dma("sp", reg.v(S["oslots"].t[s0:s0 + 128, :]), otm[sb_][:, :])
    k.stage_end()

    k.stage_begin()
    x32 = [k.sb("x32", [128, 512], F32) for _ in range(8)]
    yg = [[k.sb("yg", [128, 1024], F32) for _ in range(4)] for _ in range(2)]
    pm = [k.ps("pm", [128, 512]) for _ in range(2)]
    o_src = Buf(k, S["oslots"].t, "dram")
    for it in range(L // 512):
        t0 = it * 512
        load_x_tile(c, "sp", x32, xin, t0, 512)
        for j in range(4):
            blk = it * 4 + j
            ys = yg[j % 2]
            for r in range(4):
                k.idma(ys[r][:, :], o_src.v(o_src.t), slots_all[:, blk, r:r + 1])
            k.tt("dve", ys[0][:, :], ys[0][:, :], ys[1][:, :], ALU.add)
            k.tt("pool", ys[2][:, :], ys[2][:, :], ys[3][:, :], ALU.add)
            k.tt("dve", ys[0][:, :], ys[0][:, :], ys[2][:, :], ALU.add)
            for ch in range(8):
                p = pm[ch % 2]
                k.mm(p, [(p[:, 0:128], ys[0][:, ch * 128:(ch + 1) * 128], c.ident[:, :], True, True)], transpose=True)
                k.stt(x32[ch][:, j * 128:(j + 1) * 128], p[:, 0:128], c.mod[:, layer, 40 + ch:41 + ch],
                      x32[ch][:, j * 128:(j + 1) * 128], ALU.mult, ALU.add)
        store_x_tile(c, "sp", x32, xout, t0, 512)
    k.stage_end()
    k.stage_end()


DEPTH = 4
SEQ = 8192
BATCH = 8


def prologue_stage(c, xd, xT, Wd):
    k = c.k
    L = c.L
    depth = c.depth
    k.stage_begin()
    cl = k.sb("cl", [128, 8], F32)
    wa = [k.sb("wa", [128, 8, 1024], F32) for _ in range(2)]
    bada = k.sb("bada", [128, depth, 48], F32)
    gl = k.sb("gl", [128, depth + 1, 16], F32)
    pm = k.ps("pm", [128, 512])
    k.dma("sp", cl[:, :], Wd["c_l"].v(Wd["c_l"].t))
    k.dma("sp", bada[:, :, :], Wd["b_ada_l"].v(Wd["b_ada_l"].t))
    k.dma("sp", gl[:, :, :], Wd["g_l"].v(Wd["g_l"].t))
    k.act(cl[:, :], cl[:, :], AF.Silu)
    n = 0
    for l in range(depth):
        for pc in range(6):
            w_ = wa[n % 2]
            n += 1
            k.dma("sp", w_[:, :, :], Wd["w_ada"].v(Wd["w_ada"].t[l][:, pc * 1024:(pc + 1) * 1024]
                                                  .rearrange("(kc p) n -> p kc n", p=128)))
            for nn in range(8):
                col = l * 48 + pc * 8 + nn
                k.mm(pm, [(pm[:, col:col + 1], w_[:, kc, nn * 128:(nn + 1) * 128], cl[:, kc:kc + 1], kc == 0, kc == 7)
                          for kc in range(8)])
    k.tt("dve", c.mod.v(c.mod.t[:, 0:depth, :].rearrange("p l n -> p (l n)")), pm[:, 0:depth * 48],
         bada.v(bada.t[:, :, :].rearrange("p l n -> p (l n)")), ALU.add)
    for l in range(depth):
        k.stt(c.geff[:, l, 0:8], c.mod[:, l, 8:16], 1.0, gl[:, l, 0:8], ALU.add, ALU.mult)
        k.stt(c.geff[:, l, 8:16], c.mod[:, l, 32:40], 1.0, gl[:, l, 8:16], ALU.add, ALU.mult)
    k.copy("dve", c.geff[:, depth, 0:8], gl[:, depth, 0:8])
    k.memset("dve", c.mod[:, depth, :], 0.0)
    xtm = [k.sb("xtm", [128, 1024], F32) for _ in range(4)]
    x32 = [k.sb("x32", [128, 512], F32) for _ in range(8)]
    pt = [k.ps("pt", [128, 512]) for _ in range(2)]
    for it in range(L // 512):
        for j in range(4):
            r0 = it * 512 + j * 128
            k.dma("sp", xtm[j][:, :], xd.v(xd.t[r0:r0 + 128, :]))
        for ch in range(8):
            p = pt[ch % 2]
            k.mm(p, [(p[:, j * 128:(j + 1) * 128], xtm[j][:, ch * 128:(ch + 1) * 128], c.ident[:, :], True, True)
                     for j in range(4)], transpose=True)
            k.copy("act" if ch % 2 else "dve", x32[ch][:, :], p[:, :])
        store_x_tile(c, "sp", x32, xT, it * 512, 512)
    k.stage_end()


def final_stage(c, xT, outd):
    k = c.k
    L = c.L
    k.stage_begin()
    x32 = [k.sb("x32", [128, 512], F32) for _ in range(8)]
    sq = [k.sb("sq", [128, 512], F32) for _ in range(2)]
    rstd = k.sb("rstd", [128, 512], F32)
    ytm = [k.sb("ytm", [128, 1024], F32) for _ in range(2)]
    pst = k.ps("pst", [128, 512])
    pt = [k.ps("pt", [128, 512]) for _ in range(2)]
    n = 0
    for it in range(L // 512):
        load_x_tile(c, "sp", x32, xT, it * 512, 512)
        rmsnorm_mod(c, x32, 512, c.depth, 0, sq, rstd, pst)
        for j in range(4):
            y_ = ytm[j % 2]
            for half in range(2):
                p = pt[n % 2]
                n += 1
                k.mm(p, [(p[:, cc * 128:(cc + 1) * 128], x32[half * 4 + cc][:, j * 128:(j + 1) * 128], c.ident[:, :],
                          True, True) for cc in range(4)], transpose=True)
                k.copy("act" if half else "dve", y_[:, half * 512:(half + 1) * 512], p[:, :])
            r0 = it * 512 + j * 128
            k.dma("sp", outd.v(outd.t[r0:r0 + 128, :]), y_[:, :])
    k.stage_end()


def build_program(L=SEQ, depth=DEPTH, ne=NE):
    nc = bass.Bass("TRN2", target_bir_lowering=False)
    k = K(nc)
    c = make_ctx(k, L, depth + 1)
    c.depth = depth
    make_chunk_masks(c)
    ext = lambda name, shape: k.dram(name, shape, F32, kind="ExternalInput")
    xd = ext("x", [L, D])
    outd = k.dram("out", [L, D], F32, kind="ExternalOutput")
    xa = k.dram("xa", [8, 128, L], F32)
    xb = k.dram("xb", [8, 128, L], F32)
    NA, NB_, NC3 = (depth + 2) // 3, (depth + 1) // 3, depth // 3
    Wd = dict(c_l=ext("c_l", [128, 8]), b_ada_l=ext("b_ada_l", [128, depth, 48]), g_l=ext("g_l", [128, depth + 1, 16]),
              w_ada=ext("w_ada", [depth, D, 6 * D]))
    gla = dict(w_in=ext("gla_w_in", [NA, D, 3072]), w_gk1=ext("gla_w_gk1", [NA, D, 16]),
               w_gk2=ext("gla_w_gk2", [NA, 16, 512]), b_gk=ext("gla_b_gk", [NA, 1, 512]),
               g_onorm_l=ext("gla_g_onorm_l", [NA, 128, 2]), w_out=ext("gla_w_out", [NA, D, D]))
    ml = {}
    if NB_:
        ml = dict(w_up=ext("ml_w_up", [NB_, D, 4096]), w_down=ext("ml_w_down", [NB_, 2048, D]),
                  b_if=ext("ml_b_if", [NB_, 1, 8]))
        for n_, shp in (("bdq", [128, 16, 128]), ("bdk", [128, 16, 128]), ("bdv", [128, 16, 128]),
                        ("bdqT", [128, 16, 128]), ("bdkT", [128, 16, 128]), ("bdvT", [128, 16, 128]),
                        ("conv_l", [128, 16, 5]), ("wif_l", [128, 48, 8]), ("skip_l", [128, 16]), ("gnorm_l", [128, 16])):
            ml[n_] = ext("ml_" + n_, [NB_] + shp)
    s5 = {}
    if NC3:
        s5 = dict(w_in=ext("s5_w_in", [NC3, D, D]), w_out=ext("s5_w_out", [NC3, D, 2 * D]), d_l=ext("s5_d_l", [NC3, 128, 8]))
        for n_ in ("Ablk_re", "Ablk_im", "Lblk", "Bblk_re", "Bblk_im", "Cblk_re", "Cblk_im"):
            s5[n_] = ext("s5_" + n_, [NC3, 128, 32 * 128])
        for n_ in ("Acol_re", "Acol_im", "Lcol"):
            s5[n_] = ext("s5_" + n_, [NC3, 128, 32])
    moe = dict(w_router=ext("moe_w_router", [depth, D, 32]), b_router=ext("moe_b_router", [depth, 1, 32]),
               w_gu_rows=ext("moe_w_gu_rows", [depth, ne * 256, 4 * 2 * D]), b_gu_rows=ext("moe_b_gu_rows", [depth, ne * 128, 16]),
               w_down_rows=ext("moe_w_down_rows", [depth, ne * 128, 8 * D]), b_down=ext("moe_b_down", [depth, ne, D]))
    NG = (4 * L + ne * 511) // 512
    S = dict(h_tm=k.dram("h_tm", [L, D], BF16), rec=k.dram("rec", [NG * 512, 2], I32),
             oslots=k.dram("oslots", [NG * 512, D], F32))

    def sub(dct, j):
        return {n_: Buf(k, b.t[j], "dram") for n_, b in dct.items()}

    prologue_stage(c, xd, xa, Wd)
    for i in range(depth):
        kind, j = i % 3, i // 3
        if kind == 0:
            gla_stage(c, i, xa, xb, sub(gla, j))
        elif kind == 1:
            mlstm_stage(c, i, xa, xb, sub(ml, j))
        else:
            s5_stage(c, i, xa, xb, sub(s5, j))
        mw = sub({n_: moe[n_] for n_ in ("w_router", "b_router")}, i)
        for n_ in ("w_gu_rows", "w_down_rows", "b_gu_rows", "b_down"):
            mw[n_] = Buf(k, moe[n_].t.rearrange("l r c -> (l r) c"), "dram")
        moe_sparse_stage(c, i, xb, xa, mw, S, ne=ne, eoff=i * ne)
    final_stage(c, xa, outd)
    k.finish([outd])
    return nc, k


def host_layouts(inp, depth=DEPTH):
    f32 = lambda a: np.ascontiguousarray(np.asarray(a, dtype=np.float32))
    col = lambda v, n: f32(np.asarray(v).reshape(n, 128).T)
    sh = {}
    sh["b_ada_l"] = f32(np.asarray(inp["b_ada"]).reshape(depth, 48, 128).transpose(2, 0, 1))
    gl = np.zeros((128, depth + 1, 16), np.float32)
    for l in range(depth):
        gl[:, l, 0:8] = col(inp["g_mix"][l], 8)
        gl[:, l, 8:16] = col(inp["g_ffn"][l], 8)
    gl[:, depth, 0:8] = col(inp["g_final"], 8)
    sh["g_l"] = gl
    sh["w_ada"] = f32(inp["w_ada"])
    for n_ in ("gla_w_in", "gla_w_gk1", "gla_w_gk2", "gla_w_out", "ml_w_up", "ml_w_down", "s5_w_in", "s5_w_out",
               "moe_w_router", "moe_b_down"):
        sh[n_] = f32(inp[n_])
    na = np.asarray(inp["gla_b_gk"]).shape[0]
    sh["gla_b_gk"] = f32(np.asarray(inp["gla_b_gk"]).reshape(na, 1, 512))
    sh["gla_g_onorm_l"] = f32(np.stack([col(g, 2) for g in np.asarray(inp["gla_g_onorm"])]))
    nb = np.asarray(inp["ml_w_up"]).shape[0]
    sh["ml_b_if"] = f32(np.asarray(inp["ml_b_if"]).reshape(nb, 1, 8))
    mls = [mlstm_host_layout(*[np.asarray(inp["ml_" + n_][j]) for n_ in
                               ("conv_w", "conv_b", "w_q", "w_k", "w_v", "w_if", "skip", "g_norm")]) for j in range(nb)]
    for n_ in mls[0]:
        sh["ml_" + n_] = f32(np.stack([m[n_] for m in mls]))
    n3 = np.asarray(inp["s5_w_in"]).shape[0]
    s5s = [s5_host_layout(*[np.asarray(inp["s5_" + n_][j]) for n_ in
                            ("a_re", "a_im", "log_dt", "b_re", "b_im", "c_re", "c_im", "d")]) for j in range(n3)]
    for n_ in s5s[0]:
        sh["s5_" + n_] = f32(np.stack([m[n_] for m in s5s]))
    sh["moe_b_router"] = f32(np.asarray(inp["moe_b_router"]).reshape(depth, 1, 32))
    bgu = np.asarray(inp["moe_b_gu"])
    ne = bgu.shape[1]
    sh["moe_b_gu_rows"] = f32(bgu.reshape(depth, ne, 16, 128).transpose(0, 1, 3, 2).reshape(depth, ne * 128, 16))
    sh["moe_w_gu_rows"] = f32(np.asarray(inp["moe_w_gu"]).reshape(depth, ne, 2, 4, 128, 2 * D)
                              .transpose(0, 1, 2, 4, 3, 5)).reshape(depth, ne * 256, 4 * 2 * D)
    sh["moe_w_down_rows"] = f32(np.asarray(inp["moe_w_down"]).reshape(depth, ne, 8, 128, D)
                                .transpose(0, 1, 3, 2, 4)).reshape(depth, ne * 128, 8 * D)
    return sh


_PROG = {}


def kernel(**inputs):
    x = np.asarray(inputs["x"], dtype=np.float32)
    cvec = np.asarray(inputs["c"], dtype=np.float32)
    B, L, _ = x.shape
    key = (L,)
    if key not in _PROG:
        _PROG[key] = build_program(L=L)[0]
    nc = _PROG[key]
    sh = host_layouts(inputs)
    in_maps = []
    for b in range(B):
        m = dict(sh)
        m["x"] = np.ascontiguousarray(x[b])
        m["c_l"] = np.ascontiguousarray(cvec[b].reshape(8, 128).T)
        in_maps.append(m)
    res = run_bass_kernel_spmd(nc, in_maps, core_ids=list(range(B)))
    return np.stack([np.asarray(r["out"], dtype=np.float32) for r in res.results], axis=0)
```

```python
import numpy as np
import concourse.bass as bass
import concourse.mybir as mybir

F32 = mybir.dt.float32
BF16 = mybir.dt.bfloat16
AF = mybir.ActivationFunctionType
ALU = mybir.AluOpType
AX = mybir.AxisListType

SAME_ENGINE_SYNC = True


class V:
    __slots__ = ("b", "ap")

    def __init__(self, b, ap):
        self.b = b
        self.ap = ap


class Buf:
    def __init__(self, k, t, kind):
        self.k = k
        self.t = t
        self.kind = kind
        self.w = None
        self.r = {}
        self.ds = {}

    def __getitem__(self, idx):
        return V(self, self.t[idx])

    def v(self, ap):
        return V(self, ap)


class K:
    def __init__(self, nc):
        self.nc = nc
        self.E = dict(pe=nc.tensor, act=nc.scalar, dve=nc.vector, pool=nc.gpsimd, sp=nc.sync)
        self.sem = {k: nc.alloc_semaphore("sem_" + k) for k in self.E}
        self.cnt = {k: 0 for k in self.E}
        self.waited = {k: {} for k in self.E}
        self.semowner = {}
        self.n = 0
        self.ninstr = 0
        self.guards = []
        self.stage_bufs = []
        self.dsem_pool = {'hw': [], 'sw': []}
        self.all_dsem_bufs = []

    def sb(self, name, shape, dt=F32):
        self.n += 1
        name = "%s_%d" % (name, self.n)
        if self.guards:
            g = self.nc.sbuf_tensor(name, list(shape), dt)
            t = g.__enter__()
            self.guards[-1].append(g)
        else:
            t = self.nc.alloc_sbuf_tensor(name, list(shape), dt)
        b = Buf(self, t, "sb")
        if self.stage_bufs:
            self.stage_bufs[-1].append(b)
        return b

    def ps(self, name, shape, dt=F32):
        self.n += 1
        name = "%s_%d" % (name, self.n)
        if self.guards:
            g = self.nc.psum_tensor(name, list(shape), dt)
            t = g.__enter__()
            self.guards[-1].append(g)
        else:
            t = self.nc.alloc_psum_tensor(name, list(shape), dt)
        b = Buf(self, t, "ps")
        if self.stage_bufs:
            self.stage_bufs[-1].append(b)
        return b

    def stage_begin(self):
        self.guards.append([])
        self.stage_bufs.append([])

    def stage_end(self):
        self.barrier()
        for b in self.stage_bufs.pop():
            for kind, sc in b.ds.items():
                self.dsem_pool[kind].append(sc)
            b.ds = {}
        for g in reversed(self.guards.pop()):
            g.__exit__(None, None, None)

    def barrier(self):
        evs = [(self.sem[e], self.cnt[e], e) for e in self.E if self.cnt[e] > 0]
        dmas = [(sc[0], 16 * sc[1], "dma") for b in self.all_dsem_bufs for sc in b.ds.values() if sc[1] > 0]
        for e in self.E:
            for ev in evs:
                if ev[2] != e:
                    self._wait(e, ev)
            for ev in dmas:
                self._wait(e, ev)

    def dram(self, name, shape, dt=F32, kind="Internal"):
        return Buf(self, self.nc.dram_tensor(name, list(shape), dt, kind=kind).ap(), "dram")

    def region(self):
        return Buf(self, None, "dram")

    def _wait(self, e, ev):
        if ev is None:
            return
        sem, val, src = ev
        if src == e and (e == "pe" or not SAME_ENGINE_SYNC):
            return
        if src == "dma":
            val = 16 * self.semowner[id(sem)][1]
        w = self.waited[e]
        key = id(sem)
        if w.get(key, 0) >= val:
            return
        self.E[e].wait_ge(sem, val)
        w[key] = val

    def _deps(self, e, reads, writes):
        for b in reads:
            self._wait(e, b.w)
            if b.kind == "ps":
                for ke, ev in b.r.items():
                    if ke != e:
                        self._wait(e, ev)
        for b in writes:
            self._wait(e, b.w)
            for ev in b.r.values():
                self._wait(e, ev)

    def _commit(self, e, ins, reads, writes):
        self.cnt[e] += 1
        ev = (self.sem[e], self.cnt[e], e)
        ins.then_inc(self.sem[e], 1)
        for b in writes:
            b.w = ev
            b.r = {}
        ws = set(id(b) for b in writes)
        for b in reads:
            if id(b) not in ws:
                b.r[e] = ev
        self.ninstr += 1

    @staticmethod
    def _split(ops):
        bufs, aps = [], []
        for o in ops:
            if isinstance(o, V):
                bufs.append(o.b)
                aps.append(o.ap)
            else:
                aps.append(o)
        return bufs, aps

    def op(self, e, name, out, ins, extra_reads=(), **kw):
        rb, raps = self._split(ins)
        rb = rb + [x.b for x in extra_reads]
        kwv = {}
        wb = [out.b]
        for kk, vv in kw.items():
            if isinstance(vv, V):
                if kk == "accum_out":
                    wb.append(vv.b)
                else:
                    rb.append(vv.b)
                kwv[kk] = vv.ap
            else:
                kwv[kk] = vv
        self._deps(e, rb, wb)
        ins_ = getattr(self.E[e], name)(out.ap, *raps, **kwv)
        self._commit(e, ins_, rb, wb)
        return ins_

    def act(self, out, in_, func, bias=None, scale=None, e="act", accum_out=None):
        kw = {}
        if bias is not None:
            kw["bias"] = bias
        if scale is not None:
            kw["scale"] = scale
        rb = [in_.b]
        kwv = {}
        for kk, vv in kw.items():
            if isinstance(vv, V):
                rb.append(vv.b)
                kwv[kk] = vv.ap
            else:
                kwv[kk] = vv
        wb = [out.b]
        if accum_out is not None:
            wb.append(accum_out.b)
            kwv["accum_out"] = accum_out.ap
        self._deps("act", rb, wb)
        ins_ = self.nc.scalar.activation(out=out.ap, in_=in_.ap, func=func, **kwv)
        self._commit("act", ins_, rb, wb)

    def ts(self, e, out, in0, s1, s2, op0, op1=None):
        if op1 is None:
            return self.op(e, "tensor_scalar", out, [in0, s1, None, op0])
        return self.op(e, "tensor_scalar", out, [in0, s1, s2, op0, op1])

    def stt(self, out, in0, scalar, in1, op0, op1, e="dve", **kw):
        return self.op(e, "scalar_tensor_tensor", out, [in0, scalar, in1, op0, op1], **kw)

    def tt(self, e, out, in0, in1, op):
        return self.op(e, "tensor_tensor", out, [in0, in1, op])

    def copy(self, e, out, in_):
        if e == "act":
            return self.act(out, in_, AF.Copy)
        return self.op(e, "tensor_copy", out, [in_])

    def memset(self, e, out, val):
        self._deps(e, [], [out.b])
        ins_ = self.E[e].memset(out.ap, val)
        self._commit(e, ins_, [], [out.b])

    def mm(self, outbuf, mms, transpose=False):
        rb = []
        seen = set()
        for (o, l, r, st, sp) in mms:
            for x in (l, r):
                if id(x.b) not in seen:
                    seen.add(id(x.b))
                    rb.append(x.b)
        self._deps("pe", rb, [outbuf])
        ins_ = None
        for (o, l, r, st, sp) in mms:
            if transpose:
                ins_ = self.nc.tensor.transpose(o.ap, l.ap, r.ap)
            else:
                ins_ = self.nc.tensor.matmul(o.ap, l.ap, r.ap, start=st, stop=sp)
            self.ninstr += 1
        self._commit("pe", ins_, rb, [outbuf])

    def _dsem(self, sbuf, kind):
        if kind not in sbuf.ds:
            if self.dsem_pool[kind]:
                sc = self.dsem_pool[kind].pop()
            else:
                sc = [self.nc.alloc_semaphore("dsem%d" % self.n), 0]
                self.n += 1
            sbuf.ds[kind] = sc
            self.semowner[id(sc[0])] = sc
            if sbuf not in self.all_dsem_bufs:
                self.all_dsem_bufs.append(sbuf)
        sc = sbuf.ds[kind]
        sc[1] += 1
        return sc

    def dma(self, q, out, in_, sem_buf=None, **kw):
        ob, ib = out.b, in_.b
        self._deps(q, [ib], [ob])
        sbuf = sem_buf or (ob if ob.kind == "sb" else ib)
        sc = self._dsem(sbuf, "sw" if q == "pool" else "hw")
        ins_ = self.E[q].dma_start(out=out.ap, in_=in_.ap, **kw)
        ins_.then_inc(sc[0], 16)
        ev = (sc[0], 16 * sc[1], "dma")
        ob.w = ev
        ob.r = {}
        if ib is not ob:
            ib.r[id(sc[0])] = ev
        self.ninstr += 1

    def idma(self, out, in_, idx, scatter=False, **kw):
        ob, ib = out.b, in_.b
        self._deps("pool", [ib, idx.b], [ob])
        sbuf = ob if ob.kind == "sb" else ib
        sc = self._dsem(sbuf, "sw")
        off = bass.IndirectOffsetOnAxis(ap=idx.ap, axis=0)
        if scatter:
            ins_ = self.nc.gpsimd.indirect_dma_start(out=out.ap, out_offset=off, in_=in_.ap, in_offset=None, **kw)
        else:
            ins_ = self.nc.gpsimd.indirect_dma_start(out=out.ap, out_offset=None, in_=in_.ap, in_offset=off, **kw)
        ins_.then_inc(sc[0], 16)
        ev = (sc[0], 16 * sc[1], "dma")
        ob.w = ev
        ob.r = {}
        ib.r[id(sc[0])] = ev
        idx.b.r[id(sc[0])] = ev
        self.ninstr += 1

    def vload(self, e, v, lo, hi):
        self._deps(e, [v.b], [])
        return self.E[e].value_load(v.ap, min_val=lo, max_val=hi)

    def finish(self, bufs, e="sp"):
        for b in bufs:
            self._wait(e, b.w)
            for ev in b.r.values():
                self._wait(e, ev)

from concourse.bass_utils import run_bass_kernel_spmd

D = 1024
EPS = 1e-6
NE = 32
ALPHA = 1.702


class Ctx:
    pass


def make_ctx(k, L, depth):
    c = Ctx()
    c.k = k
    c.L = L
    c.depth = depth
    c.ones = k.sb("ones", [128, 512], F32)
    c.ident = k.sb("ident", [128, 128], F32)
    c.identb = k.sb("identb", [128, 128], BF16)
    c.mod = k.sb("mod", [128, depth, 48], F32)
    c.geff = k.sb("geff", [128, depth, 16], F32)
    k.memset("dve", c.ones[:, 0:512], 1.0)
    k.op("pool", "affine_select", c.ident[:, :], [c.ones[:, 0:128]], pattern=[[-1, 128]],
         compare_op=ALU.is_equal, fill=0.0, base=0, channel_multiplier=1)
    k.copy("dve", c.identb[:, :], c.ident[:, :])
    return c


def load_x_tile(c, q, x32, xin, t0, T):
    for ch in range(8):
        c.k.dma(q, x32[ch][:, :T], xin.v(xin.t[ch, :, t0:t0 + T]))


def store_x_tile(c, q, x32, xout, t0, T):
    for ch in range(8):
        c.k.dma(q, xout.v(xout.t[ch, :, t0:t0 + T]), x32[ch][:, :T])


def rmsnorm_mod(c, x32, T, layer, which, sq, rstd, ps_stat, out_bf=None, off=0, cp_eng="pool"):
    k = c.k
    for ch in range(8):
        s = sq[ch % len(sq)]
        k.act(s[:, :T], x32[ch][:, :T], AF.Square)
        k.mm(ps_stat, [(ps_stat[:, :T], c.ones[:, 0:128], s[:, :T], ch == 0, ch == 7)])
    k.ts("dve", rstd[:, :T], ps_stat[:, :T], 1.0 / D, EPS, ALU.mult, ALU.add)
    k.act(rstd[:, :T], rstd[:, :T], AF.Sqrt)
    k.op("dve", "reciprocal", rstd[:, :T], [rstd[:, :T]])
    shb = 0 if which == 0 else 24
    for ch in range(8):
        k.stt(x32[ch][:, :T], x32[ch][:, :T], c.geff[:, layer, which * 8 + ch:which * 8 + ch + 1],
              rstd[:, :T], ALU.mult, ALU.mult)
        k.act(x32[ch][:, :T], x32[ch][:, :T], AF.Identity,
              bias=c.mod[:, layer, shb + ch:shb + ch + 1])
        if out_bf is not None:
            k.copy(cp_eng, out_bf[:, ch, off:off + T], x32[ch][:, :T])


def moe_stage(c, layer, xin, xout, W, ne=NE):
    k = c.k
    L = c.L
    ST = min(1024, L)
    NSUB = ST // 128
    NTS = ST // 512
    k.stage_begin()
    wgu = [k.sb("wgu", [128, 8, 2048], BF16) for _ in range(2)]
    wdn = [k.sb("wdn", [128, 8, 1024], BF16) for _ in range(2)]
    acc = [k.sb("acc", [128, 1024], F32) for _ in range(NSUB)]
    hbf = k.sb("hbf", [128, 8, ST], BF16)
    x32 = [k.sb("x32", [128, 512], F32) for _ in range(8)]
    actT = [k.sb("actT", [128, 8, 512], BF16) for _ in range(2)]
    tg = [k.sb("tg", [128, 512], F32) for _ in range(2)]
    tt_ = [k.sb("tt", [128, 512], F32) for _ in range(2)]
    tu = [k.sb("tu", [128, 512], F32) for _ in range(2)]
    sq = tg
    rstd = tt_[0]
    wr = k.sb("wr", [128, 8, 32], F32)
    br = k.sb("br", [1, 32], F32)
    bgu = k.sb("bgu", [128, ne, 16], F32)
    bdn = k.sb("bdn", [32, 1024], F32)
    comb = k.sb("comb", [128, NSUB, 32], F32)
    combs = k.sb("combs", [128, NSUB, 32], F32)
    combT = k.sb("combT", [32, ST], F32)
    lg = k.sb("lg", [128, 32], F32)
    m8 = k.sb("m8", [128, 8], F32)
    negm = k.sb("negm", [128, 1], F32)
    mask = k.sb("mask", [128, 32], F32)
    ex = k.sb("ex", [128, 32], F32)
    ssum = k.sb("ssum", [128, 1], F32)
    pg = [k.ps("pg", [128, 512]) for _ in range(2)]
    pu = [k.ps("pu", [128, 512]) for _ in range(2)]
    pd = [k.ps("pd", [128, 512]) for _ in range(2)]
    pm = [k.ps("pm", [128, 512]) for _ in range(2)]

    k.dma("sp", wr[:, :, :], W["w_router"].v(W["w_router"].t.rearrange("(kc p) e -> p kc e", p=128)))
    k.dma("sp", br[:, :], W["b_router"].v(W["b_router"].t))
    k.dma("sp", bgu[:, :, :], W["b_gu_l"].v(W["b_gu_l"].t))
    k.dma("sp", bdn[:ne, :], W["b_down"].v(W["b_down"].t))
    k.ts("dve", bgu[:, :, 8:16], bgu[:, :, 8:16], 1.0, None, ALU.add)

    def load_w(gi):
        e = gi % ne
        k.dma("pool", wgu[gi % 2][:, :, :],
              W["w_gu"].v(W["w_gu"].t[e].rearrange("(kc p) n -> p kc n", p=128)))
        k.dma("pool", wdn[gi % 2][:, :, :],
              W["w_down"].v(W["w_down"].t[e].rearrange("(kc p) n -> p kc n", p=128)))

    nst = L // ST
    load_w(0)
    pmi = 0
    cnt = 0
    for s in range(nst):
        for ts_ in range(NTS):
            t0 = s * ST + ts_ * 512
            load_x_tile(c, "sp", x32, xin, t0, 512)
            rmsnorm_mod(c, x32, 512, layer, 1, sq, rstd, pm[0], out_bf=hbf, off=ts_ * 512, cp_eng="dve")
            for j in range(4):
                sub = ts_ * 4 + j
                p = pm[1]
                mms = [(p[:, :32], x32[ch][:, j * 128:(j + 1) * 128], wr[:, ch, :], ch == 0, False)
                       for ch in range(8)]
                mms.append((p[:, :32], c.ones[0:1, 0:128], br[0:1, :], False, True))
                k.mm(p, mms)
                k.copy("dve", lg[:, :], p[:, :32])
                k.op("dve", "max", m8[:, :], [lg[:, :]])
                k.ts("dve", mask[:, :], lg[:, :], m8[:, 3:4], None, ALU.is_ge)
                k.ts("dve", negm[:, :], m8[:, 0:1], -1.0, None, ALU.mult)
                k.act(ex[:, :], lg[:, :], AF.Exp, bias=negm[:, 0:1], scale=1.0)
                k.stt(ex[:, :], ex[:, :], 1.0, mask[:, :], ALU.mult, ALU.mult, accum_out=ssum[:, :])
                k.op("dve", "reciprocal", ssum[:, :], [ssum[:, :]])
                k.ts("dve", comb[:, sub, :], ex[:, :], ssum[:, 0:1], None, ALU.mult)
                k.ts("dve", combs[:, sub, :], ex[:, :], ssum[:, 0:1], 1.0 / ALPHA, ALU.mult, ALU.mult)
                k.mm(p, [(p[:32, 128:256], comb[:, sub, :], c.ident[:, :], True, True)], transpose=True)
                k.copy("dve", combT[:, sub * 128:(sub + 1) * 128], p[:32, 128:256])
        for e in range(ne):
            gi = s * ne + e
            if gi + 1 < nst * ne:
                load_w(gi + 1)
            wg, wd = wgu[gi % 2], wdn[gi % 2]
            for ts_ in range(NTS):
                aT = actT[cnt % 2]
                cnt += 1
                for n in range(8):
                    a, b = pg[n % 2], pu[n % 2]
                    k.mm(a, [(a[:, :], wg[:, kc, n * 128:(n + 1) * 128], hbf[:, kc, ts_ * 512:(ts_ + 1) * 512],
                              kc == 0, kc == 7) for kc in range(8)])
                    k.mm(b, [(b[:, :], wg[:, kc, 1024 + n * 128:1024 + (n + 1) * 128],
                              hbf[:, kc, ts_ * 512:(ts_ + 1) * 512], kc == 0, kc == 7) for kc in range(8)])
                    g, t, u = tg[n % 2], tt_[n % 2], tu[n % 2]
                    k.ts("dve", g[:, :], a[:, :], bgu[:, e, n:n + 1], 7.0, ALU.add, ALU.min)
                    k.act(t[:, :], g[:, :], AF.Silu, scale=ALPHA)
                    k.ts("dve", u[:, :], b[:, :], bgu[:, e, 8 + n:9 + n], -6.0, ALU.add, ALU.max)
                    k.stt(aT[:, n, :], u[:, :], 8.0, t[:, :], ALU.min, ALU.mult)
                for j in range(4):
                    sub = ts_ * 4 + j
                    for half in range(2):
                        p = pd[(j * 2 + half) % 2]
                        k.mm(p, [(p[:, :], aT[:, kc, j * 128:(j + 1) * 128], wd[:, kc, half * 512:(half + 1) * 512],
                                  kc == 0, kc == 7) for kc in range(8)])
                        av = acc[sub][:, half * 512:(half + 1) * 512]
                        if e == 0:
                            k.ts("dve", av, p[:, :], combs[:, sub, e:e + 1], None, ALU.mult)
                        else:
                            k.stt(av, p[:, :], combs[:, sub, e:e + 1], av, ALU.mult, ALU.add)
        for sub in range(NSUB):
            for half in range(2):
                p = pm[(sub * 2 + half) % 2]
                k.mm(p, [(p[:, :], combT[:ne, sub * 128:(sub + 1) * 128], bdn[:ne, half * 512:(half + 1) * 512],
                          True, True)])
                av = acc[sub][:, half * 512:(half + 1) * 512]
                k.tt("dve", av, av, p[:, :], ALU.add)
        for ts_ in range(NTS):
            t0 = s * ST + ts_ * 512
            load_x_tile(c, "sp", x32, xin, t0, 512)
            for ch in range(8):
                p = pm[ch % 2]
                k.mm(p, [(p[:, j * 128:(j + 1) * 128], acc[ts_ * 4 + j][:, ch * 128:(ch + 1) * 128], c.ident[:, :],
                          True, True) for j in range(4)], transpose=True)
                k.stt(x32[ch][:, :], p[:, :], c.mod[:, layer, 40 + ch:41 + ch], x32[ch][:, :], ALU.mult, ALU.add)
            store_x_tile(c, "sp", x32, xout, t0, 512)
    k.stage_end()


def make_chunk_masks(c):
    k = c.k
    c.mrev = k.sb("mrev", [128, 128], F32)
    c.mch = k.sb("mch", [128, 2], F32)
    k.op("pool", "affine_select", c.mrev[:, :], [c.ones[:, 0:128]], pattern=[[-1, 128]],
         compare_op=ALU.is_gt, fill=0.0, base=0, channel_multiplier=1)
    k.op("pool", "affine_select", c.mrev[:, 0:64], [c.mrev[:, 0:64]], pattern=[[0, 64]],
         compare_op=ALU.is_gt, fill=0.0, base=64, channel_multiplier=-1)
    k.op("pool", "affine_select", c.mch[:, 0:1], [c.ones[:, 0:1]], pattern=[[0, 1]],
         compare_op=ALU.is_gt, fill=0.0, base=64, channel_multiplier=-1)
    k.op("pool", "affine_select", c.mch[:, 1:2], [c.ones[:, 0:1]], pattern=[[0, 1]],
         compare_op=ALU.is_ge, fill=0.0, base=-64, channel_multiplier=1)


def gla_stage(c, layer, xin, xout, W):
    k = c.k
    L = c.L
    T = 512
    k.stage_begin()
    w_in = k.sb("w_in", [128, 8, 3072], BF16)
    w_out = k.sb("w_out", [128, 8, 1024], BF16)
    w_gk1 = k.sb("w_gk1", [128, 8, 16], BF16)
    w_gk2 = k.sb("w_gk2", [16, 512], F32)
    b_gk = k.sb("b_gk", [1, 512], F32)
    g_on = k.sb("g_on", [128, 2], F32)
    x32 = [k.sb("x32", [128, T], F32) for _ in range(8)]
    hbf = k.sb("hbf", [128, 8, T], BF16)
    sq = [k.sb("sq", [128, T], F32) for _ in range(2)]
    rstd = k.sb("rstd", [128, T], F32)
    qT = [k.sb("qT", [128, T], BF16) for _ in range(4)]
    kdec = [k.sb("kdec", [128, 512], BF16) for _ in range(4)]
    vbf = [k.sb("vbf", [128, 1024], BF16) for _ in range(4)]
    gs = [k.sb("gs", [128, T], F32) for _ in range(8)]
    rT = k.sb("rT", [16, T], F32)
    la = [k.sb("la", [128, 512], F32) for _ in range(2)]
    edec = [k.sb("edec", [128, 512], F32) for _ in range(2)]
    dcy = k.sb("dcy", [128, 4, 8], F32)
    S = [k.sb("S", [128, 256], F32) for _ in range(4)]
    Sb = [[k.sb("Sb", [128, 256], BF16) for _ in range(2)] for _ in range(4)]
    oT = [k.sb("oT", [128, 2, T], F32) for _ in range(4)]
    onb = k.sb("onb", [128, 8, T], BF16)
    tmp = [k.sb("tmp", [128, T], F32) for _ in range(2)]
    pA = [k.ps("pA", [128, 512]) for _ in range(2)]
    pU = [k.ps("pU", [128, 512]) for _ in range(2)]
    pO = [k.ps("pO", [128, 512]) for _ in range(2)]
    ptot = k.ps("ptot", [128, 512])
    pst = k.ps("pst", [128, 512])

    k.dma("pool", w_in[:, :, :], W["w_in"].v(W["w_in"].t.rearrange("(kc p) n -> p kc n", p=128)))
    k.dma("pool", w_out[:, :, :], W["w_out"].v(W["w_out"].t.rearrange("(kc p) n -> p kc n", p=128)))
    k.dma("pool", w_gk1[:, :, :], W["w_gk1"].v(W["w_gk1"].t.rearrange("(kc p) n -> p kc n", p=128)))
    k.dma("sp", w_gk2[:, :], W["w_gk2"].v(W["w_gk2"].t))
    k.dma("sp", b_gk[:, :], W["b_gk"].v(W["b_gk"].t))
    k.dma("sp", g_on[:, :], W["g_onorm_l"].v(W["g_onorm_l"].t))
    for h in range(4):
        k.memset("dve", S[h][:, :], 0.0)

    pai = [0]

    def nextA():
        pai[0] += 1
        return pA[pai[0] % 2]

    for it in range(L // T):
        t0 = it * T
        load_x_tile(c, "sp", x32, xin, t0, T)
        rmsnorm_mod(c, x32, T, layer, 0, sq, rstd, pst, out_bf=hbf, off=0, cp_eng="dve")
        load_x_tile(c, "sp", x32, xin, t0, T)
        for h in range(4):
            p = nextA()
            k.mm(p, [(p[:, :], w_in[:, kc, h * 128:(h + 1) * 128], hbf[:, kc, :], kc == 0, kc == 7)
                     for kc in range(8)])
            k.act(qT[h][:, :], p[:, :], AF.Copy, scale=128.0 ** -0.5)
        for n in range(8):
            p = nextA()
            k.mm(p, [(p[:, :], w_in[:, kc, 2048 + n * 128:2048 + (n + 1) * 128], hbf[:, kc, :], kc == 0, kc == 7)
                     for kc in range(8)])
            k.act(gs[n][:, :], p[:, :], AF.Silu)
        p = nextA()
        k.mm(p, [(p[:16, :], w_gk1[:, kc, :], hbf[:, kc, :], kc == 0, kc == 7) for kc in range(8)])
        k.copy("dve", rT[:, :], p[:16, :])
        for j in range(4):
            for half in range(2):
                p = nextA()
                k.mm(p, [(p[:, :], hbf[:, kc, j * 128:(j + 1) * 128],
                          w_in[:, kc, 1024 + half * 512:1024 + (half + 1) * 512], kc == 0, kc == 7)
                         for kc in range(8)])
                k.copy("act", vbf[j][:, half * 512:(half + 1) * 512], p[:, :])
            p = nextA()
            k.mm(p, [(p[:, :], rT[:16, j * 128:(j + 1) * 128], w_gk2[:16, :], True, False),
                     (p[:, :], c.ones[0:1, 0:128], b_gk[0:1, :], False, True)])
            l_ = la[j % 2]
            k.act(l_[:, :], p[:, :], AF.Exp, scale=-1.0)
            k.act(l_[:, :], l_[:, :], AF.Ln, bias=1.0)
            p = nextA()
            k.mm(p, [(p[:, :], c.mrev[:, :], l_[:, :], True, True)])
            ed = edec[j % 2]
            k.act(ed[:, :], p[:, :], AF.Exp, scale=-1.0 / 16.0)
            for h in range(4):
                k.mm(ptot, [(ptot[:, h * 8 + 2 * j:h * 8 + 2 * j + 2], l_[:, h * 128:(h + 1) * 128], c.mch[:, :],
                             True, True)])
            p = nextA()
            k.mm(p, [(p[:, :], hbf[:, kc, j * 128:(j + 1) * 128], w_in[:, kc, 512:1024], kc == 0, kc == 7)
                     for kc in range(8)])
            k.tt("dve", kdec[j][:, :], p[:, :], ed[:, :], ALU.mult)
        k.act(dcy[:, :, :], ptot[:, 0:32], AF.Exp, scale=-1.0 / 16.0)
        n_u = 0
        for cc in range(8):
            j, hf = cc // 2, cc % 2
            for h in range(4):
                pu_ = pU[n_u % 2]
                po_ = pO[n_u % 2]
                n_u += 1
                k.mm(pu_, [(pu_[:, :256], kdec[j][64 * hf:64 * hf + 64, h * 128:(h + 1) * 128],
                            vbf[j][64 * hf:64 * hf + 64, h * 256:(h + 1) * 256], True, True)])
                k.stt(S[h][:, :], S[h][:, :], dcy[:, h, cc:cc + 1], pu_[:, :256], ALU.mult, ALU.add)
                sb_ = Sb[h][cc % 2]
                k.copy("act", sb_[:, :], S[h][:, :])
                k.mm(po_, [(po_[:, dv * 64:(dv + 1) * 64], sb_[:, dv * 128:(dv + 1) * 128],
                            qT[h][:, cc * 64:(cc + 1) * 64], True, True) for dv in range(2)])
                k.copy("act", oT[h][:, :, cc * 64:(cc + 1) * 64],
                       po_.v(po_.t[:, 0:128].rearrange("p (d t) -> p d t", d=2)))
        for h in range(4):
            for dv in range(2):
                s_ = sq[dv]
                k.act(s_[:, :], oT[h][:, dv, :], AF.Square)
                k.mm(pst, [(pst[:, :], c.ones[:, 0:128], s_[:, :], dv == 0, dv == 1)])
            k.ts("dve", rstd[:, :], pst[:, :], 1.0 / 256.0, EPS, ALU.mult, ALU.add)
            k.act(rstd[:, :], rstd[:, :], AF.Sqrt)
            k.op("dve", "reciprocal", rstd[:, :], [rstd[:, :]])
            for dv in range(2):
                t_ = tmp[dv]
                k.stt(t_[:, :], oT[h][:, dv, :], g_on[:, dv:dv + 1], rstd[:, :], ALU.mult, ALU.mult)
                k.tt("dve", onb[:, h * 2 + dv, :], t_[:, :], gs[h * 2 + dv][:, :], ALU.mult)
        for n in range(8):
            p = nextA()
            k.mm(p, [(p[:, :], w_out[:, kc, n * 128:(n + 1) * 128], onb[:, kc, :], kc == 0, kc == 7)
                     for kc in range(8)])
            k.stt(x32[n][:, :], p[:, :], c.mod[:, layer, 16 + n:17 + n], x32[n][:, :], ALU.mult, ALU.add)
        store_x_tile(c, "sp", x32, xout, t0, T)
    k.stage_end()


import math
PI = math.pi


def sin_turns(k, out, x, phase, y, yi, m):
    k.ts("dve", y, x, 1.0 / (2.0 * PI), phase, ALU.mult, ALU.add)
    k.copy("dve", yi, y)
    k.copy("dve", m, yi)
    k.tt("dve", y, y, m, ALU.subtract)
    k.ts("dve", m, y, 0.5, None, ALU.is_gt)
    k.tt("dve", y, y, m, ALU.subtract)
    k.ts("dve", m, y, -0.5, None, ALU.is_lt)
    k.tt("dve", y, y, m, ALU.add)
    k.act(out, y, AF.Sin, scale=2.0 * PI)


def s5_stage(c, layer, xin, xout, W):
    k = c.k
    L = c.L
    T = 512
    TS = 128
    NB = 32
    PE_ = "dve"
    k.stage_begin()
    w_in = k.sb("w_in", [128, 8, 1024], BF16)
    w_out = k.sb("w_out", [128, 8, 2048], BF16)
    Bbr = k.sb("Bbr", [128, NB, 128], BF16)
    Bbi = k.sb("Bbi", [128, NB, 128], BF16)
    Cbr = k.sb("Cbr", [128, NB, 128], BF16)
    Cbi = k.sb("Cbi", [128, NB, 128], BF16)
    cost = k.sb("cost", [128, NB, TS], F32)
    sint = k.sb("sint", [128, NB, TS], F32)
    rcol = k.sb("rcol", [128, NB], F32)
    dl = k.sb("dl", [128, 8], F32)
    spr = k.sb("spr", [128, NB], F32)
    spi = k.sb("spi", [128, NB], F32)
    k.dma("pool", w_in[:, :, :], W["w_in"].v(W["w_in"].t.rearrange("(kc p) n -> p kc n", p=128)))
    k.dma("pool", w_out[:, :, :], W["w_out"].v(W["w_out"].t.rearrange("(kc p) n -> p kc n", p=128)))
    k.dma("sp", dl[:, :], W["d_l"].v(W["d_l"].t))
    k.memset("dve", spr[:, :], 0.0)
    k.memset("dve", spi[:, :], 0.0)

    PH = 9
    k.stage_begin()
    Q = 1024
    nm = ["are", "aim", "ldt", "bre", "bim", "t0", "t1", "t2", "t3", "t4", "t5", "t6"]
    A = {n: k.sb(n, [128, Q], F32) for n in nm}
    A["y"] = k.sb("y", [128, Q], F32)
    A["yi"] = k.sb("yi", [128, Q], mybir.dt.int32)
    for q in range(4 if PH >= 1 else 0):
        sl = slice(q * Q, (q + 1) * Q)
        for n, src in (("are", "Ablk_re"), ("aim", "Ablk_im"), ("ldt", "Lblk"), ("bre", "Bblk_re"), ("bim", "Bblk_im")):
            k.dma("sp", A[n][:, :], W[src].v(W[src].t[:, sl]))
        dt_, ar, ai, er, sn, cs, t6 = A["t0"], A["t1"], A["t2"], A["t3"], A["t4"], A["t5"], A["t6"]
        k.act(dt_[:, :], A["ldt"][:, :], AF.Exp)
        k.tt("dve", ar[:, :], A["are"][:, :], dt_[:, :], ALU.mult)
        k.tt("dve", ai[:, :], A["aim"][:, :], dt_[:, :], ALU.mult)
        k.act(er[:, :], ar[:, :], AF.Exp)
        sin_turns(k, sn[:, :], ai[:, :], 0.0, A["y"][:, :], A["yi"][:, :], t6[:, :])
        sin_turns(k, cs[:, :], ai[:, :], 0.25, A["y"][:, :], A["yi"][:, :], t6[:, :])
        nr, ni = ar, ai
        k.tt("dve", nr[:, :], er[:, :], cs[:, :], ALU.mult)
        k.ts("dve", nr[:, :], nr[:, :], -1.0, None, ALU.add)
        k.tt("dve", ni[:, :], er[:, :], sn[:, :], ALU.mult)
        den = er
        k.tt("dve", den[:, :], A["are"][:, :], A["are"][:, :], ALU.mult)
        k.tt("dve", t6[:, :], A["aim"][:, :], A["aim"][:, :], ALU.mult)
        k.tt("dve", den[:, :], den[:, :], t6[:, :], ALU.add)
        k.op("dve", "reciprocal", den[:, :], [den[:, :]])
        cr, ci = sn, cs
        k.tt("dve", cr[:, :], nr[:, :], A["are"][:, :], ALU.mult)
        k.tt("dve", t6[:, :], ni[:, :], A["aim"][:, :], ALU.mult)
        k.tt("dve", cr[:, :], cr[:, :], t6[:, :], ALU.add)
        k.tt("dve", cr[:, :], cr[:, :], den[:, :], ALU.mult)
        k.tt("dve", ci[:, :], ni[:, :], A["are"][:, :], ALU.mult)
        k.tt("dve", t6[:, :], nr[:, :], A["aim"][:, :], ALU.mult)
        k.tt("dve", ci[:, :], ci[:, :], t6[:, :], ALU.subtract)
        k.tt("dve", ci[:, :], ci[:, :], den[:, :], ALU.mult)
        x1, x2 = ar, ai
        k.tt("dve", x1[:, :], cr[:, :], A["bre"][:, :], ALU.mult)
        k.tt("dve", x2[:, :], ci[:, :], A["bim"][:, :], ALU.mult)
        k.tt("dve", Bbr.v(Bbr.t[:, q * 8:(q + 1) * 8, :].rearrange("p b m -> p (b m)")), x1[:, :], x2[:, :], ALU.subtract)
        k.tt("dve", x1[:, :], cr[:, :], A["bim"][:, :], ALU.mult)
        k.tt("dve", x2[:, :], ci[:, :], A["bre"][:, :], ALU.mult)
        k.tt("dve", Bbi.v(Bbi.t[:, q * 8:(q + 1) * 8, :].rearrange("p b m -> p (b m)")), x1[:, :], x2[:, :], ALU.add)
        k.dma("sp", A["bre"][:, :], W["Cblk_re"].v(W["Cblk_re"].t[:, sl]))
        k.dma("sp", A["bim"][:, :], W["Cblk_im"].v(W["Cblk_im"].t[:, sl]))
        k.copy("dve", Cbr.v(Cbr.t[:, q * 8:(q + 1) * 8, :].rearrange("p b m -> p (b m)")), A["bre"][:, :])
        k.ts("dve", Cbi.v(Cbi.t[:, q * 8:(q + 1) * 8, :].rearrange("p b m -> p (b m)")), A["bim"][:, :], -1.0, None, ALU.mult)
    k.stage_end()
    k.stage_begin()
    acr = k.sb("acr", [128, NB], F32)
    aci = k.sb("aci", [128, NB], F32)
    lc = k.sb("lc", [128, NB], F32)
    th = k.sb("th", [128, NB], F32)
    tI_i = k.sb("tIi", [128, TS], mybir.dt.int32)
    tI = k.sb("tI", [128, TS], F32)
    ang = k.sb("ang", [128, NB, TS], F32)
    yy = k.sb("yy", [128, NB * TS], F32)
    yyi = k.sb("yyi", [128, NB * TS], mybir.dt.int32)
    mm_ = k.sb("mm_", [128, NB * TS], F32)
    if PH < 2:
        k.stage_end()
        k.stage_end()
        return
    k.dma("sp", acr[:, :], W["Acol_re"].v(W["Acol_re"].t))
    k.dma("sp", aci[:, :], W["Acol_im"].v(W["Acol_im"].t))
    k.dma("sp", lc[:, :], W["Lcol"].v(W["Lcol"].t))
    k.act(lc[:, :], lc[:, :], AF.Exp)
    k.tt("dve", th[:, :], aci[:, :], lc[:, :], ALU.mult)
    k.tt("dve", acr[:, :], acr[:, :], lc[:, :], ALU.mult)
    k.act(rcol[:, :], acr[:, :], AF.Exp)
    k.op("pool", "iota", tI_i[:, :], [], pattern=[[1, TS]], base=1, channel_multiplier=0)
    k.copy("dve", tI[:, :], tI_i[:, :])
    for bi in range(NB):
        k.ts("dve", ang[:, bi, :], tI[:, :], th[:, bi:bi + 1], None, ALU.mult)
    fl = lambda b: b.v(b.t[:, :, :].rearrange("p b t -> p (b t)"))
    sin_turns(k, fl(sint), fl(ang), 0.0, yy[:, :], yyi[:, :], mm_[:, :])
    sin_turns(k, fl(cost), fl(ang), 0.25, yy[:, :], yyi[:, :], mm_[:, :])
    k.stage_end()
    if PH < 3:
        k.stage_end()
        return

    x32 = [k.sb("x32", [128, T], F32) for _ in range(8)]
    hbf = k.sb("hbf", [128, 8, T], BF16)
    ubf = k.sb("ubf", [128, 8, T], BF16)
    ybf = k.sb("ybf", [128, 8, T], BF16)
    sq = [k.sb("sq", [128, T], F32) for _ in range(2)]
    rstd = k.sb("rstd", [128, T], F32)
    bre = k.sb("bre", [128, T], F32)
    bim = k.sb("bim", [128, T], F32)
    ta = k.sb("ta", [128, T], F32)
    tb = k.sb("tb", [128, T], F32)
    wre = k.sb("wre", [128, T], F32)
    wim = k.sb("wim", [128, T], F32)
    zre = k.sb("zre", [128, T], F32)
    zim = k.sb("zim", [128, T], F32)
    rfull = k.sb("rfull", [128, TS], F32)
    sm1 = k.sb("sm1", [128, 1], F32)
    sm2 = k.sb("sm2", [128, 1], F32)
    inr = k.sb("inr", [128, 1], F32)
    ini = k.sb("ini", [128, 1], F32)
    sbr = [[k.sb("sbr", [128, T], BF16) for _ in range(4)] for _ in range(2)]
    sbi = [[k.sb("sbi", [128, T], BF16) for _ in range(4)] for _ in range(2)]
    pA = [k.ps("pA", [128, 512]) for _ in range(2)]
    pR = [k.ps("pR", [128, 512]) for _ in range(2)]
    pI = [k.ps("pI", [128, 512]) for _ in range(2)]
    pY = k.ps("pY", [128, 512])
    pst = k.ps("pst", [128, 512])
    pai = [0]

    def nextA():
        pai[0] += 1
        return pA[pai[0] % 2]

    TB = True

    def v3(b):
        return b.v(b.t[:, :].rearrange("p (s t) -> p s t", s=T // TS))

    def tb3(tab, bi):
        return tab.v(tab.t[:, bi, :].unsqueeze(1).to_broadcast([128, T // TS, TS]))

    def ttab(e, out, in0, tab, bi, op):
        if TB:
            k.tt(e, v3(out), v3(in0), tb3(tab, bi), op)
        else:
            for s_ in range(T // TS):
                sl = slice(s_ * TS, (s_ + 1) * TS)
                k.tt(e, out[:, sl], in0[:, sl], tab[:, bi, :], op)

    nblk = 0
    for it in range(L // T):
        t0 = it * T
        load_x_tile(c, "sp", x32, xin, t0, T)
        rmsnorm_mod(c, x32, T, layer, 0, sq, rstd, pst, out_bf=hbf, off=0, cp_eng="dve")
        for ch in range(8):
            p = nextA()
            k.mm(p, [(p[:, :], w_in[:, kc, ch * 128:(ch + 1) * 128], hbf[:, kc, :], kc == 0, kc == 7)
                     for kc in range(8)])
            k.copy("act", x32[ch][:, :], p[:, :])
            k.copy("dve", ubf[:, ch, :], x32[ch][:, :])
        for j in range(8):
            par = j % 2
            for m in range(4):
                bi = j * 4 + m
                pr, pi_ = pR[nblk % 2], pI[nblk % 2]
                nblk += 1
                k.mm(pr, [(pr[:, :], Bbr[:, bi, :], ubf[:, j, :], True, True)])
                k.mm(pi_, [(pi_[:, :], Bbi[:, bi, :], ubf[:, j, :], True, True)])
                k.copy("act", bre[:, :], pr[:, :])
                k.copy("act", bim[:, :], pi_[:, :])
                ttab("dve", ta, bre, cost, bi, ALU.mult)
                ttab(PE_, tb, bim, sint, bi, ALU.mult)
                k.tt("dve", wre[:, :], ta[:, :], tb[:, :], ALU.add)
                ttab(PE_, ta, bim, cost, bi, ALU.mult)
                ttab("dve", tb, bre, sint, bi, ALU.mult)
                k.tt(PE_, wim[:, :], ta[:, :], tb[:, :], ALU.subtract)
                if False:
                    rb = rcol.v(rcol.t[:, bi:bi + 1].to_broadcast([128, TS]))
                else:
                    k.ts("dve", rfull[:, :], c.ones[:, :TS], rcol[:, bi:bi + 1], None, ALU.mult)
                    rb = rfull[:, :]
                for s_ in range(T // TS):
                    sl = slice(s_ * TS, (s_ + 1) * TS)
                    i_r = spr[:, bi:bi + 1] if s_ == 0 else inr[:, :]
                    i_i = spi[:, bi:bi + 1] if s_ == 0 else ini[:, :]
                    k.op("dve", "tensor_tensor_scan", zre[:, sl], [rb, wre[:, sl], i_r, ALU.mult, ALU.add])
                    k.op("dve", "tensor_tensor_scan", zim[:, sl], [rb, wim[:, sl], i_i, ALU.mult, ALU.add])
                    last = s_ == T // TS - 1
                    o_r = spr[:, bi:bi + 1] if last else inr[:, :]
                    o_i = spi[:, bi:bi + 1] if last else ini[:, :]
                    e_ = (s_ + 1) * TS - 1
                    cT, sT = cost[:, bi, TS - 1:TS], sint[:, bi, TS - 1:TS]
                    k.ts("dve", sm1[:, :], zim[:, e_:e_ + 1], sT, None, ALU.mult)
                    k.ts("dve", sm2[:, :], zim[:, e_:e_ + 1], cT, None, ALU.mult)
                    k.stt(o_r, zre[:, e_:e_ + 1], cT, sm1[:, :], ALU.mult, ALU.subtract)
                    k.stt(o_i, zre[:, e_:e_ + 1], sT, sm2[:, :], ALU.mult, ALU.add)
                ttab("dve", ta, zre, cost, bi, ALU.mult)
                ttab(PE_, tb, zim, sint, bi, ALU.mult)
                k.tt("dve", sbr[par][m][:, :], ta[:, :], tb[:, :], ALU.subtract)
                ttab(PE_, ta, zre, sint, bi, ALU.mult)
                ttab("dve", tb, zim, cost, bi, ALU.mult)
                k.tt(PE_, sbi[par][m][:, :], ta[:, :], tb[:, :], ALU.add)
            mms = []
            for m in range(4):
                bi = j * 4 + m
                mms.append((pY[:, :], Cbr[:, bi, :], sbr[par][m][:, :], m == 0, False))
                mms.append((pY[:, :], Cbi[:, bi, :], sbi[par][m][:, :], False, m == 3))
            k.mm(pY, mms)
            k.stt(ta[:, :], x32[j][:, :], dl[:, j:j + 1], pY[:, :], ALU.mult, ALU.add)
            k.act(ybf[:, j, :], ta[:, :], AF.Gelu)
        load_x_tile(c, "sp", x32, xin, t0, T)
        for n in range(8):
            pa, pb = pR[n % 2], pI[n % 2]
            k.mm(pa, [(pa[:, :], w_out[:, kc, n * 128:(n + 1) * 128], ybf[:, kc, :], kc == 0, kc == 7)
                      for kc in range(8)])
            k.mm(pb, [(pb[:, :], w_out[:, kc, 1024 + n * 128:1024 + (n + 1) * 128], ybf[:, kc, :], kc == 0, kc == 7)
                      for kc in range(8)])
            k.act(tb[:, :], pb[:, :], AF.Sigmoid)
            k.tt("dve", ta[:, :], pa[:, :], tb[:, :], ALU.mult)
            k.stt(x32[n][:, :], ta[:, :], c.mod[:, layer, 16 + n:17 + n], x32[n][:, :], ALU.mult, ALU.add)
        store_x_tile(c, "sp", x32, xout, t0, T)
    k.stage_end()


def s5_host_layout(a_re, a_im, log_dt, b_re, b_im, c_re, c_im, d):
    NB = 32
    out = {}
    Ablk_re = np.zeros((128, NB, 128), np.float32); Ablk_im = np.zeros_like(Ablk_re); Lblk = np.zeros_like(Ablk_re)
    Bblk_re = np.zeros_like(Ablk_re); Bblk_im = np.zeros_like(Ablk_re)
    Cblk_re = np.zeros_like(Ablk_re); Cblk_im = np.zeros_like(Ablk_re)
    Acol_re = np.zeros((128, NB), np.float32); Acol_im = np.zeros_like(Acol_re); Lcol = np.zeros_like(Acol_re)
    for j in range(8):
        for m in range(4):
            bi = j * 4 + m
            for gg in range(2):
                g = 8 * j + 2 * m + gg
                gl = 2 * m + gg
                cs = slice(gg * 64, (gg + 1) * 64)
                Ablk_re[:, bi, cs] = a_re[g][None, :]
                Ablk_im[:, bi, cs] = a_im[g][None, :]
                Lblk[:, bi, cs] = log_dt[g]
                Bblk_re[gl * 16:(gl + 1) * 16, bi, cs] = b_re[g].T
                Bblk_im[gl * 16:(gl + 1) * 16, bi, cs] = b_im[g].T
                Cblk_re[cs, bi, gl * 16:(gl + 1) * 16] = c_re[g].T
                Cblk_im[cs, bi, gl * 16:(gl + 1) * 16] = c_im[g].T
                Acol_re[cs, bi] = a_re[g]
                Acol_im[cs, bi] = a_im[g]
                Lcol[cs, bi] = log_dt[g]
    f = lambda a: np.ascontiguousarray(a.reshape(128, NB * 128))
    return dict(Ablk_re=f(Ablk_re), Ablk_im=f(Ablk_im), Lblk=f(Lblk), Bblk_re=f(Bblk_re), Bblk_im=f(Bblk_im),
                Cblk_re=f(Cblk_re), Cblk_im=f(Cblk_im), Acol_re=Acol_re, Acol_im=Acol_im, Lcol=Lcol,
                d_l=np.ascontiguousarray(d.reshape(8, 128).T))


def mlstm_stage(c, layer, xin, xout, W):
    k = c.k
    L = c.L
    T = 256
    NC_ = T // 64
    NS = T // 128
    DH = 512
    k.stage_begin()
    wbuf = [k.sb("wbuf", [128, 8, 1024], BF16) for _ in range(3)]
    bdq = k.sb("bdq", [128, 16, 128], BF16)
    bdk = k.sb("bdk", [128, 16, 128], BF16)
    bdv = k.sb("bdv", [128, 16, 128], BF16)
    weffc = k.sb("weffc", [128, 16, 8], BF16)
    weffm = k.sb("weffm", [128, 16, 8], BF16)
    convl = k.sb("convl", [128, 16, 5], F32)
    bif = k.sb("bif", [1, 8], F32)
    skipl = k.sb("skipl", [128, 16], F32)
    gnl = k.sb("gnl", [128, 16], F32)
    cmat = [[k.sb("cmat", [128, 512], F32) for _ in range(4)] for _ in range(4)]
    cb = [[k.sb("cb", [128, 512], BF16) for _ in range(4)] for _ in range(4)]
    nvec = [k.sb("nvec", [128, 4], F32) for _ in range(4)]
    nvb = [k.sb("nvb", [128, 4], BF16) for _ in range(4)]
    halo = k.sb("halo", [128, 16, 3], F32)
    mcar = k.sb("mcar", [4, 1], F32)
    cmask = k.sb("cmask", [4, T], F32)
    sel = k.sb("sel", [4, 4, 128], F32)
    onesb = k.sb("onesb", [128, 1], BF16)
    for n_, src in (("bdq", bdq), ("bdk", bdk), ("bdv", bdv)):
        k.dma("pool", src[:, :, :], W[n_].v(W[n_].t))
    k.dma("sp", convl[:, :, :], W["conv_l"].v(W["conv_l"].t))
    k.dma("sp", bif[:, :], W["b_if"].v(W["b_if"].t))
    k.dma("sp", skipl[:, :], W["skip_l"].v(W["skip_l"].t))
    k.dma("sp", gnl[:, :], W["gnorm_l"].v(W["gnorm_l"].t))
    for h in range(4):
        for kc in range(4):
            k.memset("dve", cmat[h][kc][:, :], 0.0)
        k.memset("dve", nvec[h][:, :], 0.0)
        k.ts("dve", sel[:, h, :], c.ones[0:4, 0:128], c.ident[0:4, h:h + 1], None, ALU.mult)
    k.memset("dve", halo[:, :, :], 0.0)
    k.memset("dve", mcar[:, :], 0.0)
    k.memset("dve", cmask[:, :], 1.0)
    k.memset("dve", cmask.v(cmask.t[:, :].rearrange("p (c t) -> p c t", t=64)[:, :, 0:1]), 0.0)
    k.memset("dve", onesb[:, :], 1.0)
    k.stage_begin()
    bT = {n_: k.sb(n_, [128, 16, 128], F32) for n_ in ("bdqT", "bdkT", "bdvT")}
    wif = k.sb("wif", [128, 48, 8], F32)
    pw = k.ps("pw", [128, 512])
    for n_ in bT:
        k.dma("sp", bT[n_][:, :, :], W[n_].v(W[n_].t))
    k.dma("sp", wif[:, :, :], W["wif_l"].v(W["wif_l"].t))
    for ch in range(16):
        k.mm(pw, [(pw[:, ch * 8:ch * 8 + 8], bT["bdqT"][:, ch, :], wif[:, ch, :], True, False),
                  (pw[:, ch * 8:ch * 8 + 8], bT["bdkT"][:, ch, :], wif[:, 16 + ch, :], False, True)])
    k.copy("dve", weffc.v(weffc.t[:, :, :].rearrange("p c g -> p (c g)")), pw[:, 0:128])
    for ch in range(16):
        k.mm(pw, [(pw[:, 128 + ch * 8:128 + ch * 8 + 8], bT["bdvT"][:, ch, :], wif[:, 32 + ch, :], True, True)])
    k.copy("dve", weffm.v(weffm.t[:, :, :].rearrange("p c g -> p (c g)")), pw[:, 128:256])
    k.stage_end()

    x32 = [k.sb("x32", [128, T], F32) for _ in range(8)]
    hbf = k.sb("hbf", [128, 8, T], BF16)
    sq = [k.sb("sq", [128, T], F32) for _ in range(2)]
    rstd = k.sb("rstd", [128, T], F32)
    xmb = k.sb("xmb", [128, 16, T], BF16)
    xcb = k.sb("xcb", [128, 16, T], BF16)
    zsb = k.sb("zsb", [128, 16, T], BF16)
    qTb = k.sb("qTb", [128, 16, T], BF16)
    kw = [k.sb("kw", [128, 2048], BF16) for _ in range(NS)]
    vt = [k.sb("vt", [128, 2048], BF16) for _ in range(NS)]
    xt = [k.sb("xt", [128, T + 3], F32) for _ in range(2)]
    ca = [k.sb("ca", [128, T], F32) for _ in range(2)]
    gi = k.sb("gi", [4, T], F32)
    nl = k.sb("nl", [4, T], F32)
    cum = k.sb("cum", [4, T], F32)
    lw = k.sb("lw", [4, T], F32)
    wT = k.sb("wT", [4, T], F32)
    enx = k.sb("enx", [4, T], F32)
    g4 = {n_: k.sb(n_, [4, NC_], F32) for n_ in ("ntot", "mx", "mnew", "mp", "dec", "negm", "en")}
    wtm = [k.sb("wtm", [128, 4], F32) for _ in range(NS)]
    entm = [k.sb("entm", [64, 4], F32) for _ in range(NC_)]
    decb = k.sb("decb", [128, 4, NC_], F32)
    hc = [k.sb("hc", [64, 512], F32) for _ in range(2)]
    hnb = [k.sb("hnb", [64, 512], BF16) for _ in range(2)]
    st6 = k.sb("st6", [64, 6], F32)
    mv = k.sb("mv", [64, 2], F32)
    den = k.sb("den", [64, 1], F32)
    tmp64 = [k.sb("tmp64", [128, 64], F32) for _ in range(2)]
    pA = [k.ps("pA", [128, 512]) for _ in range(2)]
    pU = [k.ps("pU", [128, 512]) for _ in range(2)]
    pN = k.ps("pN", [128, 512])
    pM = k.ps("pM", [128, 512])
    pT = k.ps("pT", [128, 512], BF16)
    pst = k.ps("pst", [128, 512])
    pai = [0]

    def nextA():
        pai[0] += 1
        return pA[pai[0] % 2]

    wq = [0]

    def load_piece(src, rows=None, cols=None):
        b = wbuf[wq[0] % 3]
        wq[0] += 1
        t = W[src].t
        if cols is not None:
            ap = t[:, cols[0]:cols[1]].rearrange("(kc p) n -> p kc n", p=128)
        else:
            ap = t[rows[0]:rows[1], :].rearrange("(kc p) n -> p kc n", p=128)
        k.dma("pool", b[:, :, :], W[src].v(ap))
        return b

    for it in range(L // T):
        t0 = it * T
        load_x_tile(c, "sp", x32, xin, t0, T)
        rmsnorm_mod(c, x32, T, layer, 0, sq, rstd, pst, out_bf=hbf, off=0, cp_eng="dve")
        load_x_tile(c, "sp", x32, xin, t0, T)
        for pc in range(4):
            wb = load_piece("w_up", cols=(pc * 1024, (pc + 1) * 1024))
            for cc in range(8):
                ch = (pc % 2) * 8 + cc
                p = nextA()
                k.mm(p, [(p[:, :T], wb[:, kc, cc * 128:(cc + 1) * 128], hbf[:, kc, :], kc == 0, kc == 7)
                         for kc in range(8)])
                if pc < 2:
                    x_ = xt[ch % 2]
                    k.copy("dve", x_[:, 0:3], halo[:, ch, :])
                    k.copy("act", x_[:, 3:T + 3], p[:, :T])
                    k.copy("dve", halo[:, ch, :], x_[:, T:T + 3])
                    k.copy("dve", xmb[:, ch, :], x_[:, 3:T + 3])
                    a_ = ca[ch % 2]
                    k.ts("dve", a_[:, :], x_[:, 3:T + 3], convl[:, ch, 3:4], convl[:, ch, 4:5], ALU.mult, ALU.add)
                    for j in range(3):
                        k.stt(a_[:, :], x_[:, j:j + T], convl[:, ch, j:j + 1], a_[:, :], ALU.mult, ALU.add)
                    k.act(xcb[:, ch, :], a_[:, :], AF.Silu)
                else:
                    k.act(zsb[:, ch, :], p[:, :T], AF.Silu)
        for gsel, dst in ((0, gi), (1, nl)):
            p = pM
            mms = []
            for ch in range(16):
                mms.append((p[:4, :T], weffc[:, ch, gsel * 4:gsel * 4 + 4], xcb[:, ch, :], ch == 0, False))
                mms.append((p[:4, :T], weffm[:, ch, gsel * 4:gsel * 4 + 4], xmb[:, ch, :], False, False))
            mms.append((p[:4, :T], bif[0:1, gsel * 4:gsel * 4 + 4], c.ones[0:1, :T], False, True))
            k.mm(p, mms)
            if gsel == 0:
                k.copy("dve", gi[:, :], p[:4, :T])
            else:
                k.act(nl[:, :], p[:4, :T], AF.Exp, scale=-1.0)
                k.act(nl[:, :], nl[:, :], AF.Ln, bias=1.0)
        k.op("dve", "tensor_tensor_scan", cum[:, :], [cmask[:, :], nl[:, :], 0.0, ALU.mult, ALU.add])
        cum3 = cum.v(cum.t[:, :].rearrange("p (c t) -> p c t", t=64))
        k.ts("dve", g4["ntot"][:, :], cum.v(cum.t[:, :].rearrange("p (c t) -> p c t", t=64)[:, :, 63]), -1.0, None, ALU.mult)
        for cc in range(NC_):
            sl = slice(cc * 64, (cc + 1) * 64)
            k.stt(lw[:, sl], cum[:, sl], g4["ntot"][:, cc:cc + 1], gi[:, sl], ALU.add, ALU.add)
        k.op("dve", "tensor_reduce", g4["mx"][:, :], [lw.v(lw.t[:, :].rearrange("p (c t) -> p c t", t=64))],
             axis=AX.X, op=ALU.max)
        k.op("dve", "tensor_tensor_scan", g4["mnew"][:, :], [g4["ntot"][:, :], g4["mx"][:, :], mcar[:, 0:1], ALU.add, ALU.max])
        k.copy("dve", g4["mp"][:, 0:1], mcar[:, :])
        k.copy("dve", g4["mp"][:, 1:NC_], g4["mnew"][:, 0:NC_ - 1])
        k.copy("dve", mcar[:, :], g4["mnew"][:, NC_ - 1:NC_])
        k.tt("dve", g4["dec"][:, :], g4["ntot"][:, :], g4["mp"][:, :], ALU.add)
        k.tt("dve", g4["dec"][:, :], g4["dec"][:, :], g4["mnew"][:, :], ALU.subtract)
        k.act(g4["dec"][:, :], g4["dec"][:, :], AF.Exp)
        k.ts("dve", g4["negm"][:, :], g4["mnew"][:, :], -1.0, None, ALU.mult)
        k.act(g4["en"][:, :], g4["negm"][:, :], AF.Exp)
        for cc in range(NC_):
            sl = slice(cc * 64, (cc + 1) * 64)
            k.act(wT[:, sl], lw[:, sl], AF.Exp, bias=g4["negm"][:, cc:cc + 1], scale=1.0)
            k.ts("dve", enx[:, sl], c.ones[0:4, 0:64], g4["en"][:, cc:cc + 1], None, ALU.mult)
        for s_ in range(NS):
            k.mm(pM, [(pM[:, 32 + 4 * s_:36 + 4 * s_], wT[:, s_ * 128:(s_ + 1) * 128], c.ident[0:4, 0:4], True, True)],
                 transpose=True)
            k.ts("dve", wtm[s_][:, :], pM[:, 32 + 4 * s_:36 + 4 * s_], DH ** -0.5, None, ALU.mult)
        for cc in range(NC_):
            k.mm(pM, [(pM[:64, 64 + 4 * cc:68 + 4 * cc], enx[:, cc * 64:(cc + 1) * 64], c.ident[0:4, 0:4], True, True)],
                 transpose=True)
            k.copy("dve", entm[cc][:, :], pM[:64, 64 + 4 * cc:68 + 4 * cc])
        for h in range(4):
            k.mm(pM, [(pM[:, 128 + h * NC_:128 + (h + 1) * NC_], sel[:, h, :], g4["dec"][:, :], True, True)])
        k.copy("dve", decb.v(decb.t[:, :, :].rearrange("p h c -> p (h c)")), pM[:, 128:128 + 4 * NC_])
        for ch in range(16):
            p = nextA()
            k.mm(p, [(p[:, :T], bdq[:, ch, :], xcb[:, ch, :], True, True)])
            k.copy("act", qTb[:, ch, :], p[:, :T])
        for s_ in range(NS):
            for h in range(4):
                p = nextA()
                k.mm(p, [(p[:, kc * 128:(kc + 1) * 128], xcb[:, h * 4 + kc, s_ * 128:(s_ + 1) * 128], bdk[:, h * 4 + kc, :],
                          True, True) for kc in range(4)])
                k.ts("dve", kw[s_][:, h * 512:(h + 1) * 512], p[:, :], wtm[s_][:, h:h + 1], None, ALU.mult)
                p = nextA()
                k.mm(p, [(p[:, kc * 128:(kc + 1) * 128], xmb[:, h * 4 + kc, s_ * 128:(s_ + 1) * 128], bdv[:, h * 4 + kc, :],
                          True, True) for kc in range(4)])
                k.copy("act", vt[s_][:, h * 512:(h + 1) * 512], p[:, :])
        for ch in range(16):
            k.ts("dve", xcb[:, ch, :], xcb[:, ch, :], skipl[:, ch:ch + 1], None, ALU.mult)
        nu = 0
        for cc in range(NC_):
            s_, hf = cc // 2, cc % 2
            r0 = 64 * hf
            cs_ = slice(cc * 64, (cc + 1) * 64)
            for h in range(4):
                for kc in range(4):
                    pu_ = pU[nu % 2]
                    nu += 1
                    k.mm(pu_, [(pu_[:, :], kw[s_][r0:r0 + 64, h * 512 + kc * 128:h * 512 + (kc + 1) * 128],
                                vt[s_][r0:r0 + 64, h * 512:(h + 1) * 512], True, True)])
                    k.stt(cmat[h][kc][:, :], cmat[h][kc][:, :], decb[:, h, cc:cc + 1], pu_[:, :], ALU.mult, ALU.add)
                    k.copy("act", cb[h][kc][:, :], cmat[h][kc][:, :])
                k.mm(pM, [(pM[:, kc:kc + 1], kw[s_][r0:r0 + 64, h * 512 + kc * 128:h * 512 + (kc + 1) * 128],
                           onesb[r0:r0 + 64, 0:1], True, True) for kc in range(4)])
                k.stt(nvec[h][:, :], nvec[h][:, :], decb[:, h, cc:cc + 1], pM[:, 0:4], ALU.mult, ALU.add)
                k.copy("dve", nvb[h][:, :], nvec[h][:, :])
                k.mm(pN, [(pN[:64, :], qTb[:, h * 4 + kc, cs_], cb[h][kc][:, :], kc == 0, kc == 3) for kc in range(4)])
                k.mm(pM, [(pM[:64, 8:9], qTb[:, h * 4 + kc, cs_], nvb[h][:, kc:kc + 1], kc == 0, kc == 3)
                          for kc in range(4)])
                k.copy("dve", den[:, :], pM[:64, 8:9])
                k.stt(den[:, :], den[:, :], -1.0, den[:, :], ALU.mult, ALU.max)
                k.ts("dve", den[:, :], den[:, :], entm[cc][:, h:h + 1], None, ALU.max)
                k.op("dve", "reciprocal", den[:, :], [den[:, :]])
                hc_ = hc[h % 2]
                k.act(hc_[:, :], pN[:64, :], AF.Copy, scale=den[:, 0:1])
                k.op("dve", "bn_stats", st6[:, :], [hc_[:, :]])
                k.op("dve", "bn_aggr", mv[:, :], [st6[:, :]])
                k.ts("dve", mv[:, 1:2], mv[:, 1:2], EPS, None, ALU.add)
                k.act(mv[:, 1:2], mv[:, 1:2], AF.Sqrt)
                k.op("dve", "reciprocal", mv[:, 1:2], [mv[:, 1:2]])
                hb_ = hnb[h % 2]
                k.ts("dve", hb_[:, :], hc_[:, :], mv[:, 0:1], mv[:, 1:2], ALU.subtract, ALU.mult)
                k.mm(pT, [(pT[:, kc * 64:(kc + 1) * 64], hb_[:, kc * 128:(kc + 1) * 128], c.identb[0:64, 0:64], True, True)
                          for kc in range(4)], transpose=True)
                for kc in range(4):
                    ch = h * 4 + kc
                    t_ = tmp64[kc % 2]
                    k.stt(t_[:, :], pT[:, kc * 64:(kc + 1) * 64], gnl[:, ch:ch + 1], xcb[:, ch, cs_], ALU.mult, ALU.add)
                    k.tt("dve", zsb[:, ch, cs_], t_[:, :], zsb[:, ch, cs_], ALU.mult)
        wd0 = load_piece("w_down", rows=(0, 1024))
        wd1 = load_piece("w_down", rows=(1024, 2048))
        for n in range(8):
            p = nextA()
            mms = [(p[:, :T], wd0[:, kc, n * 128:(n + 1) * 128], zsb[:, kc, :], kc == 0, False) for kc in range(8)]
            mms += [(p[:, :T], wd1[:, kc, n * 128:(n + 1) * 128], zsb[:, 8 + kc, :], False, kc == 7) for kc in range(8)]
            k.mm(p, mms)
            k.stt(x32[n][:, :], p[:, :T], c.mod[:, layer, 16 + n:17 + n], x32[n][:, :], ALU.mult, ALU.add)
        store_x_tile(c, "sp", x32, xout, t0, T)
    k.stage_end()


def mlstm_host_layout(conv_w, conv_b, w_q, w_k, w_v, w_if, skip, g_norm):
    def bd(w, transpose):
        o = np.zeros((128, 16, 128), np.float32)
        for ch in range(16):
            for b in range(32):
                blk = w[ch * 32 + b]
                sl = slice(b * 4, b * 4 + 4)
                o[sl, ch, sl] = blk.T if transpose else blk
        return o
    conv_l = np.zeros((128, 16, 5), np.float32)
    conv_l[:, :, 0:4] = conv_w.reshape(4, 16, 128).transpose(2, 1, 0)
    conv_l[:, :, 4] = conv_b.reshape(16, 128).T
    return dict(bdq=bd(w_q, False), bdk=bd(w_k, False), bdv=bd(w_v, False),
                bdqT=bd(w_q, True), bdkT=bd(w_k, True), bdvT=bd(w_v, True),
                conv_l=conv_l,
                wif_l=np.ascontiguousarray(w_if.reshape(48, 128, 8).transpose(1, 0, 2)),
                skip_l=np.ascontiguousarray(skip.reshape(16, 128).T),
                gnorm_l=np.ascontiguousarray(g_norm.reshape(16, 128).T))


I32 = mybir.dt.int32


def moe_sparse_stage(c, layer, xin, xout, W, S, ne=NE, eoff=0):
    k = c.k
    L = c.L
    NBLK = L // 128
    NG = (4 * L + ne * 511) // 512
    NSLOT = NG * 512
    k.stage_begin()
    slots_all = k.sb("slots_all", [128, NBLK, 4], I32)
    egrp = k.sb("egrp", [128, NG], F32)
    eg256 = k.sb("eg256", [128, NG], F32)
    eg128 = k.sb("eg128", [128, NG], F32)
    iop = k.sb("iop", [128, 1], F32)
    iorow = k.sb("iorow", [128, 8], F32)
    ioi = k.sb("ioi", [128, 8], I32)
    k.op("pool", "iota", ioi[:, :], [], pattern=[[128, 8]], base=0, channel_multiplier=1)
    k.copy("dve", iorow[:, :], ioi[:, :])
    k.copy("dve", iop[:, :], ioi[:, 0:1])

    k.stage_begin()
    z = k.sb("z", [128, NSLOT * 2 // 128], I32)
    k.memset("dve", z[:, :], 0)
    k.dma("sp", S["rec"].v(S["rec"].t.rearrange("(p r) c -> p (r c)", p=128)), z[:, :])
    k.stage_end()

    k.stage_begin()
    x32 = [k.sb("x32", [128, 512], F32) for _ in range(8)]
    hbf = k.sb("hbf", [128, 8, 512], BF16)
    sq = [k.sb("sq", [128, 512], F32) for _ in range(2)]
    rstd = k.sb("rstd", [128, 512], F32)
    htm = [k.sb("htm", [128, 1024], BF16) for _ in range(2)]
    wr = k.sb("wr", [128, 8, 32], F32)
    br = k.sb("br", [1, 32], F32)
    lg_all = k.sb("lg_all", [128, NBLK, 32], F32)
    comb_all = k.sb("comb_all", [128, NBLK, 32], F32)
    pos_all = k.sb("pos_all", [128, NBLK, 32], F32)
    m4_all = k.sb("m4_all", [128, NBLK, 4], F32)
    runc = k.sb("runc", [128, 32], F32)
    lmat = k.sb("lmat", [128, 128], F32)
    m8 = k.sb("m8", [128, 8], F32)
    negm = k.sb("negm", [128, 1], F32)
    mask = k.sb("mask", [128, 32], F32)
    ex = k.sb("ex", [128, 32], F32)
    ssum = k.sb("ssum", [128, 1], F32)
    pm = [k.ps("pm", [128, 512]) for _ in range(2)]
    pp = k.ps("pp", [128, 512])
    ptb = [k.ps("ptb", [128, 1024], BF16) for _ in range(2)]
    k.dma("sp", wr[:, :, :], W["w_router"].v(W["w_router"].t.rearrange("(kc p) e -> p kc e", p=128)))
    k.dma("sp", br[:, :], W["b_router"].v(W["b_router"].t))
    k.memset("dve", runc[:, :], 0.0)
    k.op("pool", "affine_select", lmat[:, :], [c.ones[:, 0:128]], pattern=[[1, 128]],
         compare_op=ALU.is_gt, fill=0.0, base=0, channel_multiplier=-1)
    for it in range(L // 512):
        t0 = it * 512
        load_x_tile(c, "sp", x32, xin, t0, 512)
        rmsnorm_mod(c, x32, 512, layer, 1, sq, rstd, pm[0], out_bf=hbf, off=0, cp_eng="dve")
        for j in range(4):
            blk = it * 4 + j
            p = pm[1]
            mms = [(p[:, :32], x32[ch][:, j * 128:(j + 1) * 128], wr[:, ch, :], ch == 0, False) for ch in range(8)]
            mms.append((p[:, :32], c.ones[0:1, 0:128], br[0:1, :], False, True))
            k.mm(p, mms)
            lg = lg_all[:, blk, :]
            k.copy("dve", lg, p[:, :32])
            k.op("dve", "max", m8[:, :], [lg])
            k.copy("dve", m4_all[:, blk, :], m8[:, 0:4])
            k.ts("dve", mask[:, :], lg, m8[:, 3:4], None, ALU.is_ge)
            k.ts("dve", negm[:, :], m8[:, 0:1], -1.0, None, ALU.mult)
            k.act(ex[:, :], lg, AF.Exp, bias=negm[:, 0:1], scale=1.0)
            k.stt(ex[:, :], ex[:, :], 1.0, mask[:, :], ALU.mult, ALU.mult, accum_out=ssum[:, :])
            k.op("dve", "reciprocal", ssum[:, :], [ssum[:, :]])
            k.ts("dve", comb_all[:, blk, :], ex[:, :], ssum[:, 0:1], None, ALU.mult)
            k.mm(pp, [(pp[:, 0:32], lmat[:, :], mask[:, :], True, True),
                      (pp[:, 32:64], c.ones[:, 0:128], mask[:, :], True, True)])
            k.tt("dve", pos_all[:, blk, :], pp[:, 0:32], runc[:, :], ALU.add)
            k.tt("dve", runc[:, :], runc[:, :], pp[:, 32:64], ALU.add)
            pt_ = ptb[blk % 2]
            k.mm(pt_, [(pt_[:, ch * 128:(ch + 1) * 128], hbf[:, ch, j * 128:(j + 1) * 128], c.identb[:, :], True, True)
                       for ch in range(8)], transpose=True)
            h_ = htm[blk % 2]
            k.copy("act", h_[:, :], pt_[:, :])
            reg = k.region()
            k.dma("sp", reg.v(S["h_tm"].t[blk * 128:(blk + 1) * 128, :]), h_[:, :])
    gsz = k.sb("gsz", [128, 32], F32)
    gszi = k.sb("gszi", [128, 32], I32)
    gtmp = k.sb("gtmp", [128, 32], F32)
    ginc = k.sb("ginc", [128, 32], F32)
    base = k.sb("base", [128, 32], F32)
    gidx = k.sb("gidx", [128, NG], F32)
    gidi = k.sb("gidi", [128, NG], I32)
    gcmp = k.sb("gcmp", [128, NG], F32)
    k.ts("dve", gtmp[:, :], runc[:, :], 511.0, 1.0 / 512.0, ALU.add, ALU.mult)
    k.copy("dve", gszi[:, :], gtmp[:, :])
    k.copy("dve", gsz[:, :], gszi[:, :])
    k.tt("dve", mask[:, :], gsz[:, :], gtmp[:, :], ALU.is_gt)
    k.tt("dve", gsz[:, :], gsz[:, :], mask[:, :], ALU.subtract)
    k.op("dve", "tensor_tensor_scan", ginc[:, :], [c.ones[:, 0:32], gsz[:, :], 0.0, ALU.mult, ALU.add])
    k.tt("dve", base[:, :], ginc[:, :], gsz[:, :], ALU.subtract)
    k.ts("dve", base[:, :], base[:, :], 512.0, None, ALU.mult)
    k.op("pool", "iota", gidi[:, :], [], pattern=[[1, NG]], base=0, channel_multiplier=0)
    k.copy("dve", gidx[:, :], gidi[:, :])
    k.memset("dve", egrp[:, :], 0.0)
    for e in range(ne):
        k.ts("dve", gcmp[:, :], gidx[:, :], ginc[:, e:e + 1], None, ALU.is_ge)
        k.tt("dve", egrp[:, :], egrp[:, :], gcmp[:, :], ALU.add)
    k.ts("dve", egrp[:, :], egrp[:, :], float(ne - 1), None, ALU.min)
    k.ts("dve", egrp[:, :], egrp[:, :], float(eoff), None, ALU.add)
    k.ts("dve", eg256[:, :], egrp[:, :], 256.0, None, ALU.mult)
    k.ts("dve", eg128[:, :], egrp[:, :], 128.0, None, ALU.mult)
    slot4 = k.sb("slot4", [128, 4, 32], F32)
    oh4 = k.sb("oh4", [128, 4, 32], F32)
    pr4 = k.sb("pr4", [128, 4, 32], F32)
    sj16 = k.sb("sj16", [128, 4, 4], F32)
    wj16 = k.sb("wj16", [128, 4, 4], F32)
    tok4 = k.sb("tok4", [128, 4], F32)
    rec16 = [k.sb("rec16", [128, 16, 2], I32) for _ in range(2)]
    sji16 = [k.sb("sji16", [128, 16], I32) for _ in range(2)]
    for it in range(NBLK // 4):
        b0 = it * 4
        bs = slice(b0, b0 + 4)
        r16, s16 = rec16[it % 2], sji16[it % 2]
        k.tt("dve", slot4[:, :, :], pos_all[:, bs, :], base.v(base.t[:, :].unsqueeze(1).to_broadcast([128, 4, 32])), ALU.add)
        for j in range(4):
            k.ts("dve", tok4[:, j:j + 1], iop[:, :], float((b0 + j) * 128), None, ALU.add)
        for j in range(4):
            mb = m4_all.v(m4_all.t[:, bs, j:j + 1].to_broadcast([128, 4, 32]))
            k.tt("dve", oh4[:, :, :], lg_all[:, bs, :], mb, ALU.is_equal)
            k.tt("dve", pr4[:, :, :], oh4[:, :, :], slot4[:, :, :], ALU.mult)
            k.op("dve", "tensor_reduce", sj16[:, :, j], [pr4[:, :, :]], axis=AX.X, op=ALU.add)
            k.tt("dve", pr4[:, :, :], oh4[:, :, :], comb_all[:, bs, :], ALU.mult)
            k.op("dve", "tensor_reduce", wj16[:, :, j], [pr4[:, :, :]], axis=AX.X, op=ALU.add)
        r3 = r16.v(r16.t[:, :, 0].rearrange("p (b j) -> p b j", j=4))
        k.copy("dve", r3, tok4.v(tok4.t[:, :].unsqueeze(2).to_broadcast([128, 4, 4])))
        k.copy("dve", r16.v(r16.t[:, :, 1].bitcast(F32)), wj16.v(wj16.t[:, :, :].rearrange("p b j -> p (b j)")))
        k.copy("dve", s16[:, :], sj16.v(sj16.t[:, :, :].rearrange("p b j -> p (b j)")))
        k.copy("dve", slots_all.v(slots_all.t[:, bs, :].rearrange("p b j -> p (b j)")), sj16.v(sj16.t[:, :, :].rearrange("p b j -> p (b j)")))
        for i in range(16):
            reg = k.region()
            k.idma(reg.v(S["rec"].t), r16[:, i, :], s16[:, i:i + 1], scatter=True)
    k.stage_end()

    k.stage_begin()
    wgu = [[k.sb("wgu", [128, 4, 2048], BF16) for _ in range(2)] for _ in range(2)]
    wdn = [k.sb("wdn", [128, 8, 1024], BF16) for _ in range(2)]
    widx = [k.sb("widx", [128, 2], I32) for _ in range(2)]
    bidx = [k.sb("bidx", [128, 2], I32) for _ in range(2)]
    wtmp = k.sb("wtmp", [128, 8], F32)
    wtmp2 = k.sb("wtmp2", [128, 2], F32)
    bgu = [k.sb("bgu", [128, 16], F32) for _ in range(2)]
    bdn = [k.sb("bdn", [128, 1024], F32) for _ in range(2)]
    bdnb = [k.sb("bdnb", [1, 1024], BF16) for _ in range(2)]
    onesb = k.sb("onesb", [1, 128], BF16)
    k.memset("dve", onesb[:, :], 1.0)
    recg = [[k.sb("recg", [128, 2], I32) for _ in range(4)] for _ in range(2)]
    wsc = [k.sb("wsc", [128, 1], F32) for _ in range(4)]
    hg = [[k.sb("hg", [128, 1024], BF16) for _ in range(4)] for _ in range(2)]
    hT = [k.sb("hT", [128, 8, 512], BF16) for _ in range(2)]
    actT = [k.sb("actT", [128, 8, 512], BF16) for _ in range(2)]
    tg = [k.sb("tg", [128, 512], F32) for _ in range(2)]
    tt_ = [k.sb("tt", [128, 512], F32) for _ in range(2)]
    tu = [k.sb("tu", [128, 512], F32) for _ in range(2)]
    otm = [k.sb("otm", [128, 1024], F32) for _ in range(4)]
    pg = [k.ps("pg", [128, 512]) for _ in range(2)]
    pu = [k.ps("pu", [128, 512]) for _ in range(2)]
    pd = [k.ps("pd", [128, 512]) for _ in range(2)]
    pt2 = [k.ps("pt2", [128, 1024], BF16) for _ in range(2)]
    for hh in hg:
        for t_ in hh:
            k.memset("dve", t_[:, :], 0.0)
    h_src = Buf(k, S["h_tm"].t, "dram")
    rec_src = Buf(k, S["rec"].t, "dram")

    def prefetch(g):
        b = g % 2
        k.stt(wtmp[:, 0:2], c.ones[:, 0:2], eg256[:, g:g + 1], iorow[:, 0:2], ALU.mult, ALU.add)
        k.copy("dve", widx[b][:, :], wtmp[:, 0:2])
        k.ts("dve", wtmp2[:, 0:1], iop[:, :], eg128[:, g:g + 1], None, ALU.add)
        k.copy("dve", wtmp2[:, 1:2], egrp[:, g:g + 1])
        k.copy("dve", bidx[b][:, :], wtmp2[:, :])
        k.idma(bgu[b][:, :], W["b_gu_rows"].v(W["b_gu_rows"].t), bidx[b][:, 0:1])
        k.idma(bdn[b][:, :], W["b_down"].v(W["b_down"].t), bidx[b][:, 1:2])
        for hf in range(2):
            k.idma(wgu[b][hf].v(wgu[b][hf].t[:, :, :].rearrange("p k n -> p (k n)")),
                   W["w_gu_rows"].v(W["w_gu_rows"].t), widx[b][:, hf:hf + 1])
        k.idma(wdn[b].v(wdn[b].t[:, :, :].rearrange("p k n -> p (k n)")),
               W["w_down_rows"].v(W["w_down_rows"].t), bidx[b][:, 0:1])

    def prefetch_tok(g):
        b = g % 2
        for sb_ in range(4):
            s0 = g * 512 + sb_ * 128
            k.dma("sp", recg[b][sb_][:, :], rec_src.v(rec_src.t[s0:s0 + 128, :]))
        for sb_ in range(4):
            k.idma(hg[b][sb_][:, :], h_src.v(h_src.t), recg[b][sb_][:, 0:1])

    NGX = NG
    prefetch_tok(0)
    prefetch(0)
    for g in range(NGX):
        b = g % 2
        if g + 1 < NGX:
            prefetch_tok(g + 1)
            prefetch(g + 1)
        hT_ = hT[b]
        k.ts("dve", bgu[b][:, 8:16], bgu[b][:, 8:16], 1.0, None, ALU.add)
        k.ts("dve", bdnb[b][0:1, :], bdn[b][0:1, :], ALPHA, None, ALU.mult)
        for sb_ in range(4):
            k.ts("dve", wsc[sb_][:, :], recg[b][sb_].v(recg[b][sb_].t[:, 1:2].bitcast(F32)), 1.0 / ALPHA, None, ALU.mult)
        for ch in range(8):
            p = pt2[ch % 2]
            k.mm(p, [(p[:, sb_ * 128:(sb_ + 1) * 128], hg[b][sb_][:, ch * 128:(ch + 1) * 128], c.identb[:, :], True, True)
                     for sb_ in range(4)], transpose=True)
            k.copy("act", hT_[:, ch, :], p[:, 0:512])
        aT = actT[b]
        for n in range(8):
            a, b_ = pg[n % 2], pu[n % 2]
            k.mm(a, [(a[:, :], wgu[b][kc // 4][:, kc % 4, n * 128:(n + 1) * 128], hT_[:, kc, :], kc == 0, kc == 7) for kc in range(8)])
            k.mm(b_, [(b_[:, :], wgu[b][kc // 4][:, kc % 4, 1024 + n * 128:1024 + (n + 1) * 128], hT_[:, kc, :], kc == 0, kc == 7)
                      for kc in range(8)])
            g_, t_, u_ = tg[n % 2], tt_[n % 2], tu[n % 2]
            k.ts("dve", g_[:, :], a[:, :], bgu[b][:, n:n + 1], 7.0, ALU.add, ALU.min)
            k.act(t_[:, :], g_[:, :], AF.Silu, scale=ALPHA)
            k.ts("dve", u_[:, :], b_[:, :], bgu[b][:, 8 + n:9 + n], -6.0, ALU.add, ALU.max)
            k.stt(aT[:, n, :], u_[:, :], 8.0, t_[:, :], ALU.min, ALU.mult)
        for sb_ in range(4):
            for half in range(2):
                p = pd[(sb_ * 2 + half) % 2]
                mms = [(p[:, :], aT[:, kc, sb_ * 128:(sb_ + 1) * 128], wdn[b][:, kc, half * 512:(half + 1) * 512],
                        kc == 0, False) for kc in range(8)]
                mms.append((p[:, :], onesb[0:1, 0:128], bdnb[b][0:1, half * 512:(half + 1) * 512], False, True))
                k.mm(p, mms)
                k.act(otm[sb_][:, half * 512:(half + 1) * 512], p[:, :], AF.Copy, scale=wsc[sb_][:, 0:1])
            s0 = g * 512 + sb_ * 128
            reg = k.region()
            k.dma("sp", reg.v(S["oslots"].t[s0:s0 + 128, :]), otm[sb_][:, :])
    k.stage_end()

    k.stage_begin()
    x32 = [k.sb("x32", [128, 512], F32) for _ in range(8)]
    yg2 = [[[k.sb("yg", [128, 1024], F32) for _ in range(4)] for _ in range(4)] for _ in range(2)]
    pc = [k.ps("pc", [128, 512]) for _ in range(8)]
    o_src = Buf(k, S["oslots"].t, "dram")
    for it in range(L // 512):
        t0 = it * 512
        load_x_tile(c, "sp", x32, xin, t0, 512)
        yg = yg2[it % 2]
        for j in range(4):
            blk = it * 4 + j
            for r in range(4):
                k.idma(yg[j][r][:, :], o_src.v(o_src.t), slots_all[:, blk, r:r + 1])
        for ch in range(8):
            p = pc[ch]
            for j in range(4):
                k.mm(p, [(p[:, j * 128:(j + 1) * 128], yg[j][r][:, ch * 128:(ch + 1) * 128], c.ident[:, :], r == 0, r == 3)
                         for r in range(4)])
            k.stt(x32[ch][:, :], p[:, :], c.mod[:, layer, 40 + ch:41 + ch], x32[ch][:, :], ALU.mult, ALU.add)
        store_x_tile(c, "sp", x32, xout, t0, 512)
    k.stage_end()
    k.stage_end()


DEPTH = 4
SEQ = 8192
BATCH = 8


def prologue_stage(c, xd, xT, Wd):
    k = c.k
    L = c.L
    depth = c.depth
    k.stage_begin()
    cl = k.sb("cl", [128, 8], F32)
    wa = [k.sb("wa", [128, 8, 1024], F32) for _ in range(2)]
    bada = k.sb("bada", [128, depth, 48], F32)
    gl = k.sb("gl", [128, depth + 1, 16], F32)
    pm = k.ps("pm", [128, 512])
    k.dma("sp", cl[:, :], Wd["c_l"].v(Wd["c_l"].t))
    k.dma("sp", bada[:, :, :], Wd["b_ada_l"].v(Wd["b_ada_l"].t))
    k.dma("sp", gl[:, :, :], Wd["g_l"].v(Wd["g_l"].t))
    k.act(cl[:, :], cl[:, :], AF.Silu)
    n = 0
    for l in range(depth):
        for pc in range(6):
            w_ = wa[n % 2]
            n += 1
            k.dma("sp", w_[:, :, :], Wd["w_ada"].v(Wd["w_ada"].t[l][:, pc * 1024:(pc + 1) * 1024]
                                                  .rearrange("(kc p) n -> p kc n", p=128)))
            for nn in range(8):
                col = l * 48 + pc * 8 + nn
                k.mm(pm, [(pm[:, col:col + 1], w_[:, kc, nn * 128:(nn + 1) * 128], cl[:, kc:kc + 1], kc == 0, kc == 7)
                          for kc in range(8)])
    k.tt("dve", c.mod.v(c.mod.t[:, 0:depth, :].rearrange("p l n -> p (l n)")), pm[:, 0:depth * 48],
         bada.v(bada.t[:, :, :].rearrange("p l n -> p (l n)")), ALU.add)
    for l in range(depth):
        k.stt(c.geff[:, l, 0:8], c.mod[:, l, 8:16], 1.0, gl[:, l, 0:8], ALU.add, ALU.mult)
        k.stt(c.geff[:, l, 8:16], c.mod[:, l, 32:40], 1.0, gl[:, l, 8:16], ALU.add, ALU.mult)
    k.copy("dve", c.geff[:, depth, 0:8], gl[:, depth, 0:8])
    k.memset("dve", c.mod[:, depth, :], 0.0)
    xtm = [k.sb("xtm", [128, 1024], F32) for _ in range(4)]
    x32 = [k.sb("x32", [128, 512], F32) for _ in range(8)]
    pt = [k.ps("pt", [128, 512]) for _ in range(2)]
    for it in range(L // 512):
        for j in range(4):
            r0 = it * 512 + j * 128
            k.dma("sp", xtm[j][:, :], xd.v(xd.t[r0:r0 + 128, :]))
        for ch in range(8):
            p = pt[ch % 2]
            k.mm(p, [(p[:, j * 128:(j + 1) * 128], xtm[j][:, ch * 128:(ch + 1) * 128], c.ident[:, :], True, True)
                     for j in range(4)], transpose=True)
            k.copy("act" if ch % 2 else "dve", x32[ch][:, :], p[:, :])
        store_x_tile(c, "sp", x32, xT, it * 512, 512)
    k.stage_end()


def final_stage(c, xT, outd):
    k = c.k
    L = c.L
    k.stage_begin()
    x32 = [k.sb("x32", [128, 512], F32) for _ in range(8)]
    sq = [k.sb("sq", [128, 512], F32) for _ in range(2)]
    rstd = k.sb("rstd", [128, 512], F32)
    ytm = [k.sb("ytm", [128, 1024], F32) for _ in range(2)]
    pst = k.ps("pst", [128, 512])
    pt = [k.ps("pt", [128, 512]) for _ in range(2)]
    n = 0
    for it in range(L // 512):
        load_x_tile(c, "sp", x32, xT, it * 512, 512)
        rmsnorm_mod(c, x32, 512, c.depth, 0, sq, rstd, pst)
        for j in range(4):
            y_ = ytm[j % 2]
            for half in range(2):
                p = pt[n % 2]
                n += 1
                k.mm(p, [(p[:, cc * 128:(cc + 1) * 128], x32[half * 4 + cc][:, j * 128:(j + 1) * 128], c.ident[:, :],
                          True, True) for cc in range(4)], transpose=True)
                k.copy("act" if half else "dve", y_[:, half * 512:(half + 1) * 512], p[:, :])
            r0 = it * 512 + j * 128
            k.dma("sp", outd.v(outd.t[r0:r0 + 128, :]), y_[:, :])
    k.stage_end()


def build_program(L=SEQ, depth=DEPTH, ne=NE):
    nc = bass.Bass("TRN2", target_bir_lowering=False)
    k = K(nc)
    c = make_ctx(k, L, depth + 1)
    c.depth = depth
    make_chunk_masks(c)
    ext = lambda name, shape: k.dram(name, shape, F32, kind="ExternalInput")
    xd = ext("x", [L, D])
    outd = k.dram("out", [L, D], F32, kind="ExternalOutput")
    xa = k.dram("xa", [8, 128, L], F32)
    xb = k.dram("xb", [8, 128, L], F32)
    NA, NB_, NC3 = (depth + 2) // 3, (depth + 1) // 3, depth // 3
    Wd = dict(c_l=ext("c_l", [128, 8]), b_ada_l=ext("b_ada_l", [128, depth, 48]), g_l=ext("g_l", [128, depth + 1, 16]),
              w_ada=ext("w_ada", [depth, D, 6 * D]))
    gla = dict(w_in=ext("gla_w_in", [NA, D, 3072]), w_gk1=ext("gla_w_gk1", [NA, D, 16]),
               w_gk2=ext("gla_w_gk2", [NA, 16, 512]), b_gk=ext("gla_b_gk", [NA, 1, 512]),
               g_onorm_l=ext("gla_g_onorm_l", [NA, 128, 2]), w_out=ext("gla_w_out", [NA, D, D]))
    ml = {}
    if NB_:
        ml = dict(w_up=ext("ml_w_up", [NB_, D, 4096]), w_down=ext("ml_w_down", [NB_, 2048, D]),
                  b_if=ext("ml_b_if", [NB_, 1, 8]))
        for n_, shp in (("bdq", [128, 16, 128]), ("bdk", [128, 16, 128]), ("bdv", [128, 16, 128]),
                        ("bdqT", [128, 16, 128]), ("bdkT", [128, 16, 128]), ("bdvT", [128, 16, 128]),
                        ("conv_l", [128, 16, 5]), ("wif_l", [128, 48, 8]), ("skip_l", [128, 16]), ("gnorm_l", [128, 16])):
            ml[n_] = ext("ml_" + n_, [NB_] + shp)
    s5 = {}
    if NC3:
        s5 = dict(w_in=ext("s5_w_in", [NC3, D, D]), w_out=ext("s5_w_out", [NC3, D, 2 * D]), d_l=ext("s5_d_l", [NC3, 128, 8]))
        for n_ in ("Ablk_re", "Ablk_im", "Lblk", "Bblk_re", "Bblk_im", "Cblk_re", "Cblk_im"):
            s5[n_] = ext("s5_" + n_, [NC3, 128, 32 * 128])
        for n_ in ("Acol_re", "Acol_im", "Lcol"):
            s5[n_] = ext("s5_" + n_, [NC3, 128, 32])
    moe = dict(w_router=ext("moe_w_router", [depth, D, 32]), b_router=ext("moe_b_router", [depth, 1, 32]),
               w_gu_rows=ext("moe_w_gu_rows", [depth, ne * 256, 4 * 2 * D]), b_gu_rows=ext("moe_b_gu_rows", [depth, ne * 128, 16]),
               w_down_rows=ext("moe_w_down_rows", [depth, ne * 128, 8 * D]), b_down=ext("moe_b_down", [depth, ne, D]))
    NG = (4 * L + ne * 511) // 512
    S = dict(h_tm=k.dram("h_tm", [L, D], BF16), rec=k.dram("rec", [NG * 512, 2], I32),
             oslots=k.dram("oslots", [NG * 512, D], F32))

    def sub(dct, j):
        return {n_: Buf(k, b.t[j], "dram") for n_, b in dct.items()}

    prologue_stage(c, xd, xa, Wd)
    for i in range(depth):
        kind, j = i % 3, i // 3
        if kind == 0:
            gla_stage(c, i, xa, xb, sub(gla, j))
        elif kind == 1:
            mlstm_stage(c, i, xa, xb, sub(ml, j))
        else:
            s5_stage(c, i, xa, xb, sub(s5, j))
        mw = sub({n_: moe[n_] for n_ in ("w_router", "b_router")}, i)
        for n_ in ("w_gu_rows", "w_down_rows", "b_gu_rows", "b_down"):
            mw[n_] = Buf(k, moe[n_].t.rearrange("l r c -> (l r) c"), "dram")
        moe_sparse_stage(c, i, xb, xa, mw, S, ne=ne, eoff=i * ne)
    final_stage(c, xa, outd)
    k.finish([outd])
    return nc, k


def host_layouts(inp, depth=DEPTH):
    f32 = lambda a: np.ascontiguousarray(np.asarray(a, dtype=np.float32))
    col = lambda v, n: f32(np.asarray(v).reshape(n, 128).T)
    sh = {}
    sh["b_ada_l"] = f32(np.asarray(inp["b_ada"]).reshape(depth, 48, 128).transpose(2, 0, 1))
    gl = np.zeros((128, depth + 1, 16), np.float32)
    for l in range(depth):
        gl[:, l, 0:8] = col(inp["g_mix"][l], 8)
        gl[:, l, 8:16] = col(inp["g_ffn"][l], 8)
    gl[:, depth, 0:8] = col(inp["g_final"], 8)
    sh["g_l"] = gl
    sh["w_ada"] = f32(inp["w_ada"])
    for n_ in ("gla_w_in", "gla_w_gk1", "gla_w_gk2", "gla_w_out", "ml_w_up", "ml_w_down", "s5_w_in", "s5_w_out",
               "moe_w_router", "moe_b_down"):
        sh[n_] = f32(inp[n_])
    na = np.asarray(inp["gla_b_gk"]).shape[0]
    sh["gla_b_gk"] = f32(np.asarray(inp["gla_b_gk"]).reshape(na, 1, 512))
    sh["gla_g_onorm_l"] = f32(np.stack([col(g, 2) for g in np.asarray(inp["gla_g_onorm"])]))
    nb = np.asarray(inp["ml_w_up"]).shape[0]
    sh["ml_b_if"] = f32(np.asarray(inp["ml_b_if"]).reshape(nb, 1, 8))
    mls = [mlstm_host_layout(*[np.asarray(inp["ml_" + n_][j]) for n_ in
                               ("conv_w", "conv_b", "w_q", "w_k", "w_v", "w_if", "skip", "g_norm")]) for j in range(nb)]
    for n_ in mls[0]:
        sh["ml_" + n_] = f32(np.stack([m[n_] for m in mls]))
    n3 = np.asarray(inp["s5_w_in"]).shape[0]
    s5s = [s5_host_layout(*[np.asarray(inp["s5_" + n_][j]) for n_ in
                            ("a_re", "a_im", "log_dt", "b_re", "b_im", "c_re", "c_im", "d")]) for j in range(n3)]
    for n_ in s5s[0]:
        sh["s5_" + n_] = f32(np.stack([m[n_] for m in s5s]))
    sh["moe_b_router"] = f32(np.asarray(inp["moe_b_router"]).reshape(depth, 1, 32))
    bgu = np.asarray(inp["moe_b_gu"])
    ne = bgu.shape[1]
    sh["moe_b_gu_rows"] = f32(bgu.reshape(depth, ne, 16, 128).transpose(0, 1, 3, 2).reshape(depth, ne * 128, 16))
    sh["moe_w_gu_rows"] = f32(np.asarray(inp["moe_w_gu"]).reshape(depth, ne, 2, 4, 128, 2 * D)
                              .transpose(0, 1, 2, 4, 3, 5)).reshape(depth, ne * 256, 4 * 2 * D)
    sh["moe_w_down_rows"] = f32(np.asarray(inp["moe_w_down"]).reshape(depth, ne, 8, 128, D)
                                .transpose(0, 1, 3, 2, 4)).reshape(depth, ne * 128, 8 * D)
    return sh


_PROG = {}


def kernel(**inputs):
    x = np.asarray(inputs["x"], dtype=np.float32)
    cvec = np.asarray(inputs["c"], dtype=np.float32)
    B, L, _ = x.shape
    key = (L,)
    if key not in _PROG:
        _PROG[key] = build_program(L=L)[0]
    nc = _PROG[key]
    sh = host_layouts(inputs)
    in_maps = []
    for b in range(B):
        m = dict(sh)
        m["x"] = np.ascontiguousarray(x[b])
        m["c_l"] = np.ascontiguousarray(cvec[b].reshape(8, 128).T)
        in_maps.append(m)
    res = run_bass_kernel_spmd(nc, in_maps, core_ids=list(range(B)))
    return np.stack([np.asarray(r["out"], dtype=np.float32) for r in res.results], axis=0)
```

```python
import numpy as np
import concourse.bass as bass
import concourse.mybir as mybir

F32 = mybir.dt.float32
BF16 = mybir.dt.bfloat16
AF = mybir.ActivationFunctionType
ALU = mybir.AluOpType
AX = mybir.AxisListType

SAME_ENGINE_SYNC = True


class V:
    __slots__ = ("b", "ap")

    def __init__(self, b, ap):
        self.b = b
        self.ap = ap


class Buf:
    def __init__(self, k, t, kind):
        self.k = k
        self.t = t
        self.kind = kind
        self.w = None
        self.r = {}
        self.ds = {}

    def __getitem__(self, idx):
        return V(self, self.t[idx])

    def v(self, ap):
        return V(self, ap)


class K:
    def __init__(self, nc):
        self.nc = nc
        self.E = dict(pe=nc.tensor, act=nc.scalar, dve=nc.vector, pool=nc.gpsimd, sp=nc.sync)
        self.sem = {k: nc.alloc_semaphore("sem_" + k) for k in self.E}
        self.cnt = {k: 0 for k in self.E}
        self.waited = {k: {} for k in self.E}
        self.semowner = {}
        self.n = 0
        self.ninstr = 0
        self.guards = []
        self.stage_bufs = []
        self.dsem_pool = {'hw': [], 'sw': []}
        self.all_dsem_bufs = []

    def sb(self, name, shape, dt=F32):
        self.n += 1
        name = "%s_%d" % (name, self.n)
        if self.guards:
            g = self.nc.sbuf_tensor(name, list(shape), dt)
            t = g.__enter__()
            self.guards[-1].append(g)
        else:
            t = self.nc.alloc_sbuf_tensor(name, list(shape), dt)
        b = Buf(self, t, "sb")
        if self.stage_bufs:
            self.stage_bufs[-1].append(b)
        return b

    def ps(self, name, shape, dt=F32):
        self.n += 1
        name = "%s_%d" % (name, self.n)
        if self.guards:
            g = self.nc.psum_tensor(name, list(shape), dt)
            t = g.__enter__()
            self.guards[-1].append(g)
        else:
            t = self.nc.alloc_psum_tensor(name, list(shape), dt)
        b = Buf(self, t, "ps")
        if self.stage_bufs:
            self.stage_bufs[-1].append(b)
        return b

    def stage_begin(self):
        self.guards.append([])
        self.stage_bufs.append([])

    def stage_end(self):
        self.barrier()
        for b in self.stage_bufs.pop():
            for kind, sc in b.ds.items():
                self.dsem_pool[kind].append(sc)
            b.ds = {}
        for g in reversed(self.guards.pop()):
            g.__exit__(None, None, None)

    def barrier(self):
        evs = [(self.sem[e], self.cnt[e], e) for e in self.E if self.cnt[e] > 0]
        dmas = [(sc[0], 16 * sc[1], "dma") for b in self.all_dsem_bufs for sc in b.ds.values() if sc[1] > 0]
        for e in self.E:
            for ev in evs:
                if ev[2] != e:
                    self._wait(e, ev)
            for ev in dmas:
                self._wait(e, ev)

    def dram(self, name, shape, dt=F32, kind="Internal"):
        return Buf(self, self.nc.dram_tensor(name, list(shape), dt, kind=kind).ap(), "dram")

    def region(self):
        return Buf(self, None, "dram")

    def _wait(self, e, ev):
        if ev is None:
            return
        sem, val, src = ev
        if src == e and (e == "pe" or not SAME_ENGINE_SYNC):
            return
        if src == "dma":
            val = 16 * self.semowner[id(sem)][1]
        w = self.waited[e]
        key = id(sem)
        if w.get(key, 0) >= val:
            return
        self.E[e].wait_ge(sem, val)
        w[key] = val

    def _deps(self, e, reads, writes):
        for b in reads:
            self._wait(e, b.w)
            if b.kind == "ps":
                for ke, ev in b.r.items():
                    if ke != e:
                        self._wait(e, ev)
        for b in writes:
            self._wait(e, b.w)
            for ev in b.r.values():
                self._wait(e, ev)

    def _commit(self, e, ins, reads, writes):
        self.cnt[e] += 1
        ev = (self.sem[e], self.cnt[e], e)
        ins.then_inc(self.sem[e], 1)
        for b in writes:
            b.w = ev
            b.r = {}
        ws = set(id(b) for b in writes)
        for b in reads:
            if id(b) not in ws:
                b.r[e] = ev
        self.ninstr += 1

    @staticmethod
    def _split(ops):
        bufs, aps = [], []
        for o in ops:
            if isinstance(o, V):
                bufs.append(o.b)
                aps.append(o.ap)
            else:
                aps.append(o)
        return bufs, aps

    def op(self, e, name, out, ins, extra_reads=(), **kw):
        rb, raps = self._split(ins)
        rb = rb + [x.b for x in extra_reads]
        kwv = {}
        wb = [out.b]
        for kk, vv in kw.items():
            if isinstance(vv, V):
                if kk == "accum_out":
                    wb.append(vv.b)
                else:
                    rb.append(vv.b)
                kwv[kk] = vv.ap
            else:
                kwv[kk] = vv
        self._deps(e, rb, wb)
        ins_ = getattr(self.E[e], name)(out.ap, *raps, **kwv)
        self._commit(e, ins_, rb, wb)
        return ins_

    def act(self, out, in_, func, bias=None, scale=None, e="act", accum_out=None):
        kw = {}
        if bias is not None:
            kw["bias"] = bias
        if scale is not None:
            kw["scale"] = scale
        rb = [in_.b]
        kwv = {}
        for kk, vv in kw.items():
            if isinstance(vv, V):
                rb.append(vv.b)
                kwv[kk] = vv.ap
            else:
                kwv[kk] = vv
        wb = [out.b]
        if accum_out is not None:
            wb.append(accum_out.b)
            kwv["accum_out"] = accum_out.ap
        self._deps("act", rb, wb)
        ins_ = self.nc.scalar.activation(out=out.ap, in_=in_.ap, func=func, **kwv)
        self._commit("act", ins_, rb, wb)

    def ts(self, e, out, in0, s1, s2, op0, op1=None):
        if op1 is None:
            return self.op(e, "tensor_scalar", out, [in0, s1, None, op0])
        return self.op(e, "tensor_scalar", out, [in0, s1, s2, op0, op1])

    def stt(self, out, in0, scalar, in1, op0, op1, e="dve", **kw):
        return self.op(e, "scalar_tensor_tensor", out, [in0, scalar, in1, op0, op1], **kw)

    def tt(self, e, out, in0, in1, op):
        return self.op(e, "tensor_tensor", out, [in0, in1, op])

    def copy(self, e, out, in_):
        if e == "act":
            return self.act(out, in_, AF.Copy)
        return self.op(e, "tensor_copy", out, [in_])

    def memset(self, e, out, val):
        self._deps(e, [], [out.b])
        ins_ = self.E[e].memset(out.ap, val)
        self._commit(e, ins_, [], [out.b])

    def mm(self, outbuf, mms, transpose=False):
        rb = []
        seen = set()
        for (o, l, r, st, sp) in mms:
            for x in (l, r):
                if id(x.b) not in seen:
                    seen.add(id(x.b))
                    rb.append(x.b)
        self._deps("pe", rb, [outbuf])
        ins_ = None
        for (o, l, r, st, sp) in mms:
            if transpose:
                ins_ = self.nc.tensor.transpose(o.ap, l.ap, r.ap)
            else:
                ins_ = self.nc.tensor.matmul(o.ap, l.ap, r.ap, start=st, stop=sp)
            self.ninstr += 1
        self._commit("pe", ins_, rb, [outbuf])

    def _dsem(self, sbuf, kind):
        if kind not in sbuf.ds:
            if self.dsem_pool[kind]:
                sc = self.dsem_pool[kind].pop()
            else:
                sc = [self.nc.alloc_semaphore("dsem%d" % self.n), 0]
                self.n += 1
            sbuf.ds[kind] = sc
            self.semowner[id(sc[0])] = sc
            if sbuf not in self.all_dsem_bufs:
                self.all_dsem_bufs.append(sbuf)
        sc = sbuf.ds[kind]
        sc[1] += 1
        return sc

    def dma(self, q, out, in_, sem_buf=None, **kw):
        ob, ib = out.b, in_.b
        self._deps(q, [ib], [ob])
        sbuf = sem_buf or (ob if ob.kind == "sb" else ib)
        sc = self._dsem(sbuf, "sw" if q == "pool" else "hw")
        ins_ = self.E[q].dma_start(out=out.ap, in_=in_.ap, **kw)
        ins_.then_inc(sc[0], 16)
        ev = (sc[0], 16 * sc[1], "dma")
        ob.w = ev
        ob.r = {}
        if ib is not ob:
            ib.r[id(sc[0])] = ev
        self.ninstr += 1

    def idma(self, out, in_, idx, scatter=False, **kw):
        ob, ib = out.b, in_.b
        self._deps("pool", [ib, idx.b], [ob])
        sbuf = ob if ob.kind == "sb" else ib
        sc = self._dsem(sbuf, "sw")
        off = bass.IndirectOffsetOnAxis(ap=idx.ap, axis=0)
        if scatter:
            ins_ = self.nc.gpsimd.indirect_dma_start(out=out.ap, out_offset=off, in_=in_.ap, in_offset=None, **kw)
        else:
            ins_ = self.nc.gpsimd.indirect_dma_start(out=out.ap, out_offset=None, in_=in_.ap, in_offset=off, **kw)
        ins_.then_inc(sc[0], 16)
        ev = (sc[0], 16 * sc[1], "dma")
        ob.w = ev
        ob.r = {}
        ib.r[id(sc[0])] = ev
        idx.b.r[id(sc[0])] = ev
        self.ninstr += 1

    def vload(self, e, v, lo, hi):
        self._deps(e, [v.b], [])
        return self.E[e].value_load(v.ap, min_val=lo, max_val=hi)

    def finish(self, bufs, e="sp"):
        for b in bufs:
            self._wait(e, b.w)
            for ev in b.r.values():
                self._wait(e, ev)

from concourse.bass_utils import run_bass_kernel_spmd

D = 1024
EPS = 1e-6
NE = 32
ALPHA = 1.702


class Ctx:
    pass


def make_ctx(k, L, depth):
    c = Ctx()
    c.k = k
    c.L = L
    c.depth = depth
    c.ones = k.sb("ones", [128, 512], F32)
    c.ident = k.sb("ident", [128, 128], F32)
    c.identb = k.sb("identb", [128, 128], BF16)
    c.mod = k.sb("mod", [128, depth, 48], F32)
    c.geff = k.sb("geff", [128, depth, 16], F32)
    k.memset("dve", c.ones[:, 0:512], 1.0)
    k.op("pool", "affine_select", c.ident[:, :], [c.ones[:, 0:128]], pattern=[[-1, 128]],
         compare_op=ALU.is_equal, fill=0.0, base=0, channel_multiplier=1)
    k.copy("dve", c.identb[:, :], c.ident[:, :])
    return c


def load_x_tile(c, q, x32, xin, t0, T):
    for ch in range(8):
        c.k.dma(q, x32[ch][:, :T], xin.v(xin.t[ch, :, t0:t0 + T]))


def store_x_tile(c, q, x32, xout, t0, T):
    for ch in range(8):
        c.k.dma(q, xout.v(xout.t[ch, :, t0:t0 + T]), x32[ch][:, :T])


def rmsnorm_mod(c, x32, T, layer, which, sq, rstd, ps_stat, out_bf=None, off=0, cp_eng="pool"):
    k = c.k
    for ch in range(8):
        s = sq[ch % len(sq)]
        k.act(s[:, :T], x32[ch][:, :T], AF.Square)
        k.mm(ps_stat, [(ps_stat[:, :T], c.ones[:, 0:128], s[:, :T], ch == 0, ch == 7)])
    k.ts("dve", rstd[:, :T], ps_stat[:, :T], 1.0 / D, EPS, ALU.mult, ALU.add)
    k.act(rstd[:, :T], rstd[:, :T], AF.Sqrt)
    k.op("dve", "reciprocal", rstd[:, :T], [rstd[:, :T]])
    shb = 0 if which == 0 else 24
    for ch in range(8):
        k.stt(x32[ch][:, :T], x32[ch][:, :T], c.geff[:, layer, which * 8 + ch:which * 8 + ch + 1],
              rstd[:, :T], ALU.mult, ALU.mult)
        k.act(x32[ch][:, :T], x32[ch][:, :T], AF.Identity,
              bias=c.mod[:, layer, shb + ch:shb + ch + 1])
        if out_bf is not None:
            k.copy(cp_eng, out_bf[:, ch, off:off + T], x32[ch][:, :T])


def moe_stage(c, layer, xin, xout, W, ne=NE):
    k = c.k
    L = c.L
    ST = min(1024, L)
    NSUB = ST // 128
    NTS = ST // 512
    k.stage_begin()
    wgu = [k.sb("wgu", [128, 8, 2048], BF16) for _ in range(2)]
    wdn = [k.sb("wdn", [128, 8, 1024], BF16) for _ in range(2)]
    acc = [k.sb("acc", [128, 1024], F32) for _ in range(NSUB)]
    hbf = k.sb("hbf", [128, 8, ST], BF16)
    x32 = [k.sb("x32", [128, 512], F32) for _ in range(8)]
    actT = [k.sb("actT", [128, 8, 512], BF16) for _ in range(2)]
    tg = [k.sb("tg", [128, 512], F32) for _ in range(2)]
    tt_ = [k.sb("tt", [128, 512], F32) for _ in range(2)]
    tu = [k.sb("tu", [128, 512], F32) for _ in range(2)]
    sq = tg
    rstd = tt_[0]
    wr = k.sb("wr", [128, 8, 32], F32)
    br = k.sb("br", [1, 32], F32)
    bgu = k.sb("bgu", [128, ne, 16], F32)
    bdn = k.sb("bdn", [32, 1024], F32)
    comb = k.sb("comb", [128, NSUB, 32], F32)
    combs = k.sb("combs", [128, NSUB, 32], F32)
    combT = k.sb("combT", [32, ST], F32)
    lg = k.sb("lg", [128, 32], F32)
    m8 = k.sb("m8", [128, 8], F32)
    negm = k.sb("negm", [128, 1], F32)
    mask = k.sb("mask", [128, 32], F32)
    ex = k.sb("ex", [128, 32], F32)
    ssum = k.sb("ssum", [128, 1], F32)
    pg = [k.ps("pg", [128, 512]) for _ in range(2)]
    pu = [k.ps("pu", [128, 512]) for _ in range(2)]
    pd = [k.ps("pd", [128, 512]) for _ in range(2)]
    pm = [k.ps("pm", [128, 512]) for _ in range(2)]

    k.dma("sp", wr[:, :, :], W["w_router"].v(W["w_router"].t.rearrange("(kc p) e -> p kc e", p=128)))
    k.dma("sp", br[:, :], W["b_router"].v(W["b_router"].t))
    k.dma("sp", bgu[:, :, :], W["b_gu_l"].v(W["b_gu_l"].t))
    k.dma("sp", bdn[:ne, :], W["b_down"].v(W["b_down"].t))
    k.ts("dve", bgu[:, :, 8:16], bgu[:, :, 8:16], 1.0, None, ALU.add)

    def load_w(gi):
        e = gi % ne
        k.dma("pool", wgu[gi % 2][:, :, :],
              W["w_gu"].v(W["w_gu"].t[e].rearrange("(kc p) n -> p kc n", p=128)))
        k.dma("pool", wdn[gi % 2][:, :, :],
              W["w_down"].v(W["w_down"].t[e].rearrange("(kc p) n -> p kc n", p=128)))

    nst = L // ST
    load_w(0)
    pmi = 0
    cnt = 0
    for s in range(nst):
        for ts_ in range(NTS):
            t0 = s * ST + ts_ * 512
            load_x_tile(c, "sp", x32, xin, t0, 512)
            rmsnorm_mod(c, x32, 512, layer, 1, sq, rstd, pm[0], out_bf=hbf, off=ts_ * 512, cp_eng="dve")
            for j in range(4):
                sub = ts_ * 4 + j
                p = pm[1]
                mms = [(p[:, :32], x32[ch][:, j * 128:(j + 1) * 128], wr[:, ch, :], ch == 0, False)
                       for ch in range(8)]
                mms.append((p[:, :32], c.ones[0:1, 0:128], br[0:1, :], False, True))
                k.mm(p, mms)
                k.copy("dve", lg[:, :], p[:, :32])
                k.op("dve", "max", m8[:, :], [lg[:, :]])
                k.ts("dve", mask[:, :], lg[:, :], m8[:, 3:4], None, ALU.is_ge)
                k.ts("dve", negm[:, :], m8[:, 0:1], -1.0, None, ALU.mult)
                k.act(ex[:, :], lg[:, :], AF.Exp, bias=negm[:, 0:1], scale=1.0)
                k.stt(ex[:, :], ex[:, :], 1.0, mask[:, :], ALU.mult, ALU.mult, accum_out=ssum[:, :])
                k.op("dve", "reciprocal", ssum[:, :], [ssum[:, :]])
                k.ts("dve", comb[:, sub, :], ex[:, :], ssum[:, 0:1], None, ALU.mult)
                k.ts("dve", combs[:, sub, :], ex[:, :], ssum[:, 0:1], 1.0 / ALPHA, ALU.mult, ALU.mult)
                k.mm(p, [(p[:32, 128:256], comb[:, sub, :], c.ident[:, :], True, True)], transpose=True)
                k.copy("dve", combT[:, sub * 128:(sub + 1) * 128], p[:32, 128:256])
        for e in range(ne):
            gi = s * ne + e
            if gi + 1 < nst * ne:
                load_w(gi + 1)
            wg, wd = wgu[gi % 2], wdn[gi % 2]
            for ts_ in range(NTS):
                aT = actT[cnt % 2]
                cnt += 1
                for n in range(8):
                    a, b = pg[n % 2], pu[n % 2]
                    k.mm(a, [(a[:, :], wg[:, kc, n * 128:(n + 1) * 128], hbf[:, kc, ts_ * 512:(ts_ + 1) * 512],
                              kc == 0, kc == 7) for kc in range(8)])
                    k.mm(b, [(b[:, :], wg[:, kc, 1024 + n * 128:1024 + (n + 1) * 128],
                              hbf[:, kc, ts_ * 512:(ts_ + 1) * 512], kc == 0, kc == 7) for kc in range(8)])
                    g, t, u = tg[n % 2], tt_[n % 2], tu[n % 2]
                    k.ts("dve", g[:, :], a[:, :], bgu[:, e, n:n + 1], 7.0, ALU.add, ALU.min)
                    k.act(t[:, :], g[:, :], AF.Silu, scale=ALPHA)
                    k.ts("dve", u[:, :], b[:, :], bgu[:, e, 8 + n:9 + n], -6.0, ALU.add, ALU.max)
                    k.stt(aT[:, n, :], u[:, :], 8.0, t[:, :], ALU.min, ALU.mult)
                for j in range(4):
                    sub = ts_ * 4 + j
                    for half in range(2):
                        p = pd[(j * 2 + half) % 2]
                        k.mm(p, [(p[:, :], aT[:, kc, j * 128:(j + 1) * 128], wd[:, kc, half * 512:(half + 1) * 512],
                                  kc == 0, kc == 7) for kc in range(8)])
                        av = acc[sub][:, half * 512:(half + 1) * 512]
                        if e == 0:
                            k.ts("dve", av, p[:, :], combs[:, sub, e:e + 1], None, ALU.mult)
                        else:
                            k.stt(av, p[:, :], combs[:, sub, e:e + 1], av, ALU.mult, ALU.add)
        for sub in range(NSUB):
            for half in range(2):
                p = pm[(sub * 2 + half) % 2]
                k.mm(p, [(p[:, :], combT[:ne, sub * 128:(sub + 1) * 128], bdn[:ne, half * 512:(half + 1) * 512],
                          True, True)])
                av = acc[sub][:, half * 512:(half + 1) * 512]
                k.tt("dve", av, av, p[:, :], ALU.add)
        for ts_ in range(NTS):
            t0 = s * ST + ts_ * 512
            load_x_tile(c, "sp", x32, xin, t0, 512)
            for ch in range(8):
                p = pm[ch % 2]
                k.mm(p, [(p[:, j * 128:(j + 1) * 128], acc[ts_ * 4 + j][:, ch * 128:(ch + 1) * 128], c.ident[:, :],
                          True, True) for j in range(4)], transpose=True)
                k.stt(x32[ch][:, :], p[:, :], c.mod[:, layer, 40 + ch:41 + ch], x32[ch][:, :], ALU.mult, ALU.add)
            store_x_tile(c, "sp", x32, xout, t0, 512)
    k.stage_end()


def make_chunk_masks(c):
    k = c.k
    c.mrev = k.sb("mrev", [128, 128], F32)
    c.mch = k.sb("mch", [128, 2], F32)
    k.op("pool", "affine_select", c.mrev[:, :], [c.ones[:, 0:128]], pattern=[[-1, 128]],
         compare_op=ALU.is_gt, fill=0.0, base=0, channel_multiplier=1)
    k.op("pool", "affine_select", c.mrev[:, 0:64], [c.mrev[:, 0:64]], pattern=[[0, 64]],
         compare_op=ALU.is_gt, fill=0.0, base=64, channel_multiplier=-1)
    k.op("pool", "affine_select", c.mch[:, 0:1], [c.ones[:, 0:1]], pattern=[[0, 1]],
         compare_op=ALU.is_gt, fill=0.0, base=64, channel_multiplier=-1)
    k.op("pool", "affine_select", c.mch[:, 1:2], [c.ones[:, 0:1]], pattern=[[0, 1]],
         compare_op=ALU.is_ge, fill=0.0, base=-64, channel_multiplier=1)


def gla_stage(c, layer, xin, xout, W):
    k = c.k
    L = c.L
    T = 512
    k.stage_begin()
    w_in = k.sb("w_in", [128, 8, 3072], BF16)
    w_out = k.sb("w_out", [128, 8, 1024], BF16)
    w_gk1 = k.sb("w_gk1", [128, 8, 16], BF16)
    w_gk2 = k.sb("w_gk2", [16, 512], F32)
    b_gk = k.sb("b_gk", [1, 512], F32)
    g_on = k.sb("g_on", [128, 2], F32)
    x32 = [k.sb("x32", [128, T], F32) for _ in range(8)]
    hbf = k.sb("hbf", [128, 8, T], BF16)
    sq = [k.sb("sq", [128, T], F32) for _ in range(2)]
    rstd = k.sb("rstd", [128, T], F32)
    qT = [k.sb("qT", [128, T], BF16) for _ in range(4)]
    kdec = [k.sb("kdec", [128, 512], BF16) for _ in range(4)]
    vbf = [k.sb("vbf", [128, 1024], BF16) for _ in range(4)]
    gs = [k.sb("gs", [128, T], F32) for _ in range(8)]
    rT = k.sb("rT", [16, T], F32)
    la = [k.sb("la", [128, 512], F32) for _ in range(2)]
    edec = [k.sb("edec", [128, 512], F32) for _ in range(2)]
    dcy = k.sb("dcy", [128, 4, 8], F32)
    S = [k.sb("S", [128, 256], F32) for _ in range(4)]
    Sb = [[k.sb("Sb", [128, 256], BF16) for _ in range(2)] for _ in range(4)]
    oT = [k.sb("oT", [128, 2, T], F32) for _ in range(4)]
    onb = k.sb("onb", [128, 8, T], BF16)
    tmp = [k.sb("tmp", [128, T], F32) for _ in range(2)]
    pA = [k.ps("pA", [128, 512]) for _ in range(2)]
    pU = [k.ps("pU", [128, 512]) for _ in range(2)]
    pO = [k.ps("pO", [128, 512]) for _ in range(2)]
    ptot = k.ps("ptot", [128, 512])
    pst = k.ps("pst", [128, 512])

    k.dma("pool", w_in[:, :, :], W["w_in"].v(W["w_in"].t.rearrange("(kc p) n -> p kc n", p=128)))
    k.dma("pool", w_out[:, :, :], W["w_out"].v(W["w_out"].t.rearrange("(kc p) n -> p kc n", p=128)))
    k.dma("pool", w_gk1[:, :, :], W["w_gk1"].v(W["w_gk1"].t.rearrange("(kc p) n -> p kc n", p=128)))
    k.dma("sp", w_gk2[:, :], W["w_gk2"].v(W["w_gk2"].t))
    k.dma("sp", b_gk[:, :], W["b_gk"].v(W["b_gk"].t))
    k.dma("sp", g_on[:, :], W["g_onorm_l"].v(W["g_onorm_l"].t))
    for h in range(4):
        k.memset("dve", S[h][:, :], 0.0)

    pai = [0]

    def nextA():
        pai[0] += 1
        return pA[pai[0] % 2]

    for it in range(L // T):
        t0 = it * T
        load_x_tile(c, "sp", x32, xin, t0, T)
        rmsnorm_mod(c, x32, T, layer, 0, sq, rstd, pst, out_bf=hbf, off=0, cp_eng="dve")
        load_x_tile(c, "sp", x32, xin, t0, T)
        for h in range(4):
            p = nextA()
            k.mm(p, [(p[:, :], w_in[:, kc, h * 128:(h + 1) * 128], hbf[:, kc, :], kc == 0, kc == 7)
                     for kc in range(8)])
            k.act(qT[h][:, :], p[:, :], AF.Copy, scale=128.0 ** -0.5)
        for n in range(8):
            p = nextA()
            k.mm(p, [(p[:, :], w_in[:, kc, 2048 + n * 128:2048 + (n + 1) * 128], hbf[:, kc, :], kc == 0, kc == 7)
                     for kc in range(8)])
            k.act(gs[n][:, :], p[:, :], AF.Silu)
        p = nextA()
        k.mm(p, [(p[:16, :], w_gk1[:, kc, :], hbf[:, kc, :], kc == 0, kc == 7) for kc in range(8)])
        k.copy("dve", rT[:, :], p[:16, :])
        for j in range(4):
            for half in range(2):
                p = nextA()
                k.mm(p, [(p[:, :], hbf[:, kc, j * 128:(j + 1) * 128],
                          w_in[:, kc, 1024 + half * 512:1024 + (half + 1) * 512], kc == 0, kc == 7)
                         for kc in range(8)])
                k.copy("act", vbf[j][:, half * 512:(half + 1) * 512], p[:, :])
            p = nextA()
            k.mm(p, [(p[:, :], rT[:16, j * 128:(j + 1) * 128], w_gk2[:16, :], True, False),
                     (p[:, :], c.ones[0:1, 0:128], b_gk[0:1, :], False, True)])
            l_ = la[j % 2]
            k.act(l_[:, :], p[:, :], AF.Exp, scale=-1.0)
            k.act(l_[:, :], l_[:, :], AF.Ln, bias=1.0)
            p = nextA()
            k.mm(p, [(p[:, :], c.mrev[:, :], l_[:, :], True, True)])
            ed = edec[j % 2]
            k.act(ed[:, :], p[:, :], AF.Exp, scale=-1.0 / 16.0)
            for h in range(4):
                k.mm(ptot, [(ptot[:, h * 8 + 2 * j:h * 8 + 2 * j + 2], l_[:, h * 128:(h + 1) * 128], c.mch[:, :],
                             True, True)])
            p = nextA()
            k.mm(p, [(p[:, :], hbf[:, kc, j * 128:(j + 1) * 128], w_in[:, kc, 512:1024], kc == 0, kc == 7)
                     for kc in range(8)])
            k.tt("dve", kdec[j][:, :], p[:, :], ed[:, :], ALU.mult)
        k.act(dcy[:, :, :], ptot[:, 0:32], AF.Exp, scale=-1.0 / 16.0)
        n_u = 0
        for cc in range(8):
            j, hf = cc // 2, cc % 2
            for h in range(4):
                pu_ = pU[h % 2]
                k.mm(pu_, [(pu_[:, (h // 2) * 256:(h // 2) * 256 + 256], kdec[j][64 * hf:64 * hf + 64, h * 128:(h + 1) * 128],
                            vbf[j][64 * hf:64 * hf + 64, h * 256:(h + 1) * 256], True, True)])
            for h in range(4):
                pu_ = pU[h % 2]
                k.stt(S[h][:, :], S[h][:, :], dcy[:, h, cc:cc + 1], pu_[:, (h // 2) * 256:(h // 2) * 256 + 256], ALU.mult, ALU.add)
                k.copy("act", Sb[h][cc % 2][:, :], S[h][:, :])
            for h in range(4):
                sb_ = Sb[h][cc % 2]
                po_ = pO[h % 2]
                k.mm(po_, [(po_[:, (h // 2) * 128 + dv * 64:(h // 2) * 128 + (dv + 1) * 64], sb_[:, dv * 128:(dv + 1) * 128],
                            qT[h][:, cc * 64:(cc + 1) * 64], True, True) for dv in range(2)])
            for h in range(4):
                po_ = pO[h % 2]
                k.copy("act", oT[h][:, :, cc * 64:(cc + 1) * 64],
                       po_.v(po_.t[:, (h // 2) * 128:(h // 2) * 128 + 128].rearrange("p (d t) -> p d t", d=2)))
        for h in range(4):
            for dv in range(2):
                s_ = sq[dv]
                k.act(s_[:, :], oT[h][:, dv, :], AF.Square)
                k.mm(pst, [(pst[:, :], c.ones[:, 0:128], s_[:, :], dv == 0, dv == 1)])
            k.ts("dve", rstd[:, :], pst[:, :], 1.0 / 256.0, EPS, ALU.mult, ALU.add)
            k.act(rstd[:, :], rstd[:, :], AF.Sqrt)
            k.op("dve", "reciprocal", rstd[:, :], [rstd[:, :]])
            for dv in range(2):
                t_ = tmp[dv]
                k.stt(t_[:, :], oT[h][:, dv, :], g_on[:, dv:dv + 1], rstd[:, :], ALU.mult, ALU.mult)
                k.tt("dve", onb[:, h * 2 + dv, :], t_[:, :], gs[h * 2 + dv][:, :], ALU.mult)
        for n in range(8):
            p = nextA()
            k.mm(p, [(p[:, :], w_out[:, kc, n * 128:(n + 1) * 128], onb[:, kc, :], kc == 0, kc == 7)
                     for kc in range(8)])
            k.stt(x32[n][:, :], p[:, :], c.mod[:, layer, 16 + n:17 + n], x32[n][:, :], ALU.mult, ALU.add)
        store_x_tile(c, "sp", x32, xout, t0, T)
    k.stage_end()


import math
PI = math.pi


def sin_turns(k, out, x, phase, y, yi, m):
    k.ts("dve", y, x, 1.0 / (2.0 * PI), phase, ALU.mult, ALU.add)
    k.copy("dve", yi, y)
    k.copy("dve", m, yi)
    k.tt("dve", y, y, m, ALU.subtract)
    k.ts("dve", m, y, 0.5, None, ALU.is_gt)
    k.tt("dve", y, y, m, ALU.subtract)
    k.ts("dve", m, y, -0.5, None, ALU.is_lt)
    k.tt("dve", y, y, m, ALU.add)
    k.act(out, y, AF.Sin, scale=2.0 * PI)


def s5_stage(c, layer, xin, xout, W):
    k = c.k
    L = c.L
    T = 512
    TS = 128
    NB = 32
    PE_ = "dve"
    k.stage_begin()
    w_in = k.sb("w_in", [128, 8, 1024], BF16)
    w_out = k.sb("w_out", [128, 8, 2048], BF16)
    Bbr = k.sb("Bbr", [128, NB, 128], BF16)
    Bbi = k.sb("Bbi", [128, NB, 128], BF16)
    Cbr = k.sb("Cbr", [128, NB, 128], BF16)
    Cbi = k.sb("Cbi", [128, NB, 128], BF16)
    cost = k.sb("cost", [128, NB, TS], F32)
    sint = k.sb("sint", [128, NB, TS], F32)
    rcol = k.sb("rcol", [128, NB], F32)
    dl = k.sb("dl", [128, 8], F32)
    sp2 = k.sb("sp2", [128, NB, 2], F32)
    Rt1 = k.sb("Rt1", [128, NB, 2], F32)
    Rt2 = k.sb("Rt2", [128, NB, 2], F32)
    k.dma("pool", w_in[:, :, :], W["w_in"].v(W["w_in"].t.rearrange("(kc p) n -> p kc n", p=128)))
    k.dma("pool", w_out[:, :, :], W["w_out"].v(W["w_out"].t.rearrange("(kc p) n -> p kc n", p=128)))
    k.dma("sp", dl[:, :], W["d_l"].v(W["d_l"].t))
    k.memset("dve", sp2[:, :, :], 0.0)

    PH = 9
    k.stage_begin()
    Q = 1024
    nm = ["are", "aim", "ldt", "bre", "bim", "t0", "t1", "t2", "t3", "t4", "t5", "t6"]
    A = {n: k.sb(n, [128, Q], F32) for n in nm}
    A["y"] = k.sb("y", [128, Q], F32)
    A["yi"] = k.sb("yi", [128, Q], mybir.dt.int32)
    for q in range(4 if PH >= 1 else 0):
        sl = slice(q * Q, (q + 1) * Q)
        for n, src in (("are", "Ablk_re"), ("aim", "Ablk_im"), ("ldt", "Lblk"), ("bre", "Bblk_re"), ("bim", "Bblk_im")):
            k.dma("sp", A[n][:, :], W[src].v(W[src].t[:, sl]))
        dt_, ar, ai, er, sn, cs, t6 = A["t0"], A["t1"], A["t2"], A["t3"], A["t4"], A["t5"], A["t6"]
        k.act(dt_[:, :], A["ldt"][:, :], AF.Exp)
        k.tt("dve", ar[:, :], A["are"][:, :], dt_[:, :], ALU.mult)
        k.tt("dve", ai[:, :], A["aim"][:, :], dt_[:, :], ALU.mult)
        k.act(er[:, :], ar[:, :], AF.Exp)
        sin_turns(k, sn[:, :], ai[:, :], 0.0, A["y"][:, :], A["yi"][:, :], t6[:, :])
        sin_turns(k, cs[:, :], ai[:, :], 0.25, A["y"][:, :], A["yi"][:, :], t6[:, :])
        nr, ni = ar, ai
        k.tt("dve", nr[:, :], er[:, :], cs[:, :], ALU.mult)
        k.ts("dve", nr[:, :], nr[:, :], -1.0, None, ALU.add)
        k.tt("dve", ni[:, :], er[:, :], sn[:, :], ALU.mult)
        den = er
        k.tt("dve", den[:, :], A["are"][:, :], A["are"][:, :], ALU.mult)
        k.tt("dve", t6[:, :], A["aim"][:, :], A["aim"][:, :], ALU.mult)
        k.tt("dve", den[:, :], den[:, :], t6[:, :], ALU.add)
        k.op("dve", "reciprocal", den[:, :], [den[:, :]])
        cr, ci = sn, cs
        k.tt("dve", cr[:, :], nr[:, :], A["are"][:, :], ALU.mult)
        k.tt("dve", t6[:, :], ni[:, :], A["aim"][:, :], ALU.mult)
        k.tt("dve", cr[:, :], cr[:, :], t6[:, :], ALU.add)
        k.tt("dve", cr[:, :], cr[:, :], den[:, :], ALU.mult)
        k.tt("dve", ci[:, :], ni[:, :], A["are"][:, :], ALU.mult)
        k.tt("dve", t6[:, :], nr[:, :], A["aim"][:, :], ALU.mult)
        k.tt("dve", ci[:, :], ci[:, :], t6[:, :], ALU.subtract)
        k.tt("dve", ci[:, :], ci[:, :], den[:, :], ALU.mult)
        x1, x2 = ar, ai
        k.tt("dve", x1[:, :], cr[:, :], A["bre"][:, :], ALU.mult)
        k.tt("dve", x2[:, :], ci[:, :], A["bim"][:, :], ALU.mult)
        k.tt("dve", Bbr.v(Bbr.t[:, q * 8:(q + 1) * 8, :].rearrange("p b m -> p (b m)")), x1[:, :], x2[:, :], ALU.subtract)
        k.tt("dve", x1[:, :], cr[:, :], A["bim"][:, :], ALU.mult)
        k.tt("dve", x2[:, :], ci[:, :], A["bre"][:, :], ALU.mult)
        k.tt("dve", Bbi.v(Bbi.t[:, q * 8:(q + 1) * 8, :].rearrange("p b m -> p (b m)")), x1[:, :], x2[:, :], ALU.add)
        k.dma("sp", A["bre"][:, :], W["Cblk_re"].v(W["Cblk_re"].t[:, sl]))
        k.dma("sp", A["bim"][:, :], W["Cblk_im"].v(W["Cblk_im"].t[:, sl]))
        k.copy("dve", Cbr.v(Cbr.t[:, q * 8:(q + 1) * 8, :].rearrange("p b m -> p (b m)")), A["bre"][:, :])
        k.ts("dve", Cbi.v(Cbi.t[:, q * 8:(q + 1) * 8, :].rearrange("p b m -> p (b m)")), A["bim"][:, :], -1.0, None, ALU.mult)
    k.stage_end()
    k.stage_begin()
    acr = k.sb("acr", [128, NB], F32)
    aci = k.sb("aci", [128, NB], F32)
    lc = k.sb("lc", [128, NB], F32)
    th = k.sb("th", [128, NB], F32)
    tI_i = k.sb("tIi", [128, TS], mybir.dt.int32)
    tI = k.sb("tI", [128, TS], F32)
    ang = k.sb("ang", [128, NB, TS], F32)
    yy = k.sb("yy", [128, NB * TS], F32)
    yyi = k.sb("yyi", [128, NB * TS], mybir.dt.int32)
    mm_ = k.sb("mm_", [128, NB * TS], F32)
    if PH < 2:
        k.stage_end()
        k.stage_end()
        return
    k.dma("sp", acr[:, :], W["Acol_re"].v(W["Acol_re"].t))
    k.dma("sp", aci[:, :], W["Acol_im"].v(W["Acol_im"].t))
    k.dma("sp", lc[:, :], W["Lcol"].v(W["Lcol"].t))
    k.act(lc[:, :], lc[:, :], AF.Exp)
    k.tt("dve", th[:, :], aci[:, :], lc[:, :], ALU.mult)
    k.tt("dve", acr[:, :], acr[:, :], lc[:, :], ALU.mult)
    k.act(rcol[:, :], acr[:, :], AF.Exp)
    k.op("pool", "iota", tI_i[:, :], [], pattern=[[1, TS]], base=1, channel_multiplier=0)
    k.copy("dve", tI[:, :], tI_i[:, :])
    for bi in range(NB):
        k.ts("dve", ang[:, bi, :], tI[:, :], th[:, bi:bi + 1], None, ALU.mult)
    fl = lambda b: b.v(b.t[:, :, :].rearrange("p b t -> p (b t)"))
    sin_turns(k, fl(sint), fl(ang), 0.0, yy[:, :], yyi[:, :], mm_[:, :])
    sin_turns(k, fl(cost), fl(ang), 0.25, yy[:, :], yyi[:, :], mm_[:, :])
    k.copy("dve", Rt1[:, :, 0], cost[:, :, TS - 1])
    k.copy("dve", Rt1[:, :, 1], sint[:, :, TS - 1])
    k.ts("dve", Rt2[:, :, 0], sint[:, :, TS - 1], -1.0, None, ALU.mult)
    k.copy("dve", Rt2[:, :, 1], cost[:, :, TS - 1])
    k.stage_end()
    if PH < 3:
        k.stage_end()
        return

    x32 = [k.sb("x32", [128, T], F32) for _ in range(8)]
    hbf = k.sb("hbf", [128, 8, T], BF16)
    ubf = k.sb("ubf", [128, 8, T], BF16)
    ybf = k.sb("ybf", [128, 8, T], BF16)
    sq = [k.sb("sq", [128, T], F32) for _ in range(2)]
    rstd = k.sb("rstd", [128, T], F32)
    bre2 = [k.sb("bre", [128, T], F32) for _ in range(2)]
    bim2 = [k.sb("bim", [128, T], F32) for _ in range(2)]
    ta = k.sb("ta", [128, T], F32)
    tb = k.sb("tb", [128, T], F32)
    wre = k.sb("wre", [128, T], F32)
    wim = k.sb("wim", [128, T], F32)
    zre = k.sb("zre", [128, T], F32)
    zim = k.sb("zim", [128, T], F32)
    rfull = k.sb("rfull", [128, TS], F32)
    car = k.sb("car", [128, 2], F32)
    ctmp = k.sb("ctmp", [128, 2], F32)
    sbr = [[k.sb("sbr", [128, T], BF16) for _ in range(4)] for _ in range(2)]
    sbi = [[k.sb("sbi", [128, T], BF16) for _ in range(4)] for _ in range(2)]
    pA = [k.ps("pA", [128, 512]) for _ in range(2)]
    pR = [k.ps("pR", [128, 512]) for _ in range(2)]
    pI = [k.ps("pI", [128, 512]) for _ in range(2)]
    pY = k.ps("pY", [128, 512])
    pst = k.ps("pst", [128, 512])
    pai = [0]

    def nextA():
        pai[0] += 1
        return pA[pai[0] % 2]

    TB = True

    def v3(b):
        return b.v(b.t[:, :].rearrange("p (s t) -> p s t", s=T // TS))

    def tb3(tab, bi):
        return tab.v(tab.t[:, bi, :].unsqueeze(1).to_broadcast([128, T // TS, TS]))

    def ttab(e, out, in0, tab, bi, op):
        if TB:
            k.tt(e, v3(out), v3(in0), tb3(tab, bi), op)
        else:
            for s_ in range(T // TS):
                sl = slice(s_ * TS, (s_ + 1) * TS)
                k.tt(e, out[:, sl], in0[:, sl], tab[:, bi, :], op)

    nblk = 0
    for it in range(L // T):
        t0 = it * T
        load_x_tile(c, "sp", x32, xin, t0, T)
        rmsnorm_mod(c, x32, T, layer, 0, sq, rstd, pst, out_bf=hbf, off=0, cp_eng="dve")
        for ch in range(8):
            p = nextA()
            k.mm(p, [(p[:, :], w_in[:, kc, ch * 128:(ch + 1) * 128], hbf[:, kc, :], kc == 0, kc == 7)
                     for kc in range(8)])
            k.copy("act", x32[ch][:, :], p[:, :])
            k.copy("dve", ubf[:, ch, :], x32[ch][:, :])
        for j in range(8):
            par = j % 2
            for m in range(4):
                bi = j * 4 + m
                pr, pi_ = pR[nblk % 2], pI[nblk % 2]
                bre, bim = bre2[nblk % 2], bim2[nblk % 2]
                nblk += 1
                k.mm(pr, [(pr[:, :], Bbr[:, bi, :], ubf[:, j, :], True, True)])
                k.mm(pi_, [(pi_[:, :], Bbi[:, bi, :], ubf[:, j, :], True, True)])
                k.copy("act", bre[:, :], pr[:, :])
                k.copy("act", bim[:, :], pi_[:, :])
                ttab("dve", ta, bre, cost, bi, ALU.mult)
                ttab(PE_, tb, bim, sint, bi, ALU.mult)
                k.tt("dve", wre[:, :], ta[:, :], tb[:, :], ALU.add)
                ttab(PE_, ta, bim, cost, bi, ALU.mult)
                ttab("dve", tb, bre, sint, bi, ALU.mult)
                k.tt(PE_, wim[:, :], ta[:, :], tb[:, :], ALU.subtract)
                if False:
                    rb = rcol.v(rcol.t[:, bi:bi + 1].to_broadcast([128, TS]))
                else:
                    k.ts("dve", rfull[:, :], c.ones[:, :TS], rcol[:, bi:bi + 1], None, ALU.mult)
                    rb = rfull[:, :]
                for s_ in range(T // TS):
                    sl = slice(s_ * TS, (s_ + 1) * TS)
                    i_r = sp2[:, bi, 0:1] if s_ == 0 else car[:, 0:1]
                    i_i = sp2[:, bi, 1:2] if s_ == 0 else car[:, 1:2]
                    k.op("dve", "tensor_tensor_scan", zre[:, sl], [rb, wre[:, sl], i_r, ALU.mult, ALU.add])
                    k.op("dve", "tensor_tensor_scan", zim[:, sl], [rb, wim[:, sl], i_i, ALU.mult, ALU.add])
                    last = s_ == T // TS - 1
                    e_ = (s_ + 1) * TS - 1
                    o_ = sp2[:, bi, :] if last else car[:, :]
                    k.ts("dve", ctmp[:, :], Rt1[:, bi, :], zre[:, e_:e_ + 1], None, ALU.mult)
                    k.stt(o_, Rt2[:, bi, :], zim[:, e_:e_ + 1], ctmp[:, :], ALU.mult, ALU.add)
                ttab("dve", ta, zre, cost, bi, ALU.mult)
                ttab(PE_, tb, zim, sint, bi, ALU.mult)
                k.tt("dve", sbr[par][m][:, :], ta[:, :], tb[:, :], ALU.subtract)
                ttab(PE_, ta, zre, sint, bi, ALU.mult)
                ttab("dve", tb, zim, cost, bi, ALU.mult)
                k.tt(PE_, sbi[par][m][:, :], ta[:, :], tb[:, :], ALU.add)
            mms = []
            for m in range(4):
                bi = j * 4 + m
                mms.append((pY[:, :], Cbr[:, bi, :], sbr[par][m][:, :], m == 0, False))
                mms.append((pY[:, :], Cbi[:, bi, :], sbi[par][m][:, :], False, m == 3))
            k.mm(pY, mms)
            k.stt(ta[:, :], x32[j][:, :], dl[:, j:j + 1], pY[:, :], ALU.mult, ALU.add)
            k.act(ybf[:, j, :], ta[:, :], AF.Gelu)
        load_x_tile(c, "sp", x32, xin, t0, T)
        for n in range(8):
            pa, pb = pR[n % 2], pI[n % 2]
            k.mm(pa, [(pa[:, :], w_out[:, kc, n * 128:(n + 1) * 128], ybf[:, kc, :], kc == 0, kc == 7)
                      for kc in range(8)])
            k.mm(pb, [(pb[:, :], w_out[:, kc, 1024 + n * 128:1024 + (n + 1) * 128], ybf[:, kc, :], kc == 0, kc == 7)
                      for kc in range(8)])
            k.act(tb[:, :], pb[:, :], AF.Sigmoid)
            k.tt("dve", ta[:, :], pa[:, :], tb[:, :], ALU.mult)
            k.stt(x32[n][:, :], ta[:, :], c.mod[:, layer, 16 + n:17 + n], x32[n][:, :], ALU.mult, ALU.add)
        store_x_tile(c, "sp", x32, xout, t0, T)
    k.stage_end()


def s5_host_layout(a_re, a_im, log_dt, b_re, b_im, c_re, c_im, d):
    NB = 32
    out = {}
    Ablk_re = np.zeros((128, NB, 128), np.float32); Ablk_im = np.zeros_like(Ablk_re); Lblk = np.zeros_like(Ablk_re)
    Bblk_re = np.zeros_like(Ablk_re); Bblk_im = np.zeros_like(Ablk_re)
    Cblk_re = np.zeros_like(Ablk_re); Cblk_im = np.zeros_like(Ablk_re)
    Acol_re = np.zeros((128, NB), np.float32); Acol_im = np.zeros_like(Acol_re); Lcol = np.zeros_like(Acol_re)
    for j in range(8):
        for m in range(4):
            bi = j * 4 + m
            for gg in range(2):
                g = 8 * j + 2 * m + gg
                gl = 2 * m + gg
                cs = slice(gg * 64, (gg + 1) * 64)
                Ablk_re[:, bi, cs] = a_re[g][None, :]
                Ablk_im[:, bi, cs] = a_im[g][None, :]
                Lblk[:, bi, cs] = log_dt[g]
                Bblk_re[gl * 16:(gl + 1) * 16, bi, cs] = b_re[g].T
                Bblk_im[gl * 16:(gl + 1) * 16, bi, cs] = b_im[g].T
                Cblk_re[cs, bi, gl * 16:(gl + 1) * 16] = c_re[g].T
                Cblk_im[cs, bi, gl * 16:(gl + 1) * 16] = c_im[g].T
                Acol_re[cs, bi] = a_re[g]
                Acol_im[cs, bi] = a_im[g]
                Lcol[cs, bi] = log_dt[g]
    f = lambda a: np.ascontiguousarray(a.reshape(128, NB * 128))
    return dict(Ablk_re=f(Ablk_re), Ablk_im=f(Ablk_im), Lblk=f(Lblk), Bblk_re=f(Bblk_re), Bblk_im=f(Bblk_im),
                Cblk_re=f(Cblk_re), Cblk_im=f(Cblk_im), Acol_re=Acol_re, Acol_im=Acol_im, Lcol=Lcol,
                d_l=np.ascontiguousarray(d.reshape(8, 128).T))


def mlstm_stage(c, layer, xin, xout, W):
    k = c.k
    L = c.L
    T = 256
    NC_ = T // 64
    NS = T // 128
    DH = 512
    k.stage_begin()
    wbuf = [k.sb("wbuf", [128, 8, 1024], BF16) for _ in range(3)]
    bdq = k.sb("bdq", [128, 16, 128], BF16)
    bdk = k.sb("bdk", [128, 16, 128], BF16)
    bdv = k.sb("bdv", [128, 16, 128], BF16)
    weffc = k.sb("weffc", [128, 16, 8], BF16)
    weffm = k.sb("weffm", [128, 16, 8], BF16)
    convl = k.sb("convl", [128, 16, 5], F32)
    bif = k.sb("bif", [1, 8], F32)
    skipl = k.sb("skipl", [128, 16], F32)
    gnl = k.sb("gnl", [128, 16], F32)
    cmat = [[k.sb("cmat", [128, 512], F32) for _ in range(4)] for _ in range(4)]
    cb = [[k.sb("cb", [128, 512], BF16) for _ in range(4)] for _ in range(4)]
    nvec = [k.sb("nvec", [128, 4], F32) for _ in range(4)]
    nvb = [k.sb("nvb", [128, 4], BF16) for _ in range(4)]
    halo = k.sb("halo", [128, 16, 3], F32)
    mcar = k.sb("mcar", [4, 1], F32)
    cmask = k.sb("cmask", [4, T], F32)
    sel = k.sb("sel", [4, 4, 128], F32)
    onesb = k.sb("onesb", [128, 1], BF16)
    for n_, src in (("bdq", bdq), ("bdk", bdk), ("bdv", bdv)):
        k.dma("pool", src[:, :, :], W[n_].v(W[n_].t))
    k.dma("sp", convl[:, :, :], W["conv_l"].v(W["conv_l"].t))
    k.dma("sp", bif[:, :], W["b_if"].v(W["b_if"].t))
    k.dma("sp", skipl[:, :], W["skip_l"].v(W["skip_l"].t))
    k.dma("sp", gnl[:, :], W["gnorm_l"].v(W["gnorm_l"].t))
    for h in range(4):
        for kc in range(4):
            k.memset("dve", cmat[h][kc][:, :], 0.0)
        k.memset("dve", nvec[h][:, :], 0.0)
        k.ts("dve", sel[:, h, :], c.ones[0:4, 0:128], c.ident[0:4, h:h + 1], None, ALU.mult)
    k.memset("dve", halo[:, :, :], 0.0)
    k.memset("dve", mcar[:, :], 0.0)
    k.memset("dve", cmask[:, :], 1.0)
    k.memset("dve", cmask.v(cmask.t[:, :].rearrange("p (c t) -> p c t", t=64)[:, :, 0:1]), 0.0)
    k.memset("dve", onesb[:, :], 1.0)
    k.stage_begin()
    bT = {n_: k.sb(n_, [128, 16, 128], F32) for n_ in ("bdqT", "bdkT", "bdvT")}
    wif = k.sb("wif", [128, 48, 8], F32)
    pw = k.ps("pw", [128, 512])
    for n_ in bT:
        k.dma("sp", bT[n_][:, :, :], W[n_].v(W[n_].t))
    k.dma("sp", wif[:, :, :], W["wif_l"].v(W["wif_l"].t))
    for ch in range(16):
        k.mm(pw, [(pw[:, ch * 8:ch * 8 + 8], bT["bdqT"][:, ch, :], wif[:, ch, :], True, False),
                  (pw[:, ch * 8:ch * 8 + 8], bT["bdkT"][:, ch, :], wif[:, 16 + ch, :], False, True)])
    k.copy("dve", weffc.v(weffc.t[:, :, :].rearrange("p c g -> p (c g)")), pw[:, 0:128])
    for ch in range(16):
        k.mm(pw, [(pw[:, 128 + ch * 8:128 + ch * 8 + 8], bT["bdvT"][:, ch, :], wif[:, 32 + ch, :], True, True)])
    k.copy("dve", weffm.v(weffm.t[:, :, :].rearrange("p c g -> p (c g)")), pw[:, 128:256])
    k.stage_end()

    x32 = [k.sb("x32", [128, T], F32) for _ in range(8)]
    hbf = k.sb("hbf", [128, 8, T], BF16)
    sq = [k.sb("sq", [128, T], F32) for _ in range(2)]
    rstd = k.sb("rstd", [128, T], F32)
    xmb = k.sb("xmb", [128, 16, T], BF16)
    xcb = k.sb("xcb", [128, 16, T], BF16)
    zsb = k.sb("zsb", [128, 16, T], BF16)
    qTb = k.sb("qTb", [128, 16, T], BF16)
    kw = [k.sb("kw", [128, 2048], BF16) for _ in range(NS)]
    vt = [k.sb("vt", [128, 2048], BF16) for _ in range(NS)]
    xt = [k.sb("xt", [128, T + 3], F32) for _ in range(2)]
    ca = [k.sb("ca", [128, T], F32) for _ in range(2)]
    gi = k.sb("gi", [4, T], F32)
    nl = k.sb("nl", [4, T], F32)
    cum = k.sb("cum", [4, T], F32)
    lw = k.sb("lw", [4, T], F32)
    wT = k.sb("wT", [4, T], F32)
    enx = k.sb("enx", [4, T], F32)
    g4 = {n_: k.sb(n_, [4, NC_], F32) for n_ in ("ntot", "mx", "mnew", "mp", "dec", "negm", "en")}
    wtm = [k.sb("wtm", [128, 4], F32) for _ in range(NS)]
    entm = [k.sb("entm", [64, 4], F32) for _ in range(NC_)]
    decb = k.sb("decb", [128, 4, NC_], F32)
    hc = [k.sb("hc", [64, 512], F32) for _ in range(2)]
    hnb = [k.sb("hnb", [64, 512], BF16) for _ in range(2)]
    st6 = k.sb("st6", [64, 6], F32)
    mv = k.sb("mv", [64, 2], F32)
    den4 = [k.sb("den", [64, 1], F32) for _ in range(4)]
    st64 = [k.sb("st64", [64, 6], F32) for _ in range(2)]
    mv4 = [k.sb("mv4", [64, 2], F32) for _ in range(2)]
    tmp256 = [k.sb("tmp256", [128, 256], F32) for _ in range(2)]
    pA = [k.ps("pA", [128, 512]) for _ in range(2)]
    pU = [k.ps("pU", [128, 512]) for _ in range(2)]
    pN2 = [k.ps("pN", [128, 512]) for _ in range(2)]
    pM = k.ps("pM", [128, 512])
    pT = k.ps("pT", [128, 1024], BF16)
    pst = pA[0]
    pai = [0]

    def nextA():
        pai[0] += 1
        return pA[pai[0] % 2]

    wq = [0]

    def load_piece(src, rows=None, cols=None):
        b = wbuf[wq[0] % 3]
        wq[0] += 1
        t = W[src].t
        if cols is not None:
            ap = t[:, cols[0]:cols[1]].rearrange("(kc p) n -> p kc n", p=128)
        else:
            ap = t[rows[0]:rows[1], :].rearrange("(kc p) n -> p kc n", p=128)
        k.dma("pool", b[:, :, :], W[src].v(ap))
        return b

    for it in range(L // T):
        t0 = it * T
        load_x_tile(c, "sp", x32, xin, t0, T)
        rmsnorm_mod(c, x32, T, layer, 0, sq, rstd, pst, out_bf=hbf, off=0, cp_eng="dve")
        load_x_tile(c, "sp", x32, xin, t0, T)
        for pc in range(4):
            wb = load_piece("w_up", cols=(pc * 1024, (pc + 1) * 1024))
            for cc in range(8):
                ch = (pc % 2) * 8 + cc
                p = nextA()
                k.mm(p, [(p[:, :T], wb[:, kc, cc * 128:(cc + 1) * 128], hbf[:, kc, :], kc == 0, kc == 7)
                         for kc in range(8)])
                if pc < 2:
                    x_ = xt[ch % 2]
                    k.copy("dve", x_[:, 0:3], halo[:, ch, :])
                    k.copy("act", x_[:, 3:T + 3], p[:, :T])
                    k.copy("dve", halo[:, ch, :], x_[:, T:T + 3])
                    k.copy("dve", xmb[:, ch, :], x_[:, 3:T + 3])
                    a_ = ca[ch % 2]
                    k.ts("dve", a_[:, :], x_[:, 3:T + 3], convl[:, ch, 3:4], convl[:, ch, 4:5], ALU.mult, ALU.add)
                    for j in range(3):
                        k.stt(a_[:, :], x_[:, j:j + T], convl[:, ch, j:j + 1], a_[:, :], ALU.mult, ALU.add)
                    k.act(xcb[:, ch, :], a_[:, :], AF.Silu)
                else:
                    k.act(zsb[:, ch, :], p[:, :T], AF.Silu)
        for gsel, dst in ((0, gi), (1, nl)):
            p = pM
            mms = []
            for ch in range(16):
                mms.append((p[:4, :T], weffc[:, ch, gsel * 4:gsel * 4 + 4], xcb[:, ch, :], ch == 0, False))
                mms.append((p[:4, :T], weffm[:, ch, gsel * 4:gsel * 4 + 4], xmb[:, ch, :], False, False))
            mms.append((p[:4, :T], bif[0:1, gsel * 4:gsel * 4 + 4], c.ones[0:1, :T], False, True))
            k.mm(p, mms)
            if gsel == 0:
                k.copy("dve", gi[:, :], p[:4, :T])
            else:
                k.act(nl[:, :], p[:4, :T], AF.Exp, scale=-1.0)
                k.act(nl[:, :], nl[:, :], AF.Ln, bias=1.0)
        k.op("dve", "tensor_tensor_scan", cum[:, :], [cmask[:, :], nl[:, :], 0.0, ALU.mult, ALU.add])
        cum3 = cum.v(cum.t[:, :].rearrange("p (c t) -> p c t", t=64))
        k.ts("dve", g4["ntot"][:, :], cum.v(cum.t[:, :].rearrange("p (c t) -> p c t", t=64)[:, :, 63]), -1.0, None, ALU.mult)
        for cc in range(NC_):
            sl = slice(cc * 64, (cc + 1) * 64)
            k.stt(lw[:, sl], cum[:, sl], g4["ntot"][:, cc:cc + 1], gi[:, sl], ALU.add, ALU.add)
        k.op("dve", "tensor_reduce", g4["mx"][:, :], [lw.v(lw.t[:, :].rearrange("p (c t) -> p c t", t=64))],
             axis=AX.X, op=ALU.max)
        k.op("dve", "tensor_tensor_scan", g4["mnew"][:, :], [g4["ntot"][:, :], g4["mx"][:, :], mcar[:, 0:1], ALU.add, ALU.max])
        k.copy("dve", g4["mp"][:, 0:1], mcar[:, :])
        k.copy("dve", g4["mp"][:, 1:NC_], g4["mnew"][:, 0:NC_ - 1])
        k.copy("dve", mcar[:, :], g4["mnew"][:, NC_ - 1:NC_])
        k.tt("dve", g4["dec"][:, :], g4["ntot"][:, :], g4["mp"][:, :], ALU.add)
        k.tt("dve", g4["dec"][:, :], g4["dec"][:, :], g4["mnew"][:, :], ALU.subtract)
        k.act(g4["dec"][:, :], g4["dec"][:, :], AF.Exp)
        k.ts("dve", g4["negm"][:, :], g4["mnew"][:, :], -1.0, None, ALU.mult)
        k.act(g4["en"][:, :], g4["negm"][:, :], AF.Exp)
        for cc in range(NC_):
            sl = slice(cc * 64, (cc + 1) * 64)
            k.act(wT[:, sl], lw[:, sl], AF.Exp, bias=g4["negm"][:, cc:cc + 1], scale=1.0)
            k.ts("dve", enx[:, sl], c.ones[0:4, 0:64], g4["en"][:, cc:cc + 1], None, ALU.mult)
        for s_ in range(NS):
            k.mm(pM, [(pM[:, 32 + 4 * s_:36 + 4 * s_], wT[:, s_ * 128:(s_ + 1) * 128], c.ident[0:4, 0:4], True, True)],
                 transpose=True)
            k.ts("dve", wtm[s_][:, :], pM[:, 32 + 4 * s_:36 + 4 * s_], DH ** -0.5, None, ALU.mult)
        for cc in range(NC_):
            k.mm(pM, [(pM[:64, 64 + 4 * cc:68 + 4 * cc], enx[:, cc * 64:(cc + 1) * 64], c.ident[0:4, 0:4], True, True)],
                 transpose=True)
            k.copy("dve", entm[cc][:, :], pM[:64, 64 + 4 * cc:68 + 4 * cc])
        for h in range(4):
            k.mm(pM, [(pM[:, 128 + h * NC_:128 + (h + 1) * NC_], sel[:, h, :], g4["dec"][:, :], True, True)])
        k.copy("dve", decb.v(decb.t[:, :, :].rearrange("p h c -> p (h c)")), pM[:, 128:128 + 4 * NC_])
        for ch in range(16):
            p = nextA()
            k.mm(p, [(p[:, :T], bdq[:, ch, :], xcb[:, ch, :], True, True)])
            k.copy("act", qTb[:, ch, :], p[:, :T])
        for s_ in range(NS):
            for h in range(4):
                p = nextA()
                k.mm(p, [(p[:, kc * 128:(kc + 1) * 128], xcb[:, h * 4 + kc, s_ * 128:(s_ + 1) * 128], bdk[:, h * 4 + kc, :],
                          True, True) for kc in range(4)])
                k.ts("dve", kw[s_][:, h * 512:(h + 1) * 512], p[:, :], wtm[s_][:, h:h + 1], None, ALU.mult)
                p = nextA()
                k.mm(p, [(p[:, kc * 128:(kc + 1) * 128], xmb[:, h * 4 + kc, s_ * 128:(s_ + 1) * 128], bdv[:, h * 4 + kc, :],
                          True, True) for kc in range(4)])
                k.copy("act", vt[s_][:, h * 512:(h + 1) * 512], p[:, :])
        for ch in range(16):
            k.ts("dve", xcb[:, ch, :], xcb[:, ch, :], skipl[:, ch:ch + 1], None, ALU.mult)
        nu = 0
        for cc in range(NC_):
            s_, hf = cc // 2, cc % 2
            r0 = 64 * hf
            cs_ = slice(cc * 64, (cc + 1) * 64)
            for h in range(4):
                for kc in range(4):
                    pu_ = pU[nu % 2]
                    nu += 1
                    k.mm(pu_, [(pu_[:, :], kw[s_][r0:r0 + 64, h * 512 + kc * 128:h * 512 + (kc + 1) * 128],
                                vt[s_][r0:r0 + 64, h * 512:(h + 1) * 512], True, True)])
                    k.stt(cmat[h][kc][:, :], cmat[h][kc][:, :], decb[:, h, cc:cc + 1], pu_[:, :], ALU.mult, ALU.add)
                    k.copy("act", cb[h][kc][:, :], cmat[h][kc][:, :])
                k.mm(pM, [(pM[:, 16 + h * 4 + kc:17 + h * 4 + kc], kw[s_][r0:r0 + 64, h * 512 + kc * 128:h * 512 + (kc + 1) * 128],
                           onesb[r0:r0 + 64, 0:1], True, True) for kc in range(4)])
                k.stt(nvec[h][:, :], nvec[h][:, :], decb[:, h, cc:cc + 1], pM[:, 16 + h * 4:20 + h * 4], ALU.mult, ALU.add)
                k.copy("dve", nvb[h][:, :], nvec[h][:, :])
            for h in range(4):
                pn_ = pN2[h % 2]
                k.mm(pn_, [(pn_[:64, :], qTb[:, h * 4 + kc, cs_], cb[h][kc][:, :], kc == 0, kc == 3) for kc in range(4)])
                k.mm(pM, [(pM[:64, 8 + h:9 + h], qTb[:, h * 4 + kc, cs_], nvb[h][:, kc:kc + 1], kc == 0, kc == 3)
                          for kc in range(4)])
                den_ = den4[h]
                k.copy("dve", den_[:, :], pM[:64, 8 + h:9 + h])
                k.stt(den_[:, :], den_[:, :], -1.0, den_[:, :], ALU.mult, ALU.max)
                k.ts("dve", den_[:, :], den_[:, :], entm[cc][:, h:h + 1], None, ALU.max)
                k.op("dve", "reciprocal", den_[:, :], [den_[:, :]])
                hc_ = hc[h % 2]
                k.act(hc_[:, :], pn_[:64, :], AF.Copy, scale=den_[:, 0:1])
                st_, mv_ = st64[h % 2], mv4[h % 2]
                k.op("dve", "bn_stats", st_[:, :], [hc_[:, :]])
                k.op("dve", "bn_aggr", mv_[:, :], [st_[:, :]])
                k.ts("dve", mv_[:, 1:2], mv_[:, 1:2], EPS, None, ALU.add)
                k.act(mv_[:, 1:2], mv_[:, 1:2], AF.Sqrt)
                k.op("dve", "reciprocal", mv_[:, 1:2], [mv_[:, 1:2]])
                hb_ = hnb[h % 2]
                k.ts("dve", hb_[:, :], hc_[:, :], mv_[:, 0:1], mv_[:, 1:2], ALU.subtract, ALU.mult)
                po = (h % 2) * 512
                k.mm(pT, [(pT[:, po + kc * 64:po + (kc + 1) * 64], hb_[:, kc * 128:(kc + 1) * 128], c.identb[0:64, 0:64], True, True)
                          for kc in range(4)], transpose=True)
                t_ = tmp256[h % 2]
                t3 = t_.v(t_.t[:, :].rearrange("p (k t) -> p k t", k=4))
                p3 = pT.v(pT.t[:, po:po + 256].rearrange("p (k t) -> p k t", k=4))
                g3 = gnl.v(gnl.t[:, h * 4:h * 4 + 4].unsqueeze(2).to_broadcast([128, 4, 64]))
                k.tt("dve", t3, p3, g3, ALU.mult)
                k.tt("dve", t3, t3, xcb[:, h * 4:h * 4 + 4, cs_], ALU.add)
                k.tt("dve", zsb[:, h * 4:h * 4 + 4, cs_], t3, zsb[:, h * 4:h * 4 + 4, cs_], ALU.mult)
        wd0 = load_piece("w_down", rows=(0, 1024))
        wd1 = load_piece("w_down", rows=(1024, 2048))
        for n in range(8):
            p = nextA()
            mms = [(p[:, :T], wd0[:, kc, n * 128:(n + 1) * 128], zsb[:, kc, :], kc == 0, False) for kc in range(8)]
            mms += [(p[:, :T], wd1[:, kc, n * 128:(n + 1) * 128], zsb[:, 8 + kc, :], False, kc == 7) for kc in range(8)]
            k.mm(p, mms)
            k.stt(x32[n][:, :], p[:, :T], c.mod[:, layer, 16 + n:17 + n], x32[n][:, :], ALU.mult, ALU.add)
        store_x_tile(c, "sp", x32, xout, t0, T)
    k.stage_end()


def mlstm_host_layout(conv_w, conv_b, w_q, w_k, w_v, w_if, skip, g_norm):
    def bd(w, transpose):
        o = np.zeros((128, 16, 128), np.float32)
        for ch in range(16):
            for b in range(32):
                blk = w[ch * 32 + b]
                sl = slice(b * 4, b * 4 + 4)
                o[sl, ch, sl] = blk.T if transpose else blk
        return o
    conv_l = np.zeros((128, 16, 5), np.float32)
    conv_l[:, :, 0:4] = conv_w.reshape(4, 16, 128).transpose(2, 1, 0)
    conv_l[:, :, 4] = conv_b.reshape(16, 128).T
    return dict(bdq=bd(w_q, False), bdk=bd(w_k, False), bdv=bd(w_v, False),
                bdqT=bd(w_q, True), bdkT=bd(w_k, True), bdvT=bd(w_v, True),
                conv_l=conv_l,
                wif_l=np.ascontiguousarray(w_if.reshape(48, 128, 8).transpose(1, 0, 2)),
                skip_l=np.ascontiguousarray(skip.reshape(16, 128).T),
                gnorm_l=np.ascontiguousarray(g_norm.reshape(16, 128).T))


I32 = mybir.dt.int32


def moe_sparse_stage(c, layer, xin, xout, W, S, ne=NE, eoff=0):
    k = c.k
    L = c.L
    NBLK = L // 128
    NG = (4 * L + ne * 511) // 512
    NSLOT = NG * 512
    k.stage_begin()
    slots_all = k.sb("slots_all", [128, NBLK, 4], I32)
    egrp = k.sb("egrp", [128, NG], F32)
    eg256 = k.sb("eg256", [128, NG], F32)
    eg128 = k.sb("eg128", [128, NG], F32)
    iop = k.sb("iop", [128, 1], F32)
    iorow = k.sb("iorow", [128, 8], F32)
    ioi = k.sb("ioi", [128, 8], I32)
    k.op("pool", "iota", ioi[:, :], [], pattern=[[128, 8]], base=0, channel_multiplier=1)
    k.copy("dve", iorow[:, :], ioi[:, :])
    k.copy("dve", iop[:, :], ioi[:, 0:1])

    k.stage_begin()
    z = k.sb("z", [128, NSLOT * 2 // 128], I32)
    k.memset("dve", z[:, :], 0)
    k.dma("sp", S["rec"].v(S["rec"].t.rearrange("(p r) c -> p (r c)", p=128)), z[:, :])
    k.stage_end()

    k.stage_begin()
    x32s = [[k.sb("x32", [128, 512], F32) for _ in range(8)] for _ in range(2)]
    hbf = k.sb("hbf", [128, 8, 512], BF16)
    sq = [k.sb("sq", [128, 512], F32) for _ in range(2)]
    rstd = k.sb("rstd", [128, 512], F32)
    htm = [k.sb("htm", [128, 1024], BF16) for _ in range(2)]
    wr = k.sb("wr", [128, 8, 32], F32)
    br = k.sb("br", [1, 32], F32)
    lg_all = k.sb("lg_all", [128, NBLK, 32], F32)
    comb_all = k.sb("comb_all", [128, NBLK, 32], F32)
    pos_all = k.sb("pos_all", [128, NBLK, 32], F32)
    m4_all = k.sb("m4_all", [128, NBLK, 4], F32)
    runc = k.sb("runc", [128, 32], F32)
    lmat = k.sb("lmat", [128, 128], F32)
    m8 = k.sb("m8", [128, 8], F32)
    negm = k.sb("negm", [128, 1], F32)
    mask = k.sb("mask", [128, 32], F32)
    ex = k.sb("ex", [128, 32], F32)
    ssum = k.sb("ssum", [128, 1], F32)
    pm = [k.ps("pm", [128, 512]) for _ in range(2)]
    pp = k.ps("pp", [128, 512])
    ptb = [k.ps("ptb", [128, 1024], BF16) for _ in range(2)]
    k.dma("sp", wr[:, :, :], W["w_router"].v(W["w_router"].t.rearrange("(kc p) e -> p kc e", p=128)))
    k.dma("sp", br[:, :], W["b_router"].v(W["b_router"].t))
    k.memset("dve", runc[:, :], 0.0)
    k.op("pool", "affine_select", lmat[:, :], [c.ones[:, 0:128]], pattern=[[1, 128]],
         compare_op=ALU.is_gt, fill=0.0, base=0, channel_multiplier=-1)
    for it in range(L // 512):
        t0 = it * 512
        x32 = x32s[it % 2]
        load_x_tile(c, "sp", x32, xin, t0, 512)
        rmsnorm_mod(c, x32, 512, layer, 1, sq, rstd, pm[0], out_bf=hbf, off=0, cp_eng="dve")
        for j in range(4):
            blk = it * 4 + j
            p = pm[1]
            mms = [(p[:, :32], x32[ch][:, j * 128:(j + 1) * 128], wr[:, ch, :], ch == 0, False) for ch in range(8)]
            mms.append((p[:, :32], c.ones[0:1, 0:128], br[0:1, :], False, True))
            k.mm(p, mms)
            lg = lg_all[:, blk, :]
            k.copy("dve", lg, p[:, :32])
            k.op("dve", "max", m8[:, :], [lg])
            k.copy("dve", m4_all[:, blk, :], m8[:, 0:4])
            k.ts("dve", mask[:, :], lg, m8[:, 3:4], None, ALU.is_ge)
            k.ts("dve", negm[:, :], m8[:, 0:1], -1.0, None, ALU.mult)
            k.act(ex[:, :], lg, AF.Exp, bias=negm[:, 0:1], scale=1.0)
            k.stt(ex[:, :], ex[:, :], 1.0, mask[:, :], ALU.mult, ALU.mult, accum_out=ssum[:, :])
            k.op("dve", "reciprocal", ssum[:, :], [ssum[:, :]])
            k.ts("dve", comb_all[:, blk, :], ex[:, :], ssum[:, 0:1], None, ALU.mult)
            k.mm(pp, [(pp[:, 0:32], lmat[:, :], mask[:, :], True, True),
                      (pp[:, 32:64], c.ones[:, 0:128], mask[:, :], True, True)])
            k.tt("dve", pos_all[:, blk, :], pp[:, 0:32], runc[:, :], ALU.add)
            k.tt("dve", runc[:, :], runc[:, :], pp[:, 32:64], ALU.add)
            pt_ = ptb[blk % 2]
            k.mm(pt_, [(pt_[:, ch * 128:(ch + 1) * 128], hbf[:, ch, j * 128:(j + 1) * 128], c.identb[:, :], True, True)
                       for ch in range(8)], transpose=True)
            h_ = htm[blk % 2]
            k.copy("act", h_[:, :], pt_[:, :])
            reg = k.region()
            k.dma("sp", reg.v(S["h_tm"].t[blk * 128:(blk + 1) * 128, :]), h_[:, :])
    gsz = k.sb("gsz", [128, 32], F32)
    gszi = k.sb("gszi", [128, 32], I32)
    gtmp = k.sb("gtmp", [128, 32], F32)
    ginc = k.sb("ginc", [128, 32], F32)
    base = k.sb("base", [128, 32], F32)
    gidx = k.sb("gidx", [128, NG], F32)
    gidi = k.sb("gidi", [128, NG], I32)
    gcmp = k.sb("gcmp", [128, NG], F32)
    k.ts("dve", gtmp[:, :], runc[:, :], 511.0, 1.0 / 512.0, ALU.add, ALU.mult)
    k.copy("dve", gszi[:, :], gtmp[:, :])
    k.copy("dve", gsz[:, :], gszi[:, :])
    k.tt("dve", mask[:, :], gsz[:, :], gtmp[:, :], ALU.is_gt)
    k.tt("dve", gsz[:, :], gsz[:, :], mask[:, :], ALU.subtract)
    k.op("dve", "tensor_tensor_scan", ginc[:, :], [c.ones[:, 0:32], gsz[:, :], 0.0, ALU.mult, ALU.add])
    k.tt("dve", base[:, :], ginc[:, :], gsz[:, :], ALU.subtract)
    k.ts("dve", base[:, :], base[:, :], 512.0, None, ALU.mult)
    k.op("pool", "iota", gidi[:, :], [], pattern=[[1, NG]], base=0, channel_multiplier=0)
    k.copy("dve", gidx[:, :], gidi[:, :])
    k.memset("dve", egrp[:, :], 0.0)
    for e in range(ne):
        k.ts("dve", gcmp[:, :], gidx[:, :], ginc[:, e:e + 1], None, ALU.is_ge)
        k.tt("dve", egrp[:, :], egrp[:, :], gcmp[:, :], ALU.add)
    k.ts("dve", egrp[:, :], egrp[:, :], float(ne - 1), None, ALU.min)
    k.ts("dve", egrp[:, :], egrp[:, :], float(eoff), None, ALU.add)
    k.ts("dve", eg256[:, :], egrp[:, :], 256.0, None, ALU.mult)
    k.ts("dve", eg128[:, :], egrp[:, :], 128.0, None, ALU.mult)
    slot4 = k.sb("slot4", [128, 4, 32], F32)
    oh4 = k.sb("oh4", [128, 4, 32], F32)
    pr4 = k.sb("pr4", [128, 4, 32], F32)
    sj16 = k.sb("sj16", [128, 4, 4], F32)
    wj16 = k.sb("wj16", [128, 4, 4], F32)
    tok4 = k.sb("tok4", [128, 4], F32)
    rec16 = [k.sb("rec16", [128, 16, 2], I32) for _ in range(2)]
    sji16 = [k.sb("sji16", [128, 16], I32) for _ in range(2)]
    for it in range(NBLK // 4):
        b0 = it * 4
        bs = slice(b0, b0 + 4)
        r16, s16 = rec16[it % 2], sji16[it % 2]
        k.tt("dve", slot4[:, :, :], pos_all[:, bs, :], base.v(base.t[:, :].unsqueeze(1).to_broadcast([128, 4, 32])), ALU.add)
        for j in range(4):
            k.ts("dve", tok4[:, j:j + 1], iop[:, :], float((b0 + j) * 128), None, ALU.add)
        for j in range(4):
            mb = m4_all.v(m4_all.t[:, bs, j:j + 1].to_broadcast([128, 4, 32]))
            k.tt("dve", oh4[:, :, :], lg_all[:, bs, :], mb, ALU.is_equal)
            k.tt("dve", pr4[:, :, :], oh4[:, :, :], slot4[:, :, :], ALU.mult)
            k.op("dve", "tensor_reduce", sj16[:, :, j], [pr4[:, :, :]], axis=AX.X, op=ALU.add)
            k.tt("dve", pr4[:, :, :], oh4[:, :, :], comb_all[:, bs, :], ALU.mult)
            k.op("dve", "tensor_reduce", wj16[:, :, j], [pr4[:, :, :]], axis=AX.X, op=ALU.add)
        r3 = r16.v(r16.t[:, :, 0].rearrange("p (b j) -> p b j", j=4))
        k.copy("dve", r3, tok4.v(tok4.t[:, :].unsqueeze(2).to_broadcast([128, 4, 4])))
        k.copy("dve", r16.v(r16.t[:, :, 1].bitcast(F32)), wj16.v(wj16.t[:, :, :].rearrange("p b j -> p (b j)")))
        k.copy("dve", s16[:, :], sj16.v(sj16.t[:, :, :].rearrange("p b j -> p (b j)")))
        k.copy("dve", slots_all.v(slots_all.t[:, bs, :].rearrange("p b j -> p (b j)")), sj16.v(sj16.t[:, :, :].rearrange("p b j -> p (b j)")))
        for i in range(16):
            reg = k.region()
            k.idma(reg.v(S["rec"].t), r16[:, i, :], s16[:, i:i + 1], scatter=True)
    k.stage_end()

    k.stage_begin()
    wgu = [[k.sb("wgu", [128, 4, 2048], BF16) for _ in range(2)] for _ in range(2)]
    wdn = [k.sb("wdn", [128, 8, 1024], BF16) for _ in range(2)]
    widx = [k.sb("widx", [128, 2], I32) for _ in range(2)]
    bidx = [k.sb("bidx", [128, 2], I32) for _ in range(2)]
    wtmp = k.sb("wtmp", [128, 8], F32)
    wtmp2 = k.sb("wtmp2", [128, 2], F32)
    bgu = [k.sb("bgu", [128, 16], F32) for _ in range(2)]
    bdn = [k.sb("bdn", [128, 1024], F32) for _ in range(2)]
    bdnb = [k.sb("bdnb", [1, 1024], BF16) for _ in range(2)]
    onesb = k.sb("onesb", [1, 128], BF16)
    k.memset("dve", onesb[:, :], 1.0)
    recg = [[k.sb("recg", [128, 2], I32) for _ in range(4)] for _ in range(2)]
    wsc = [k.sb("wsc", [128, 1], F32) for _ in range(4)]
    hg = [[k.sb("hg", [128, 1024], BF16) for _ in range(4)] for _ in range(2)]
    hT = [k.sb("hT", [128, 8, 512], BF16) for _ in range(2)]
    actT = [k.sb("actT", [128, 8, 512], BF16) for _ in range(2)]
    tg = [k.sb("tg", [128, 512], F32) for _ in range(2)]
    tt_ = [k.sb("tt", [128, 512], F32) for _ in range(2)]
    tu = [k.sb("tu", [128, 512], F32) for _ in range(2)]
    otm = [k.sb("otm", [128, 1024], F32) for _ in range(4)]
    pg = [k.ps("pg", [128, 512]) for _ in range(2)]
    pu = [k.ps("pu", [128, 512]) for _ in range(2)]
    pd = [k.ps("pd", [128, 512]) for _ in range(2)]
    pt2 = [k.ps("pt2", [128, 1024], BF16) for _ in range(2)]
    for hh in hg:
        for t_ in hh:
            k.memset("dve", t_[:, :], 0.0)
    h_src = Buf(k, S["h_tm"].t, "dram")
    rec_src = Buf(k, S["rec"].t, "dram")

    def prefetch(g):
        b = g % 2
        k.stt(wtmp[:, 0:2], c.ones[:, 0:2], eg256[:, g:g + 1], iorow[:, 0:2], ALU.mult, ALU.add)
        k.copy("dve", widx[b][:, :], wtmp[:, 0:2])
        k.ts("dve", wtmp2[:, 0:1], iop[:, :], eg128[:, g:g + 1], None, ALU.add)
        k.copy("dve", wtmp2[:, 1:2], egrp[:, g:g + 1])
        k.copy("dve", bidx[b][:, :], wtmp2[:, :])
        k.idma(bgu[b][:, :], W["b_gu_rows"].v(W["b_gu_rows"].t), bidx[b][:, 0:1])
        k.idma(bdn[b][:, :], W["b_down"].v(W["b_down"].t), bidx[b][:, 1:2])
        for hf in range(2):
            k.idma(wgu[b][hf].v(wgu[b][hf].t[:, :, :].rearrange("p k n -> p (k n)")),
                   W["w_gu_rows"].v(W["w_gu_rows"].t), widx[b][:, hf:hf + 1])
        k.idma(wdn[b].v(wdn[b].t[:, :, :].rearrange("p k n -> p (k n)")),
               W["w_down_rows"].v(W["w_down_rows"].t), bidx[b][:, 0:1])

    def prefetch_tok(g):
        b = g % 2
        for sb_ in range(4):
            s0 = g * 512 + sb_ * 128
            k.dma("sp", recg[b][sb_][:, :], rec_src.v(rec_src.t[s0:s0 + 128, :]))
        for sb_ in range(4):
            k.idma(hg[b][sb_][:, :], h_src.v(h_src.t), recg[b][sb_][:, 0:1])

    NGX = NG
    def transposes(g):
        b = g % 2
        for ch in range(8):
            p = pt2[ch % 2]
            k.mm(p, [(p[:, sb_ * 128:(sb_ + 1) * 128], hg[b][sb_][:, ch * 128:(ch + 1) * 128], c.identb[:, :], True, True)
                     for sb_ in range(4)], transpose=True)
            k.copy("act", hT[b][:, ch, :], p[:, 0:512])

    prefetch_tok(0)
    prefetch(0)
    for g in range(NGX):
        b = g % 2
        if g + 1 < NGX:
            prefetch_tok(g + 1)
            prefetch(g + 1)
        hT_ = hT[b]
        k.ts("dve", bgu[b][:, 8:16], bgu[b][:, 8:16], 1.0, None, ALU.add)
        k.ts("dve", bdnb[b][0:1, :], bdn[b][0:1, :], ALPHA, None, ALU.mult)
        for sb_ in range(4):
            k.ts("dve", wsc[sb_][:, :], recg[b][sb_].v(recg[b][sb_].t[:, 1:2].bitcast(F32)), 1.0 / ALPHA, None, ALU.mult)
        if g == 0:
            transposes(0)
        aT = actT[b]
        for n in range(8):
            a, b_ = pg[n % 2], pu[n % 2]
            k.mm(a, [(a[:, :], wgu[b][kc // 4][:, kc % 4, n * 128:(n + 1) * 128], hT_[:, kc, :], kc == 0, kc == 7) for kc in range(8)])
            k.mm(b_, [(b_[:, :], wgu[b][kc // 4][:, kc % 4, 1024 + n * 128:1024 + (n + 1) * 128], hT_[:, kc, :], kc == 0, kc == 7)
                      for kc in range(8)])
            g_, t_, u_ = tg[n % 2], tt_[n % 2], tu[n % 2]
            k.ts("dve", g_[:, :], a[:, :], bgu[b][:, n:n + 1], 7.0, ALU.add, ALU.min)
            k.act(t_[:, :], g_[:, :], AF.Silu, scale=ALPHA)
            k.ts("dve", u_[:, :], b_[:, :], bgu[b][:, 8 + n:9 + n], -6.0, ALU.add, ALU.max)
            k.stt(aT[:, n, :], u_[:, :], 8.0, t_[:, :], ALU.min, ALU.mult)
        if g + 1 < NGX:
            transposes(g + 1)
        for sb_ in range(4):
            for half in range(2):
                p = pd[(sb_ * 2 + half) % 2]
                mms = [(p[:, :], aT[:, kc, sb_ * 128:(sb_ + 1) * 128], wdn[b][:, kc, half * 512:(half + 1) * 512],
                        kc == 0, False) for kc in range(8)]
                mms.append((p[:, :], onesb[0:1, 0:128], bdnb[b][0:1, half * 512:(half + 1) * 512], False, True))
                k.mm(p, mms)
                k.act(otm[sb_][:, half * 512:(half + 1) * 512], p[:, :], AF.Copy, scale=wsc[sb_][:, 0:1])
            s0 = g * 512 + sb_ * 128
            reg = k.region()
            k.dma("sp", reg.v(S["oslots"].t[s0:s0 + 128, :]), otm[sb_][:, :])
    k.stage_end()

    k.stage_begin()
    x32 = [k.sb("x32", [128, 512], F32) for _ in range(8)]
    yg2 = [[[k.sb("yg", [128, 1024], F32) for _ in range(4)] for _ in range(4)] for _ in range(2)]
    pc = [k.ps("pc", [128, 512]) for _ in range(8)]
    o_src = Buf(k, S["oslots"].t, "dram")
    for it in range(L // 512):
        t0 = it * 512
        load_x_tile(c, "sp", x32, xin, t0, 512)
        yg = yg2[it % 2]
        for j in range(4):
            blk = it * 4 + j
            for r in range(4):
                k.idma(yg[j][r][:, :], o_src.v(o_src.t), slots_all[:, blk, r:r + 1])
        for ch in range(8):
            p = pc[ch]
            for j in range(4):
                k.mm(p, [(p[:, j * 128:(j + 1) * 128], yg[j][r][:, ch * 128:(ch + 1) * 128], c.ident[:, :], r == 0, r == 3)
                         for r in range(4)])
            k.stt(x32[ch][:, :], p[:, :], c.mod[:, layer, 40 + ch:41 + ch], x32[ch][:, :], ALU.mult, ALU.add)
        store_x_tile(c, "sp", x32, xout, t0, 512)
    k.stage_end()
    k.stage_end()


DEPTH = 4
SEQ = 8192
BATCH = 8


def prologue_stage(c, xd, xT, Wd):
    k = c.k
    L = c.L
    depth = c.depth
    k.stage_begin()
    cl = k.sb("cl", [128, 8], F32)
    wa = [k.sb("wa", [128, 8, 1024], F32) for _ in range(2)]
    bada = k.sb("bada", [128, depth, 48], F32)
    gl = k.sb("gl", [128, depth + 1, 16], F32)
    pm = k.ps("pm", [128, 512])
    k.dma("sp", cl[:, :], Wd["c_l"].v(Wd["c_l"].t))
    k.dma("sp", bada[:, :, :], Wd["b_ada_l"].v(Wd["b_ada_l"].t))
    k.dma("sp", gl[:, :, :], Wd["g_l"].v(Wd["g_l"].t))
    k.act(cl[:, :], cl[:, :], AF.Silu)
    n = 0
    for l in range(depth):
        for pc in range(6):
            w_ = wa[n % 2]
            n += 1
            k.dma("sp", w_[:, :, :], Wd["w_ada"].v(Wd["w_ada"].t[l][:, pc * 1024:(pc + 1) * 1024]
                                                  .rearrange("(kc p) n -> p kc n", p=128)))
            for nn in range(8):
                col = l * 48 + pc * 8 + nn
                k.mm(pm, [(pm[:, col:col + 1], w_[:, kc, nn * 128:(nn + 1) * 128], cl[:, kc:kc + 1], kc == 0, kc == 7)
                          for kc in range(8)])
    k.tt("dve", c.mod.v(c.mod.t[:, 0:depth, :].rearrange("p l n -> p (l n)")), pm[:, 0:depth * 48],
         bada.v(bada.t[:, :, :].rearrange("p l n -> p (l n)")), ALU.add)
    for l in range(depth):
        k.stt(c.geff[:, l, 0:8], c.mod[:, l, 8:16], 1.0, gl[:, l, 0:8], ALU.add, ALU.mult)
        k.stt(c.geff[:, l, 8:16], c.mod[:, l, 32:40], 1.0, gl[:, l, 8:16], ALU.add, ALU.mult)
    k.copy("dve", c.geff[:, depth, 0:8], gl[:, depth, 0:8])
    k.memset("dve", c.mod[:, depth, :], 0.0)
    xtm = [k.sb("xtm", [128, 1024], F32) for _ in range(4)]
    x32 = [k.sb("x32", [128, 512], F32) for _ in range(8)]
    pt = [k.ps("pt", [128, 512]) for _ in range(2)]
    for it in range(L // 512):
        for j in range(4):
            r0 = it * 512 + j * 128
            k.dma("sp", xtm[j][:, :], xd.v(xd.t[r0:r0 + 128, :]))
        for ch in range(8):
            p = pt[ch % 2]
            k.mm(p, [(p[:, j * 128:(j + 1) * 128], xtm[j][:, ch * 128:(ch + 1) * 128], c.ident[:, :], True, True)
                     for j in range(4)], transpose=True)
            k.copy("act" if ch % 2 else "dve", x32[ch][:, :], p[:, :])
        store_x_tile(c, "sp", x32, xT, it * 512, 512)
    k.stage_end()


def final_stage(c, xT, outd):
    k = c.k
    L = c.L
    k.stage_begin()
    x32 = [k.sb("x32", [128, 512], F32) for _ in range(8)]
    sq = [k.sb("sq", [128, 512], F32) for _ in range(2)]
    rstd = k.sb("rstd", [128, 512], F32)
    ytm = [k.sb("ytm", [128, 1024], F32) for _ in range(2)]
    pst = k.ps("pst", [128, 512])
    pt = [k.ps("pt", [128, 512]) for _ in range(2)]
    n = 0
    for it in range(L // 512):
        load_x_tile(c, "sp", x32, xT, it * 512, 512)
        rmsnorm_mod(c, x32, 512, c.depth, 0, sq, rstd, pst)
        for j in range(4):
            y_ = ytm[j % 2]
            for half in range(2):
                p = pt[n % 2]
                n += 1
                k.mm(p, [(p[:, cc * 128:(cc + 1) * 128], x32[half * 4 + cc][:, j * 128:(j + 1) * 128], c.ident[:, :],
                          True, True) for cc in range(4)], transpose=True)
                k.copy("act" if half else "dve", y_[:, half * 512:(half + 1) * 512], p[:, :])
            r0 = it * 512 + j * 128
            k.dma("sp", outd.v(outd.t[r0:r0 + 128, :]), y_[:, :])
    k.stage_end()


def build_program(L=SEQ, depth=DEPTH, ne=NE):
    nc = bass.Bass("TRN2", target_bir_lowering=False)
    k = K(nc)
    c = make_ctx(k, L, depth + 1)
    c.depth = depth
    make_chunk_masks(c)
    ext = lambda name, shape: k.dram(name, shape, F32, kind="ExternalInput")
    xd = ext("x", [L, D])
    outd = k.dram("out", [L, D], F32, kind="ExternalOutput")
    xa = k.dram("xa", [8, 128, L], F32)
    xb = k.dram("xb", [8, 128, L], F32)
    NA, NB_, NC3 = (depth + 2) // 3, (depth + 1) // 3, depth // 3
    Wd = dict(c_l=ext("c_l", [128, 8]), b_ada_l=ext("b_ada_l", [128, depth, 48]), g_l=ext("g_l", [128, depth + 1, 16]),
              w_ada=ext("w_ada", [depth, D, 6 * D]))
    gla = dict(w_in=ext("gla_w_in", [NA, D, 3072]), w_gk1=ext("gla_w_gk1", [NA, D, 16]),
               w_gk2=ext("gla_w_gk2", [NA, 16, 512]), b_gk=ext("gla_b_gk", [NA, 1, 512]),
               g_onorm_l=ext("gla_g_onorm_l", [NA, 128, 2]), w_out=ext("gla_w_out", [NA, D, D]))
    ml = {}
    if NB_:
        ml = dict(w_up=ext("ml_w_up", [NB_, D, 4096]), w_down=ext("ml_w_down", [NB_, 2048, D]),
                  b_if=ext("ml_b_if", [NB_, 1, 8]))
        for n_, shp in (("bdq", [128, 16, 128]), ("bdk", [128, 16, 128]), ("bdv", [128, 16, 128]),
                        ("bdqT", [128, 16, 128]), ("bdkT", [128, 16, 128]), ("bdvT", [128, 16, 128]),
                        ("conv_l", [128, 16, 5]), ("wif_l", [128, 48, 8]), ("skip_l", [128, 16]), ("gnorm_l", [128, 16])):
            ml[n_] = ext("ml_" + n_, [NB_] + shp)
    s5 = {}
    if NC3:
        s5 = dict(w_in=ext("s5_w_in", [NC3, D, D]), w_out=ext("s5_w_out", [NC3, D, 2 * D]), d_l=ext("s5_d_l", [NC3, 128, 8]))
        for n_ in ("Ablk_re", "Ablk_im", "Lblk", "Bblk_re", "Bblk_im", "Cblk_re", "Cblk_im"):
            s5[n_] = ext("s5_" + n_, [NC3, 128, 32 * 128])
        for n_ in ("Acol_re", "Acol_im", "Lcol"):
            s5[n_] = ext("s5_" + n_, [NC3, 128, 32])
    moe = dict(w_router=ext("moe_w_router", [depth, D, 32]), b_router=ext("moe_b_router", [depth, 1, 32]),
               w_gu_rows=ext("moe_w_gu_rows", [depth, ne * 256, 4 * 2 * D]), b_gu_rows=ext("moe_b_gu_rows", [depth, ne * 128, 16]),
               w_down_rows=ext("moe_w_down_rows", [depth, ne * 128, 8 * D]), b_down=ext("moe_b_down", [depth, ne, D]))
    NG = (4 * L + ne * 511) // 512
    S = dict(h_tm=k.dram("h_tm", [L, D], BF16), rec=k.dram("rec", [NG * 512, 2], I32),
             oslots=k.dram("oslots", [NG * 512, D], F32))

    def sub(dct, j):
        return {n_: Buf(k, b.t[j], "dram") for n_, b in dct.items()}

    prologue_stage(c, xd, xa, Wd)
    for i in range(depth):
        kind, j = i % 3, i // 3
        if kind == 0:
            gla_stage(c, i, xa, xb, sub(gla, j))
        elif kind == 1:
            mlstm_stage(c, i, xa, xb, sub(ml, j))
        else:
            s5_stage(c, i, xa, xb, sub(s5, j))
        mw = sub({n_: moe[n_] for n_ in ("w_router", "b_router")}, i)
        for n_ in ("w_gu_rows", "w_down_rows", "b_gu_rows", "b_down"):
            mw[n_] = Buf(k, moe[n_].t.rearrange("l r c -> (l r) c"), "dram")
        moe_sparse_stage(c, i, xb, xa, mw, S, ne=ne, eoff=i * ne)
    final_stage(c, xa, outd)
    k.finish([outd])
    return nc, k


def host_layouts(inp, depth=DEPTH):
    f32 = lambda a: np.ascontiguousarray(np.asarray(a, dtype=np.float32))
    col = lambda v, n: f32(np.asarray(v).reshape(n, 128).T)
    sh = {}
    sh["b_ada_l"] = f32(np.asarray(inp["b_ada"]).reshape(depth, 48, 128).transpose(2, 0, 1))
    gl = np.zeros((128, depth + 1, 16), np.float32)
    for l in range(depth):
        gl[:, l, 0:8] = col(inp["g_mix"][l], 8)
        gl[:, l, 8:16] = col(inp["g_ffn"][l], 8)
    gl[:, depth, 0:8] = col(inp["g_final"], 8)
    sh["g_l"] = gl
    sh["w_ada"] = f32(inp["w_ada"])
    for n_ in ("gla_w_in", "gla_w_gk1", "gla_w_gk2", "gla_w_out", "ml_w_up", "ml_w_down", "s5_w_in", "s5_w_out",
               "moe_w_router", "moe_b_down"):
        sh[n_] = f32(inp[n_])
    na = np.asarray(inp["gla_b_gk"]).shape[0]
    sh["gla_b_gk"] = f32(np.asarray(inp["gla_b_gk"]).reshape(na, 1, 512))
    sh["gla_g_onorm_l"] = f32(np.stack([col(g, 2) for g in np.asarray(inp["gla_g_onorm"])]))
    nb = np.asarray(inp["ml_w_up"]).shape[0]
    sh["ml_b_if"] = f32(np.asarray(inp["ml_b_if"]).reshape(nb, 1, 8))
    mls = [mlstm_host_layout(*[np.asarray(inp["ml_" + n_][j]) for n_ in
                               ("conv_w", "conv_b", "w_q", "w_k", "w_v", "w_if", "skip", "g_norm")]) for j in range(nb)]
    for n_ in mls[0]:
        sh["ml_" + n_] = f32(np.stack([m[n_] for m in mls]))
    n3 = np.asarray(inp["s5_w_in"]).shape[0]
    s5s = [s5_host_layout(*[np.asarray(inp["s5_" + n_][j]) for n_ in
                            ("a_re", "a_im", "log_dt", "b_re", "b_im", "c_re", "c_im", "d")]) for j in range(n3)]
    for n_ in s5s[0]:
        sh["s5_" + n_] = f32(np.stack([m[n_] for m in s5s]))
    sh["moe_b_router"] = f32(np.asarray(inp["moe_b_router"]).reshape(depth, 1, 32))
    bgu = np.asarray(inp["moe_b_gu"])
    ne = bgu.shape[1]
    sh["moe_b_gu_rows"] = f32(bgu.reshape(depth, ne, 16, 128).transpose(0, 1, 3, 2).reshape(depth, ne * 128, 16))
    sh["moe_w_gu_rows"] = f32(np.asarray(inp["moe_w_gu"]).reshape(depth, ne, 2, 4, 128, 2 * D)
                              .transpose(0, 1, 2, 4, 3, 5)).reshape(depth, ne * 256, 4 * 2 * D)
    sh["moe_w_down_rows"] = f32(np.asarray(inp["moe_w_down"]).reshape(depth, ne, 8, 128, D)
                                .transpose(0, 1, 3, 2, 4)).reshape(depth, ne * 128, 8 * D)
    return sh


_PROG = {}


def kernel(**inputs):
    x = np.asarray(inputs["x"], dtype=np.float32)
    cvec = np.asarray(inputs["c"], dtype=np.float32)
    B, L, _ = x.shape
    key = (L,)
    if key not in _PROG:
        _PROG[key] = build_program(L=L)[0]
    nc = _PROG[key]
    sh = host_layouts(inputs)
    in_maps = []
    for b in range(B):
        m = dict(sh)
        m["x"] = np.ascontiguousarray(x[b])
        m["c_l"] = np.ascontiguousarray(cvec[b].reshape(8, 128).T)
        in_maps.append(m)
    res = run_bass_kernel_spmd(nc, in_maps, core_ids=list(range(B)))
    return np.stack([np.asarray(r["out"], dtype=np.float32) for r in res.results], axis=0)
```
